# Optimizing a Trainium2 kernel written in Bass

```python
import jax, jax.numpy as jnp
from jax import lax
import numpy as np

D_MODEL = 1024
BATCH = 16
SEQ = 2048
DEPTH = 1

D_MIX = D_MODEL
HEAD_DIM = 64
ATTN_WIDTH = D_MIX // 2
CONV_WIDTH = D_MIX - ATTN_WIDTH
N_HEADS = ATTN_WIDTH // HEAD_DIM
N_KV_HEADS = 2
GQA_GROUP = N_HEADS // N_KV_HEADS
KV_WIDTH = N_KV_HEADS * HEAD_DIM
N_CONV_GROUPS = CONV_WIDTH // HEAD_DIM
WINDOW = 128
CONV_K = 3
IN_WIDTH = ATTN_WIDTH + 2 * KV_WIDTH + 3 * CONV_WIDTH
N_KEYS = 128
N_EXPERTS = N_KEYS * N_KEYS
PEER_HEADS = 8
PEER_TOPK = 16
D_KEY = 256
D_HALF = D_KEY // 2
PEER_CHUNK = 128
EPS = 1e-6
NEG = -1e30

kernel_name = "hybrid_swa_sink_shortconv_peer_adaln"


def rms_norm(x, g):
    x32 = x.astype(jnp.float32)
    y = x32 * lax.rsqrt(jnp.mean(x32 * x32, axis=-1, keepdims=True) + EPS)
    return (y * g.astype(jnp.float32)).astype(x.dtype)


def head_rms_norm(x, g, n_groups):
    shp = x.shape
    xh = x.reshape(shp[:-1] + (n_groups, HEAD_DIM))
    y = rms_norm(xh, g.reshape(n_groups, HEAD_DIM))
    return y.reshape(shp)


def modulate(h, shift, scale):
    return h * (1 + scale[:, None, :]) + shift[:, None, :]


def sliding_window_sink_attention(q, k, v, sinks):
    bsz, s_len = q.shape[0], q.shape[1]
    nb = s_len // WINDOW
    qb = q.reshape(bsz, nb, WINDOW, N_KV_HEADS, GQA_GROUP, HEAD_DIM)
    kb = k.reshape(bsz, nb, WINDOW, N_KV_HEADS, HEAD_DIM)
    vb = v.reshape(bsz, nb, WINDOW, N_KV_HEADS, HEAD_DIM)

    def with_prev(t):
        prev = jnp.pad(t[:, :-1], ((0, 0), (1, 0), (0, 0), (0, 0), (0, 0)))
        return jnp.concatenate([prev, t], axis=2)

    kk, vv = with_prev(kb), with_prev(vb)
    s = jnp.einsum('bnqkgd,bnjkd->bnkgqj', qb, kk).astype(jnp.float32) * (HEAD_DIM ** -0.5)
    qi = jnp.arange(WINDOW)[:, None]
    kj = jnp.arange(2 * WINDOW)[None, :]
    diff = qi + WINDOW - kj
    rel_ok = (diff >= 0) & (diff < WINDOW)
    blk = jnp.arange(nb)[:, None, None]
    valid = rel_ok[None] & ((blk > 0) | (kj[None] >= WINDOW))
    s = jnp.where(valid[None, :, None, None], s, NEG)
    sink = sinks.astype(jnp.float32).reshape(N_KV_HEADS, GQA_GROUP)[None, None, :, :, None, None]
    m = jnp.maximum(jnp.max(s, axis=-1, keepdims=True), sink)
    e = jnp.exp(s - m)
    p = e / (jnp.sum(e, axis=-1, keepdims=True) + jnp.exp(sink - m))
    o = jnp.einsum('bnkgqj,bnjkd->bnqkgd', p.astype(v.dtype), vv)
    return o.reshape(bsz, s_len, ATTN_WIDTH)


def short_gated_conv(b_gate, c_gate, u, conv_w):
    z = c_gate * u
    zp = jnp.pad(z, ((0, 0), (CONV_K - 1, 0), (0, 0)))
    s_len = z.shape[1]
    conv = zp[:, 0:s_len] * conv_w[0] + zp[:, 1:s_len + 1] * conv_w[1] + zp[:, 2:s_len + 2] * conv_w[2]
    return b_gate * conv


def peer_layer(h, w_query, peer_keys1, peer_keys2, peer_u, peer_v):
    bsz, s_len, d = h.shape
    t = bsz * s_len
    hf = h.reshape(t, d)
    q = (hf @ w_query).reshape(t, PEER_HEADS, D_KEY)
    s1 = jnp.einsum('thd,hnd->thn', q[..., :D_HALF], peer_keys1).astype(jnp.float32)
    s2 = jnp.einsum('thd,hnd->thn', q[..., D_HALF:], peer_keys2).astype(jnp.float32)
    v1, i1 = lax.top_k(s1, PEER_TOPK)
    v2, i2 = lax.top_k(s2, PEER_TOPK)
    cand = (v1[..., :, None] + v2[..., None, :]).reshape(t, PEER_HEADS, PEER_TOPK * PEER_TOPK)
    sc, ci = lax.top_k(cand, PEER_TOPK)
    e1 = jnp.take_along_axis(i1, ci // PEER_TOPK, axis=-1)
    e2 = jnp.take_along_axis(i2, ci % PEER_TOPK, axis=-1)
    idx = e1 * N_KEYS + e2
    gates = jax.nn.softmax(sc, axis=-1).astype(h.dtype)

    nchunk = t // PEER_CHUNK
    hc = hf.reshape(nchunk, PEER_CHUNK, d)
    ic = idx.reshape(nchunk, PEER_CHUNK, PEER_HEADS, PEER_TOPK)
    gc = gates.reshape(nchunk, PEER_CHUNK, PEER_HEADS, PEER_TOPK)

    def chunk_fn(args):
        hx, ix, gx = args
        u = peer_u[ix]
        a = jax.nn.gelu(jnp.einsum('td,thkd->thk', hx, u), approximate=False)
        vsel = peer_v[ix]
        return jnp.einsum('thk,thkd->td', gx * a, vsel)

    out = lax.map(chunk_fn, (hc, ic, gc))
    return out.reshape(bsz, s_len, d)


def setup_inputs(seed: int = 0) -> dict:
    key = jax.random.key(seed)
    ks = jax.random.split(key, 20)
    D = D_MODEL
    f32 = jnp.float32
    return {
        "x": jax.random.normal(ks[0], (BATCH, SEQ, D), f32),
        "c": jax.random.normal(ks[1], (BATCH, D), f32),
        "w_ada": jax.random.normal(ks[2], (D, 6 * D), f32) * D ** -0.5,
        "b_ada": jax.random.normal(ks[3], (6 * D,), f32) * 0.02,
        "norm1_g": 1.0 + 0.02 * jax.random.normal(ks[4], (D,), f32),
        "w_in": jax.random.normal(ks[5], (D, IN_WIDTH), f32) * D ** -0.5,
        "b_in": jax.random.normal(ks[6], (IN_WIDTH,), f32) * 0.02,
        "attn_sinks": jax.random.normal(ks[7], (N_HEADS,), f32) * 0.5,
        "conv_w": jax.random.normal(ks[8], (CONV_K, CONV_WIDTH), f32) * CONV_K ** -0.5,
        "attn_out_g": 1.0 + 0.02 * jax.random.normal(ks[9], (ATTN_WIDTH,), f32),
        "conv_out_g": 1.0 + 0.02 * jax.random.normal(ks[10], (CONV_WIDTH,), f32),
        "w_out": jax.random.normal(ks[11], (D_MIX, D), f32) * D_MIX ** -0.5,
        "b_out": jax.random.normal(ks[12], (D,), f32) * 0.02,
        "norm2_g": 1.0 + 0.02 * jax.random.normal(ks[13], (D,), f32),
        "w_query": jax.random.normal(ks[14], (D, PEER_HEADS * D_KEY), f32) * D ** -0.5,
        "peer_keys1": jax.random.normal(ks[15], (PEER_HEADS, N_KEYS, D_HALF), f32) * D_HALF ** -0.5,
        "peer_keys2": jax.random.normal(ks[16], (PEER_HEADS, N_KEYS, D_HALF), f32) * D_HALF ** -0.5,
        "peer_u": jax.random.normal(ks[17], (N_EXPERTS, D), f32) * D ** -0.5,
        "peer_v": jax.random.normal(ks[18], (N_EXPERTS, D), f32) * PEER_HEADS ** -0.5,
        "final_g": 1.0 + 0.02 * jax.random.normal(ks[19], (D,), f32),
    }


def reference(x, c, w_ada, b_ada, norm1_g, w_in, b_in, attn_sinks, conv_w, attn_out_g,
              conv_out_g, w_out, b_out, norm2_g, w_query, peer_keys1, peer_keys2,
              peer_u, peer_v, final_g):
    bsz, s_len, _ = x.shape
    for _layer in range(DEPTH):
        ada = jax.nn.silu(c) @ w_ada + b_ada
        shift1, scale1, gate1, shift2, scale2, gate2 = jnp.split(ada, 6, axis=-1)

        h = modulate(rms_norm(x, norm1_g), shift1, scale1)
        proj = h @ w_in + b_in
        o1 = ATTN_WIDTH
        o2 = o1 + KV_WIDTH
        o3 = o2 + KV_WIDTH
        o4 = o3 + CONV_WIDTH
        o5 = o4 + CONV_WIDTH
        q = proj[..., :o1].reshape(bsz, s_len, N_HEADS, HEAD_DIM)
        k = proj[..., o1:o2].reshape(bsz, s_len, N_KV_HEADS, HEAD_DIM)
        v = proj[..., o2:o3].reshape(bsz, s_len, N_KV_HEADS, HEAD_DIM)
        b_gate = proj[..., o3:o4]
        c_gate = proj[..., o4:o5]
        u_conv = proj[..., o5:]
        attn = head_rms_norm(sliding_window_sink_attention(q, k, v, attn_sinks), attn_out_g, N_HEADS)
        conv = head_rms_norm(short_gated_conv(b_gate, c_gate, u_conv, conv_w), conv_out_g, N_CONV_GROUPS)
        mix = jnp.concatenate([attn, conv], axis=-1) @ w_out + b_out
        x = x + gate1[:, None, :] * mix

        h2 = modulate(rms_norm(x, norm2_g), shift2, scale2)
        x = x + gate2[:, None, :] * peer_layer(h2, w_query, peer_keys1, peer_keys2, peer_u, peer_v)
    return rms_norm(x, final_g)
```

```python
import numpy as np
from contextlib import ExitStack
import concourse.bass as bass
import concourse.mybir as mybir
from concourse.bass_utils import run_bass_kernel_spmd

F32 = mybir.dt.float32
BF16 = mybir.dt.bfloat16
U32 = mybir.dt.uint32
AF = mybir.ActivationFunctionType
ALU = mybir.AluOpType
AX = mybir.AxisListType

ENGS = ("tensor", "vector", "scalar", "gpsimd", "sync")
N_CORES = 8
D = 1024
EPS = 1e-6
NEGBIG = -1e30


class Prog:
    def __init__(self, nc, same_engine_sync=True):
        self.nc = nc
        self.ops = []
        self.last_w = {}
        self.readers = {}
        self.same_engine_sync = same_engine_sync
        self.dma_groups = {}

    def op(self, eng, fn, reads=(), writes=(), dma=None):
        i = len(self.ops)
        deps = set()
        for r in reads:
            if r in self.last_w:
                deps.add(self.last_w[r])
        for w in writes:
            if w in self.last_w:
                deps.add(self.last_w[w])
            for rd in self.readers.get(w, ()):
                deps.add(rd)
        deps.discard(i)
        for r in reads:
            lst = self.readers.setdefault(r, [])
            if dma is None:
                lst[:] = [q for q in lst if not (self.ops[q]["dma"] is None and self.ops[q]["eng"] == eng)]
            lst.append(i)
        for w in writes:
            self.last_w[w] = i
            self.readers[w] = []
        dma_idx = None
        if dma is not None:
            self.dma_groups[dma] = self.dma_groups.get(dma, 0) + 1
            dma_idx = self.dma_groups[dma]
        self.ops.append(dict(eng=eng, fn=fn, deps=deps, dma=dma, dma_idx=dma_idx, w=list(writes), r=list(reads)))
        return i

    def emit(self, max_ops=None):
        nc = self.nc
        if max_ops is not None:
            self.ops = self.ops[:max_ops]
            self.dma_groups = {}
            for o in self.ops:
                if o["dma"] is not None:
                    self.dma_groups[o["dma"]] = max(self.dma_groups.get(o["dma"], 0), o["dma_idx"])
        ops = self.ops
        ses = self.same_engine_sync

        def skip(p, o):
            return (p["dma"] is None and o["dma"] is None and p["eng"] == o["eng"]
                    and (p["eng"] == "tensor" or not ses))

        needs_inc = [False] * len(ops)
        for o in ops:
            for i in o["deps"]:
                p = ops[i]
                if p["dma"] is None and not skip(p, o):
                    needs_inc[i] = True
        cnt = {e: 0 for e in ENGS}
        sig = [0] * len(ops)
        for i, o in enumerate(ops):
            if o["dma"] is None and needs_inc[i]:
                cnt[o["eng"]] += 1
                sig[i] = cnt[o["eng"]]
        import os as _os
        if _os.environ.get("PROG_VERBOSE"):
            print("Prog: ops", len(ops), "sem counts", cnt, "dma", self.dma_groups)
        with ExitStack() as es:
            esem = {e: es.enter_context(nc.semaphore(f"pe_{e}")) for e in ENGS}
            dsem = {g: es.enter_context(nc.semaphore(f"pd_{g}")) for g in self.dma_groups}
            block = es.enter_context(nc.Block())
            per_eng = {e: [i for i, o in enumerate(ops) if o["eng"] == e] for e in ENGS}

            def make(ename):
                def body(eng):
                    waited = {}
                    for i in per_eng[ename]:
                        o = ops[i]
                        need = {}
                        for d in o["deps"]:
                            p = ops[d]
                            if p["dma"] is not None:
                                key = ("d", p["dma"])
                                val = 16 * p["dma_idx"]
                            else:
                                if skip(p, o):
                                    continue
                                key = ("e", p["eng"])
                                val = sig[d]
                            if val > need.get(key, 0):
                                need[key] = val
                        for key, val in need.items():
                            if waited.get(key, 0) >= val:
                                continue
                            waited[key] = val
                            s = dsem[key[1]] if key[0] == "d" else esem[key[1]]
                            eng.wait_ge(s, val)
                        ins = o["fn"](eng)
                        if o["dma"] is not None:
                            ins.then_inc(dsem[o["dma"]], 16)
                        elif needs_inc[i]:
                            ins.then_inc(esem[ename], 1)
                    if ename == "sync":
                        for g, n in self.dma_groups.items():
                            eng.wait_ge(dsem[g], 16 * n)
                        for e2 in ENGS:
                            if e2 != "sync" and cnt[e2] > 0:
                                eng.wait_ge(esem[e2], cnt[e2])
                return body

            block.tensor(make("tensor"))
            block.vector(make("vector"))
            block.scalar(make("scalar"))
            block.gpsimd(make("gpsimd"))
            block.sync(make("sync"))


P_BADA, P_G1, P_G2, P_GF, P_BOUT, P_BIN, P_AG, P_CG, P_CW, P_SINK, P_BV, NP = 0, 48, 56, 64, 72, 80, 98, 102, 106, 118, 122, 250
D_ADA, D_A1, D_GB1, D_A2, D_ESINK, ND = 0, 96, 112, 128, 144, 148
NIN = 2432


def build_nc(seq, nseq=2, tga=256, tgb=384, stop_after=9):
    ntok = seq * nseq
    TA = min(tga, seq)
    nc = bass.Bass("TRN2", target_bir_lowering=False)
    dt_in = lambda name, shape: nc.dram_tensor(name, shape, F32, kind="ExternalInput").ap()
    xT = dt_in("xT", [D, ntok])
    cT = dt_in("cT", [128, 8, nseq])
    w_ada = dt_in("w_ada", [128, 8, 6144])
    prm = dt_in("prm", [128, NP])
    w_in = dt_in("w_in", [128, 8, NIN])
    w_out = dt_in("w_out", [128, 8, D])
    w_q = dt_in("w_q", [128, 8, 2048])
    keysT = dt_in("keysT", [128, 16, 128])
    UT = dt_in("UT", [D, 16384])
    Vr = dt_in("Vr", [16384, D])
    yT = nc.dram_tensor("yT", [D, ntok], F32, kind="ExternalOutput").ap()
    UTb = nc.dram_tensor("UTb", [D, 16384], BF16, kind="Internal").ap()
    Vb = nc.dram_tensor("Vb", [16384, D], BF16, kind="Internal").ap()
    x1s = nc.dram_tensor("x1s", [128, 8, ntok], F32, kind="Internal").ap()
    h2s = nc.dram_tensor("h2s", [128, 8, ntok], BF16, kind="Internal").ap()
    sels = nc.dram_tensor("sels", [128, 3, ntok], F32, kind="Internal").ap()

    with ExitStack() as g_es:
        gsb = lambda name, shape, dt: g_es.enter_context(nc.sbuf_tensor(name, shape, dt))
        prm_sb = gsb("prm_sb", [128, NP], F32)
        drv = gsb("drv", [128, ND], F32)
        ones_bf = gsb("ones_bf", [128, 128], BF16)
        blk_bf = gsb("blk_bf", [128, 128], BF16)
        mask2 = gsb("mask2", [128, 4, 128], BF16)
        ident_f = gsb("ident_f", [128, 128], F32)
        iota_f = gsb("iota_f", [128, 128], F32)
        iota_b = gsb("iota_b", [128, 128], BF16)
        esink_bc = gsb("esink_bc", [128, 4, 128], F32)
        attng_bc = gsb("attng_bc", [128, 4, 128], F32)
        eps_t = gsb("eps_t", [128, 1], F32)

        def pcol(off, n=1):
            return prm_sb[:, off:off + n]

        def dcol(off, n=1):
            return drv[:, off:off + n]

        with ExitStack() as es:
            sb = lambda name, shape, dt: es.enter_context(nc.sbuf_tensor(name, shape, dt))
            c_sb = sb("c_sb", [128, 8, nseq], F32)
            sc = sb("sc", [128, 8, nseq], F32)
            wbuf = [sb(f"wada{i}", [128, 8, 1024], F32) for i in range(2)]
            tmpf = sb("tmpf", [128, 128], F32)
            tmp16 = sb("tmp16", [128, 8, nseq], F32)
            pa = es.enter_context(nc.psum_tensor("pa", [128, 512], F32))
            P = Prog(nc)
            P.op("sync", lambda e: e.dma_start(out=prm_sb[:], in_=prm), writes=["prm"], dma="prm")
            P.op("sync", lambda e: e.dma_start(out=c_sb[:], in_=cT), writes=["c"], dma="c")
            P.op("vector", lambda e: e.memset(ones_bf[:], 1.0), writes=["ones"])
            P.op("vector", lambda e: e.memset(blk_bf[:], 0.0), writes=["blk"])
            P.op("vector", lambda e: e.memset(blk_bf[0:64, 0:64], 1.0 / 64), reads=["blk"], writes=["blk"])
            P.op("vector", lambda e: e.memset(blk_bf[64:128, 64:128], 1.0 / 64), reads=["blk"], writes=["blk"])
            P.op("vector", lambda e: e.memset(eps_t[:], EPS), writes=["eps"])
            P.op("gpsimd", lambda e: e.iota(iota_f[:], pattern=[[1, 128]], base=0, channel_multiplier=0, allow_small_or_imprecise_dtypes=True), writes=["iota"])
            P.op("vector", lambda e: e.tensor_copy(out=iota_b[:], in_=iota_f[:]), reads=["iota"], writes=["iota_b"])
            P.op("gpsimd", lambda e: e.iota(tmpf[:], pattern=[[1, 128]], base=0, channel_multiplier=-1, allow_small_or_imprecise_dtypes=True), writes=["tmpf"])
            P.op("vector", lambda e: e.tensor_single_scalar(out=ident_f[:], in_=tmpf[:], scalar=0.0, op=ALU.is_equal), reads=["tmpf"], writes=["ident"])
            for hh in range(2):
                P.op("vector", (lambda hh: lambda e: e.tensor_single_scalar(out=mask2[:, 2 * hh, :], in_=tmpf[:], scalar=0.0, op=ALU.is_lt))(hh), reads=["tmpf"], writes=["mask"])
                P.op("vector", (lambda hh: lambda e: e.tensor_single_scalar(out=mask2[:, 2 * hh + 1, :], in_=tmpf[:], scalar=0.0, op=ALU.is_ge))(hh), reads=["tmpf"], writes=["mask"])
            P.op("scalar", lambda e: e.activation(out=sc[:], in_=c_sb[:], func=AF.Silu), reads=["c"], writes=["sc"])
            for piece in range(6):
                wb = wbuf[piece % 2]
                P.op("sync", (lambda piece, wb: lambda e: e.dma_start(out=wb[:], in_=w_ada[:, :, piece * 1024:(piece + 1) * 1024]))(piece, wb),
                     writes=[f"wb{piece % 2}"], dma=f"wada{piece % 2}")
                for jj in range(8):
                    j = piece * 8 + jj
                    for kc in range(8):
                        P.op("tensor", (lambda wb, jj, j, kc: lambda e: e.matmul(pa[:, j * nseq:(j + 1) * nseq], lhsT=wb[:, kc, jj * 128:(jj + 1) * 128],
                                                                                 rhs=sc[:, kc, :], start=(kc == 0), stop=(kc == 7)))(wb, jj, j, kc),
                             reads=[f"wb{piece % 2}", "sc"], writes=["pa"])
            ada_v = drv[:, D_ADA:D_ADA + 48 * nseq].rearrange("p (j b) -> p j b", b=nseq)
            P.op("vector", lambda e: e.tensor_tensor(out=ada_v, in0=pa[:, 0:48 * nseq].rearrange("p (j b) -> p j b", b=nseq),
                                                     in1=prm_sb[:, P_BADA:P_BADA + 48].unsqueeze(2).to_broadcast([128, 48, nseq]), op=ALU.add),
                 reads=["pa", "prm"], writes=["drv"])

            def adav(j0):
                return drv[:, D_ADA + j0 * nseq:D_ADA + (j0 + 8) * nseq].rearrange("p (j b) -> p j b", b=nseq)

            def dv(off):
                return drv[:, off:off + 8 * nseq].rearrange("p (j b) -> p j b", b=nseq)

            def pb(off):
                return prm_sb[:, off:off + 8].unsqueeze(2).to_broadcast([128, 8, nseq])
            P.op("vector", lambda e: e.tensor_scalar(out=tmp16[:], in0=adav(8), scalar1=1.0, scalar2=None, op0=ALU.add), reads=["drv"], writes=["tmp16"])
            P.op("vector", lambda e: e.tensor_tensor(out=dv(D_A1), in0=tmp16[:], in1=pb(P_G1), op=ALU.mult), reads=["tmp16", "prm"], writes=["drvA1"])
            P.op("vector", lambda e: e.tensor_tensor(out=dv(D_GB1), in0=adav(16), in1=pb(P_BOUT), op=ALU.mult), reads=["drv", "prm"], writes=["drvGB1"])
            P.op("vector", lambda e: e.tensor_scalar(out=tmp16[:], in0=adav(32), scalar1=1.0, scalar2=None, op0=ALU.add), reads=["drv", "drvA1"], writes=["tmp16"])
            P.op("vector", lambda e: e.tensor_tensor(out=dv(D_A2), in0=tmp16[:], in1=pb(P_G2), op=ALU.mult), reads=["tmp16", "prm"], writes=["drvA2"])
            P.op("scalar", lambda e: e.activation(out=drv[:, D_ESINK:D_ESINK + 4], in_=prm_sb[:, P_SINK:P_SINK + 4], func=AF.Exp), reads=["prm"], writes=["esink"])
            P.op("vector", lambda e: e.tensor_copy(out=esink_bc[:], in_=drv[:, D_ESINK:D_ESINK + 4].unsqueeze(2).to_broadcast([128, 4, 128])), reads=["esink"], writes=["esink_bc"])
            P.op("vector", lambda e: e.tensor_copy(out=attng_bc[:], in_=prm_sb[:, P_AG:P_AG + 4].unsqueeze(2).to_broadcast([128, 4, 128])), reads=["prm"], writes=["attng_bc"])
            P.emit()
        nc.all_engine_barrier()
        if stop_after < 1:
            return nc

        with ExitStack() as es:
            sb = lambda name, shape, dt: es.enter_context(nc.sbuf_tensor(name, shape, dt))
            win_sb = sb("win_sb", [128, 8, NIN], BF16)
            wout_sb = sb("wout_sb", [128, 8, D], BF16)
            wq_sb = sb("wq_sb", [128, 8, 2048], BF16)
            keys_sb = sb("keys_sb", [128, 16, 128], BF16)
            xt = [sb(f"xt{i}", [128, 8, TA], F32) for i in range(2)]
            sq = sb("sq", [128, 8, TA], BF16)
            rstd = sb("rstd", [128, TA], F32)
            tmpn = sb("tmpn", [128, 8, TA], F32)
            hT = sb("hT", [128, 8, TA], BF16)
            qT = sb("qT", [128, 4, TA], BF16)
            kT = sb("kT", [128, 2, 128 + TA], BF16)
            NB = TA // 128
            vtok = sb("vtok", [128, 1 + NB, 128], BF16)
            Bt = sb("Bt", [128, 4, TA], F32)
            zb = sb("zb", [128, 4, 2 + TA], F32)
            acc = sb("acc", [128, 4, TA], F32)
            Ct = acc
            sqc = sq[:, 0:4, :]
            rsc = sb("rsc", [128, TA], F32)
            catT = sb("catT", [128, 8, TA], BF16)
            pT = [sb(f"pT{i}", [128, 4, 128], BF16) for i in range(4)]
            t1 = sb("t1", [128, 4, 128], F32)
            yat = sb("yat", [128, 4, 128], F32)
            sqa = sb("sqa", [128, 4, 128], BF16)
            t2 = sb("t2", [128, 4, 128], F32)
            otmp = sb("otmp", [128, TA], F32)
            h2T = hT
            qpT = sb("qpT", [128, 16, TA], BF16)
            S_sb = sb("S_sb", [128, NB, 16, 128], F32)
            Vt = sb("Vt", [128, 16, 16], F32)
            It = sb("It", [128, 16, 16], U32)
            Itf = sb("Itf", [128, 16, 16], F32)
            cand = sb("cand", [128, 8, 256], F32)
            SC = sb("SC", [128, 8, 16], F32)
            CI = sb("CI", [128, 8, 16], U32)
            au = sb("au", [128, 8, 16], U32)
            bu = sb("bu", [128, 8, 16], U32)
            af_ = sb("af_", [128, 128], F32)
            bf_ = sb("bf_", [128, 128], F32)
            oh = tmpn[:].rearrange("p c t -> p (c t)").rearrange("p (a b) -> p a b", b=16)
            sel = sb("sel", [128, 3, 128], F32)
            ee = sb("ee", [128, 8, 16], F32)
            zz = sb("zz", [128, 8], F32)
            selT = sb("selT", [128, 3, 128], F32)
            ps = [es.enter_context(nc.psum_tensor(f"ps{i}", [128, 512], F32)) for i in range(8)]
            P = Prog(nc)
            for kc in range(8):
                P.op("gpsimd", (lambda kc: lambda e: e.dma_start(out=win_sb[:, kc, :], in_=w_in[:, kc, :]))(kc), writes=[f"win{kc}"], dma=f"w_in{kc}")
            P.op("gpsimd", lambda e: e.dma_start(out=keys_sb[:], in_=keysT), writes=["keys"], dma="w_k")
            for kc in range(0, 8, 2):
                P.op("gpsimd", (lambda kc: lambda e: e.dma_start(out=wout_sb[:, kc:kc + 2, :], in_=w_out[:, kc:kc + 2, :]))(kc), writes=[f"wout{kc}", f"wout{kc + 1}"], dma=f"w_out{kc}")
            for kc in range(8):
                P.op("gpsimd", (lambda kc: lambda e: e.dma_start(out=wq_sb[:, kc, :], in_=w_q[:, kc, :]))(kc), writes=[f"wq{kc}"], dma=f"w_q{kc}")
            NPC = 16
            for i in range(NPC):
                r0, r1 = i * (D // NPC), (i + 1) * (D // NPC)
                P.op("gpsimd", (lambda r0, r1: lambda e: e.dma_start(out=UTb[r0:r1, :], in_=UT[r0:r1, :]))(r0, r1), dma="cvt")
            for i in range(NPC):
                r0, r1 = i * (16384 // NPC), (i + 1) * (16384 // NPC)
                P.op("gpsimd", (lambda r0, r1: lambda e: e.dma_start(out=Vb[r0:r1, :], in_=Vr[r0:r1, :]))(r0, r1), dma="cvt")
            WIN = [f"win{k}" for k in range(8)]
            WOUT = [f"wout{k}" for k in range(8)]
            WQ = [f"wq{k}" for k in range(8)]
            ntile = ntok // TA
            tiles_per_seq = seq // TA

            def rms_stats(P, src, srckey, pbank, tag):
                P.op("scalar", lambda e: e.activation(out=sq[:], in_=src[:], func=AF.Square), reads=[srckey], writes=["sq"])
                for kc in range(8):
                    P.op("tensor", (lambda kc: lambda e: e.matmul(pbank[:, 0:TA], lhsT=ones_bf[:], rhs=sq[:, kc, :], start=(kc == 0), stop=(kc == 7)))(kc),
                         reads=["sq"], writes=[tag])
                P.op("scalar", lambda e: e.activation(out=rstd[:], in_=pbank[:, 0:TA], func=AF.Sqrt, bias=eps_t[:], scale=1.0 / D), reads=[tag], writes=["rstd"])
                P.op("vector", lambda e: e.reciprocal(out=rstd[:], in_=rstd[:]), reads=["rstd"], writes=["rstd"])

            pending = []

            def drain(n):
                for _ in range(min(n, len(pending))):
                    pending.pop(0)()

            def topk_pieces(blk, tq):
                pcs = []
                S = lambda j: S_sb[:, blk, j, :]
                sk = lambda j: f"S{blk}_{j}"
                J = range(16)
                H = range(8)
                pcs.append(lambda: [P.op("vector", (lambda j: lambda e: e.max(out=Vt[:, j, 0:8], in_=S(j)))(j), reads=[sk(j)], writes=[f"Vta{j}"]) for j in J])
                pcs.append(lambda: [P.op("vector", (lambda j: lambda e: e.max_index(out=It[:, j, 0:8], in_max=Vt[:, j, 0:8], in_values=S(j)))(j), reads=[sk(j), f"Vta{j}"], writes=[f"Ita{j}"]) for j in J])
                pcs.append(lambda: [P.op("vector", (lambda j: lambda e: e.match_replace(out=S(j), in_to_replace=Vt[:, j, 0:8], in_values=S(j), imm_value=NEGBIG))(j), reads=[sk(j), f"Vta{j}"], writes=[sk(j)]) for j in J])
                pcs.append(lambda: [P.op("vector", (lambda j: lambda e: e.max(out=Vt[:, j, 8:16], in_=S(j)))(j), reads=[sk(j)], writes=[f"Vtb{j}"]) for j in J])
                pcs.append(lambda: [P.op("vector", (lambda j: lambda e: e.max_index(out=It[:, j, 8:16], in_max=Vt[:, j, 8:16], in_values=S(j)))(j), reads=[sk(j), f"Vtb{j}"], writes=[f"Itb{j}"]) for j in J])
                VT = [f"Vta{j}" for j in J] + [f"Vtb{j}" for j in J]
                IT = [f"Ita{j}" for j in J] + [f"Itb{j}" for j in J]
                CAND = [f"cand{h}" for h in H]
                Vv = Vt[:].rearrange("p (h s) a -> p h s a", s=2)
                pcs.append(lambda: P.op("vector", lambda e: e.tensor_tensor(out=cand[:].rearrange("p h (a b) -> p h a b", a=16), in0=Vv[:, :, 0, :].unsqueeze(3).to_broadcast([128, 8, 16, 16]),
                                                                          in1=Vv[:, :, 1, :].unsqueeze(2).to_broadcast([128, 8, 16, 16]), op=ALU.add), reads=VT, writes=CAND))
                pcs.append(lambda: [P.op("vector", (lambda h: lambda e: e.max(out=SC[:, h, 0:8], in_=cand[:, h, :]))(h), reads=[f"cand{h}"], writes=[f"SCa{h}"]) for h in H])
                pcs.append(lambda: [P.op("vector", (lambda h: lambda e: e.max_index(out=CI[:, h, 0:8], in_max=SC[:, h, 0:8], in_values=cand[:, h, :]))(h), reads=[f"cand{h}", f"SCa{h}"], writes=[f"CIa{h}"]) for h in H])
                pcs.append(lambda: [P.op("vector", (lambda h: lambda e: e.match_replace(out=cand[:, h, :], in_to_replace=SC[:, h, 0:8], in_values=cand[:, h, :], imm_value=NEGBIG))(h), reads=[f"cand{h}", f"SCa{h}"], writes=[f"cand{h}"]) for h in H])
                pcs.append(lambda: [P.op("vector", (lambda h: lambda e: e.max(out=SC[:, h, 8:16], in_=cand[:, h, :]))(h), reads=[f"cand{h}"], writes=[f"SCb{h}"]) for h in H])
                pcs.append(lambda: [P.op("vector", (lambda h: lambda e: e.max_index(out=CI[:, h, 8:16], in_max=SC[:, h, 8:16], in_values=cand[:, h, :]))(h), reads=[f"cand{h}", f"SCb{h}"], writes=[f"CIb{h}"]) for h in H])
                SCK = [f"SCa{h}" for h in H] + [f"SCb{h}" for h in H]
                CIK = [f"CIa{h}" for h in H] + [f"CIb{h}" for h in H]

                def gates():
                    P.op("vector", lambda e: e.tensor_tensor(out=ee[:], in0=SC[:], in1=SC[:, :, 0:1].to_broadcast([128, 8, 16]), op=ALU.subtract), reads=SCK, writes=["ee"])
                    P.op("scalar", lambda e: e.activation(out=ee[:], in_=ee[:], func=AF.Exp), reads=["ee"], writes=["ee"])
                    P.op("vector", lambda e: e.tensor_reduce(out=zz[:], in_=ee[:], axis=AX.X, op=ALU.add), reads=["ee"], writes=["zz"])
                    P.op("vector", lambda e: e.reciprocal(out=zz[:], in_=zz[:]), reads=["zz"], writes=["zz"])
                    P.op("vector", lambda e: e.tensor_tensor(out=sel[:, 2, :].rearrange("p (h k) -> p h k", h=8), in0=ee[:], in1=zz[:].unsqueeze(2).to_broadcast([128, 8, 16]), op=ALU.mult),
                         reads=["ee", "zz"], writes=["sel2"])
                    P.op("vector", lambda e: e.tensor_single_scalar(out=au[:], in_=CI[:], scalar=4, op=ALU.logical_shift_right), reads=CIK, writes=["au"])
                    P.op("vector", lambda e: e.tensor_single_scalar(out=bu[:], in_=CI[:], scalar=15, op=ALU.bitwise_and), reads=CIK, writes=["bu"])
                    P.op("vector", lambda e: e.tensor_copy(out=af_[:], in_=au[:].rearrange("p h k -> p (h k)")), reads=["au"], writes=["af"])
                    P.op("vector", lambda e: e.tensor_copy(out=bf_[:], in_=bu[:].rearrange("p h k -> p (h k)")), reads=["bu"], writes=["bf"])
                    P.op("vector", lambda e: e.tensor_copy(out=Itf[:], in_=It[:]), reads=IT, writes=["Itf"])
                pcs.append(gates)

                def decode():
                    Iv = Itf[:].rearrange("p (h s) a -> p h s a", s=2)
                    for s_, src in ((0, af_), (1, bf_)):
                        P.op("vector", (lambda src: lambda e: e.tensor_tensor(out=oh, in0=iota_f[:, 0:16].unsqueeze(1).to_broadcast([128, 128, 16]),
                                                                              in1=src[:].unsqueeze(2).to_broadcast([128, 128, 16]), op=ALU.is_equal))(src),
                             reads=["af", "bf"], writes=["tmpn"])
                        P.op("gpsimd", (lambda s_: lambda e: e.tensor_tensor(out=oh.rearrange("p (h k) a -> p h k a", h=8), in0=oh.rearrange("p (h k) a -> p h k a", h=8),
                                                                             in1=Iv[:, :, s_, :].unsqueeze(2).to_broadcast([128, 8, 16, 16]), op=ALU.mult))(s_),
                             reads=["tmpn", "Itf"], writes=["tmpn"])
                        P.op("vector", (lambda s_: lambda e: e.tensor_reduce(out=sel[:, s_, :], in_=oh, axis=AX.X, op=ALU.add))(s_), reads=["tmpn"], writes=[f"sel{s_}"])
                    for s_ in range(3):
                        P.op("tensor", (lambda s_: lambda e: e.transpose(out=ps[6][:, s_ * 128:(s_ + 1) * 128], in_=sel[:, s_, :], identity=ident_f[:]))(s_),
                             reads=[f"sel{s_}"], writes=["ps6"])
                    P.op("scalar", lambda e: e.copy(out=selT[:].rearrange("p a b -> p (a b)"), in_=ps[6][:, 0:384]), reads=["ps6"], writes=["selT"])
                    P.op("sync", (lambda tq: lambda e: e.dma_start(out=sels[:, :, tq:tq + 128], in_=selT[:]))(tq), reads=["selT"], dma="selst")
                pcs.append(decode)
                return pcs

            for m in range(ntile):
                b = m // tiles_per_seq
                first = (m % tiles_per_seq == 0)
                tok0 = m * TA
                x = xt[m % 2]
                xk = f"xt{m % 2}"
                P.op("sync", (lambda x, tok0: lambda e: e.dma_start(out=x[:], in_=xT[:, tok0:tok0 + TA].rearrange("(c p) t -> p c t", p=128)))(x, tok0),
                     writes=[xk], dma=xk)
                rms_stats(P, x, xk, ps[7], "ps7")
                P.op("gpsimd", (lambda x: lambda e: e.tensor_tensor(out=tmpn[:], in0=x[:], in1=rstd[:].unsqueeze(1).to_broadcast([128, 8, TA]), op=ALU.mult))(x),
                     reads=[xk, "rstd"], writes=["tmpn"])
                for c in range(8):
                    P.op("scalar", (lambda c, b: lambda e: e.activation(out=hT[:, c, :], in_=tmpn[:, c, :], func=AF.Identity,
                                                                        scale=dcol(D_A1 + c * nseq + b), bias=dcol(D_ADA + (0 + c) * nseq + b)))(c, b),
                         reads=["tmpn"], writes=[f"hT{c}"])
                HT = [f"hT{c}" for c in range(8)]
                if first:
                    P.op("vector", lambda e: e.memset(zb[:, :, 0:2], 0.0), reads=["zb"], writes=["zb"])
                for j in range(18):
                    if j % 3 == 0 and j < 15:
                        drain(1)
                    pb_ = ps[j % 6]
                    pk = f"ps{j % 6}"
                    for kc in range(8):
                        P.op("tensor", (lambda pb_, j, kc: lambda e: e.matmul(pb_[:, 0:TA], lhsT=win_sb[:, kc, j * 128:(j + 1) * 128], rhs=hT[:, kc, :],
                                                                              start=(kc == 0), stop=(kc == 7)))(pb_, j, kc),
                             reads=[f"win{kc}", f"hT{kc}"], writes=[pk])
                    bias = pcol(P_BIN + j)
                    if j < 4:
                        P.op("scalar", (lambda pb_, j, bias: lambda e: e.activation(out=qT[:, j, :], in_=pb_[:, 0:TA], func=AF.Identity, bias=bias))(pb_, j, bias),
                             reads=[pk], writes=["qT"])
                    elif j < 6:
                        P.op("scalar", (lambda pb_, j, bias: lambda e: e.activation(out=kT[:, j - 4, 128:128 + TA], in_=pb_[:, 0:TA], func=AF.Identity, bias=bias))(pb_, j, bias),
                             reads=[pk], writes=["kTcur"])
                    elif j < 10:
                        P.op("scalar", (lambda pb_, j, bias: lambda e: e.activation(out=Bt[:, j - 6, :], in_=pb_[:, 0:TA], func=AF.Identity, bias=bias))(pb_, j, bias),
                             reads=[pk], writes=["Bt"])
                    elif j < 14:
                        P.op("scalar", (lambda pb_, j, bias: lambda e: e.activation(out=Ct[:, j - 10, :], in_=pb_[:, 0:TA], func=AF.Identity, bias=bias))(pb_, j, bias),
                             reads=[pk], writes=[f"acc{j - 10}"])
                    else:
                        P.op("vector", (lambda pb_, j, bias: lambda e: e.scalar_tensor_tensor(out=zb[:, j - 14, 2:2 + TA], in0=pb_[:, 0:TA], scalar=bias,
                                                                                               in1=Ct[:, j - 14, :], op0=ALU.add, op1=ALU.mult))(pb_, j, bias),
                             reads=[pk, f"acc{j - 14}"], writes=["zb"])
                for blk in range(NB):
                    pb_ = ps[blk % 2]
                    pk = f"ps{blk % 2}"
                    for kc in range(8):
                        P.op("tensor", (lambda pb_, blk, kc: lambda e: e.matmul(pb_[:, 0:128], lhsT=hT[:, kc, blk * 128:(blk + 1) * 128], rhs=win_sb[:, kc, 2304:2432],
                                                                                start=(kc == 0), stop=(kc == 7)))(pb_, blk, kc),
                             reads=[f"win{kc}", f"hT{kc}"], writes=[pk])
                    P.op("vector", (lambda pb_, blk: lambda e: e.tensor_tensor(out=vtok[:, 1 + blk, :], in0=pb_[:, 0:128], in1=prm_sb[:, P_BV:P_BV + 128], op=ALU.add))(pb_, blk),
                         reads=[pk], writes=["vcur"])
                for cc in range(4):
                    P.op("scalar", (lambda cc: lambda e: e.activation(out=acc[:, cc, :], in_=zb[:, cc, 2:2 + TA], func=AF.Copy, scale=pcol(P_CW + 2 * 4 + cc)))(cc),
                         reads=["zb"], writes=[f"acc{cc}"])
                    P.op("vector", (lambda cc: lambda e: e.scalar_tensor_tensor(out=acc[:, cc, :], in0=zb[:, cc, 1:1 + TA], scalar=pcol(P_CW + 1 * 4 + cc), in1=acc[:, cc, :],
                                                                                op0=ALU.mult, op1=ALU.add))(cc), reads=["zb", f"acc{cc}"], writes=[f"acc{cc}"])
                    P.op("vector", (lambda cc: lambda e: e.scalar_tensor_tensor(out=acc[:, cc, :], in0=zb[:, cc, 0:TA], scalar=pcol(P_CW + 0 * 4 + cc), in1=acc[:, cc, :],
                                                                                op0=ALU.mult, op1=ALU.add))(cc), reads=["zb", f"acc{cc}"], writes=[f"acc{cc}"])
                    P.op("gpsimd", (lambda cc: lambda e: e.tensor_tensor(out=acc[:, cc, :], in0=acc[:, cc, :], in1=Bt[:, cc, :], op=ALU.mult))(cc),
                         reads=["Bt", f"acc{cc}"], writes=[f"acc{cc}"])
                ACC = [f"acc{cc}" for cc in range(4)]
                P.op("gpsimd", lambda e: e.tensor_copy(out=zb[:, :, 0:2], in_=zb[:, :, TA:TA + 2]), reads=["zb"] + ACC, writes=["zb"])
                P.op("scalar", lambda e: e.activation(out=sqc, in_=acc[:], func=AF.Square), reads=ACC, writes=["sq"])
                for cc in range(4):
                    pb_ = ps[2 + cc % 2]
                    pk = f"ps{2 + cc % 2}"
                    P.op("tensor", (lambda pb_, cc: lambda e: e.matmul(pb_[:, 0:TA], lhsT=blk_bf[:], rhs=sqc[:, cc, :], start=True, stop=True))(pb_, cc),
                         reads=["sq"], writes=[pk])
                    P.op("scalar", (lambda pb_: lambda e: e.activation(out=rsc[:], in_=pb_[:, 0:TA], func=AF.Sqrt, bias=eps_t[:], scale=1.0))(pb_), reads=[pk], writes=["rsc"])
                    P.op("vector", lambda e: e.reciprocal(out=rsc[:], in_=rsc[:]), reads=["rsc"], writes=["rsc"])
                    P.op("vector", (lambda cc: lambda e: e.scalar_tensor_tensor(out=catT[:, 4 + cc, :], in0=acc[:, cc, :], scalar=pcol(P_CG + cc), in1=rsc[:],
                                                                                op0=ALU.mult, op1=ALU.mult))(cc), reads=[f"acc{cc}", "rsc"], writes=[f"cat{4 + cc}"])
                for blk in range(NB):
                    hasprev = not (first and blk == 0)
                    q0 = blk * 128
                    kprev = slice(q0, q0 + 128)
                    kcur = slice(128 + q0, 256 + q0)
                    vprev, vcur = blk, blk + 1
                    po = ps[4]
                    pd = ps[5]
                    for pp in range(2):
                        g = pp
                        for ci in range(2):
                            c = 2 * pp + ci
                            for hh in range(2):
                                lo, hi = hh * 64, hh * 64 + 64
                                bank = ps[2 * pp + hh]
                                pk = f"ps{2 * pp + hh}"
                                if hasprev:
                                    P.op("tensor", (lambda bank, ci, lo, hi, g, c, kprev, q0: lambda e: e.matmul(bank[:, ci * 256:ci * 256 + 128], lhsT=kT[lo:hi, g, kprev],
                                                                                                               rhs=qT[lo:hi, c, q0:q0 + 128], start=True, stop=True))(bank, ci, lo, hi, g, c, kprev, q0),
                                         reads=["qT", "kTcur", "kTprev"], writes=[pk])
                                P.op("tensor", (lambda bank, ci, lo, hi, g, c, kcur, q0: lambda e: e.matmul(bank[:, ci * 256 + 128:ci * 256 + 256], lhsT=kT[lo:hi, g, kcur],
                                                                                                          rhs=qT[lo:hi, c, q0:q0 + 128], start=True, stop=True))(bank, ci, lo, hi, g, c, kcur, q0),
                                     reads=["qT", "kTcur", "kTprev"], writes=[pk])
                        drain(1)
                        for hh in range(2):
                            bank = ps[2 * pp + hh]
                            pk = f"ps{2 * pp + hh}"
                            pt = pT[2 * pp + hh]
                            ptk = f"pT{2 * pp + hh}"
                            if hasprev:
                                P.op("scalar", (lambda bank, pt: lambda e: e.activation(out=pt[:].rearrange("p a b -> p (a b)"), in_=bank[:, :], func=AF.Exp, scale=0.125))(bank, pt),
                                     reads=[pk], writes=[ptk])
                                P.op("gpsimd", (lambda pt: lambda e: e.tensor_tensor(out=pt[:], in0=pt[:], in1=mask2[:], op=ALU.mult))(pt), reads=[ptk], writes=[ptk])
                            else:
                                psv = bank[:, :].rearrange("p (h a i) -> p h a i", h=2, a=2)[:, :, 1, :]
                                ptv = pt[:].rearrange("p (h a) i -> p h a i", h=2)[:, :, 1, :]
                                mkv = mask2[:].rearrange("p (h a) i -> p h a i", h=2)[:, :, 1, :]
                                P.op("scalar", (lambda psv, ptv: lambda e: e.activation(out=ptv, in_=psv, func=AF.Exp, scale=0.125))(psv, ptv), reads=[pk], writes=[ptk])
                                P.op("gpsimd", (lambda ptv, mkv: lambda e: e.tensor_tensor(out=ptv, in0=ptv, in1=mkv, op=ALU.mult))(ptv, mkv), reads=[ptk], writes=[ptk])
                        for ci in range(2):
                            c = 2 * pp + ci
                            for hh in range(2):
                                lo, hi = hh * 64, hh * 64 + 64
                                pt = pT[2 * pp + hh]
                                ptk = f"pT{2 * pp + hh}"
                                oc = slice(c * 128, (c + 1) * 128)
                                if hasprev:
                                    P.op("tensor", (lambda pt, ci, lo, hi, g, oc, vprev: lambda e: e.matmul(po[lo:hi, oc], lhsT=vtok[:, vprev, g * 64:(g + 1) * 64], rhs=pt[:, 2 * ci, :],
                                                                                                          start=True, stop=False))(pt, ci, lo, hi, g, oc, vprev),
                                         reads=[ptk, "vcur", "vprev"], writes=["ps4"])
                                P.op("tensor", (lambda pt, ci, lo, hi, g, oc, vcur, hasprev: lambda e: e.matmul(po[lo:hi, oc], lhsT=vtok[:, vcur, g * 64:(g + 1) * 64], rhs=pt[:, 2 * ci + 1, :],
                                                                                                              start=(not hasprev), stop=True))(pt, ci, lo, hi, g, oc, vcur, hasprev),
                                     reads=[ptk, "vcur", "vprev"], writes=["ps4"])
                                if hasprev:
                                    P.op("tensor", (lambda pt, ci, lo, hi, oc: lambda e: e.matmul(pd[lo:hi, oc], lhsT=ones_bf[:, 0:64], rhs=pt[:, 2 * ci, :], start=True, stop=False))(pt, ci, lo, hi, oc),
                                         reads=[ptk], writes=["ps5"])
                                P.op("tensor", (lambda pt, ci, lo, hi, oc, hasprev: lambda e: e.matmul(pd[lo:hi, oc], lhsT=ones_bf[:, 0:64], rhs=pt[:, 2 * ci + 1, :], start=(not hasprev), stop=True))(pt, ci, lo, hi, oc, hasprev),
                                     reads=[ptk], writes=["ps5"])
                    pov = po[:, :].rearrange("p (c i) -> p c i", c=4)
                    pdv = pd[:, :].rearrange("p (c i) -> p c i", c=4)
                    P.op("vector", lambda e: e.tensor_tensor(out=t1[:], in0=pdv, in1=esink_bc[:], op=ALU.add), reads=["ps5"], writes=["t1"])
                    P.op("vector", lambda e: e.reciprocal(out=t1[:], in_=t1[:]), reads=["t1"], writes=["t1"])
                    P.op("vector", lambda e: e.tensor_tensor(out=yat[:], in0=pov, in1=t1[:], op=ALU.mult), reads=["ps4", "t1"], writes=["yat"])
                    P.op("scalar", lambda e: e.activation(out=sqa[:], in_=yat[:], func=AF.Square), reads=["yat"], writes=["sqa"])
                    P.op("tensor", lambda e: e.matmul(ps[6][:, :], lhsT=blk_bf[:], rhs=sqa[:].rearrange("p c i -> p (c i)"), start=True, stop=True), reads=["sqa"], writes=["ps6"])
                    P.op("scalar", lambda e: e.activation(out=t2[:].rearrange("p c i -> p (c i)"), in_=ps[6][:, :], func=AF.Sqrt, bias=eps_t[:], scale=1.0), reads=["ps6"], writes=["t2"])
                    P.op("vector", lambda e: e.reciprocal(out=t2[:], in_=t2[:]), reads=["t2"], writes=["t2"])
                    P.op("vector", lambda e: e.tensor_tensor(out=yat[:], in0=yat[:], in1=t2[:], op=ALU.mult), reads=["yat", "t2"], writes=["yat"])
                    P.op("vector", (lambda q0: lambda e: e.tensor_tensor(out=catT[:, 0:4, q0:q0 + 128], in0=yat[:], in1=attng_bc[:], op=ALU.mult))(q0),
                         reads=["yat"], writes=[f"cat{c}" for c in range(4)])
                P.op("gpsimd", lambda e: e.tensor_copy(out=kT[:, :, 0:128], in_=kT[:, :, TA:TA + 128]), reads=["kTcur", "kTprev"], writes=["kTprev", "kTcur"])
                P.op("gpsimd", lambda e: e.tensor_copy(out=vtok[:, 0, :], in_=vtok[:, NB, :]), reads=["vcur", "vprev"], writes=["vprev", "vcur"])
                CAT = [f"cat{c}" for c in range(8)]
                for j in range(8):
                    if j % 2 == 0:
                        drain(1)
                    pb_ = ps[j % 6]
                    pk = f"ps{j % 6}"
                    for kc in range(8):
                        P.op("tensor", (lambda pb_, j, kc: lambda e: e.matmul(pb_[:, 0:TA], lhsT=wout_sb[:, kc, j * 128:(j + 1) * 128], rhs=catT[:, kc, :], start=(kc == 0), stop=(kc == 7)))(pb_, j, kc),
                             reads=[f"wout{kc}", f"cat{kc}"], writes=[pk])
                    P.op("scalar", (lambda pb_, j, b: lambda e: e.activation(out=otmp[:], in_=pb_[:, 0:TA], func=AF.Identity, scale=dcol(D_ADA + (16 + j) * nseq + b),
                                                                             bias=dcol(D_GB1 + j * nseq + b)))(pb_, j, b), reads=[pk], writes=["otmp"])
                    P.op("gpsimd", (lambda x, j: lambda e: e.tensor_tensor(out=x[:, j, :], in0=x[:, j, :], in1=otmp[:], op=ALU.add))(x, j), reads=["otmp", xk], writes=[xk])
                P.op("sync", (lambda x, tok0: lambda e: e.dma_start(out=x1s[:, :, tok0:tok0 + TA], in_=x[:]))(x, tok0), reads=[xk], dma=f"x1st{m % 2}")
                rms_stats(P, x, xk, ps[7], "ps7")
                P.op("gpsimd", (lambda x: lambda e: e.tensor_tensor(out=tmpn[:], in0=x[:], in1=rstd[:].unsqueeze(1).to_broadcast([128, 8, TA]), op=ALU.mult))(x),
                     reads=[xk, "rstd"], writes=["tmpn"])
                for c in range(8):
                    P.op("scalar", (lambda c, b: lambda e: e.activation(out=h2T[:, c, :], in_=tmpn[:, c, :], func=AF.Identity,
                                                                        scale=dcol(D_A2 + c * nseq + b), bias=dcol(D_ADA + (24 + c) * nseq + b)))(c, b),
                         reads=["tmpn"], writes=[f"hT{c}"])
                H2 = [f"hT{c}" for c in range(8)]
                P.op("sync", (lambda tok0: lambda e: e.dma_start(out=h2s[:, :, tok0:tok0 + TA], in_=h2T[:]))(tok0), reads=H2, dma="h2st")
                for j in range(16):
                    if j % 2 == 0:
                        drain(1)
                    pb_ = ps[j % 6]
                    pk = f"ps{j % 6}"
                    for kc in range(8):
                        P.op("tensor", (lambda pb_, j, kc: lambda e: e.matmul(pb_[:, 0:TA], lhsT=wq_sb[:, kc, j * 128:(j + 1) * 128], rhs=h2T[:, kc, :], start=(kc == 0), stop=(kc == 7)))(pb_, j, kc),
                             reads=[f"wq{kc}", f"hT{kc}"], writes=[pk])
                    P.op("scalar", (lambda pb_, j: lambda e: e.copy(out=qpT[:, j, :], in_=pb_[:, 0:TA]))(pb_, j), reads=[pk], writes=[f"qp{j}"])
                drain(len(pending))
                for blk in range(NB):
                    q0 = blk * 128
                    for j4 in range(4):
                        pb_ = ps[2 + j4]
                        pk = f"ps{2 + j4}"
                        for jj in range(4):
                            j = j4 * 4 + jj
                            P.op("tensor", (lambda pb_, j, jj, q0: lambda e: e.matmul(pb_[:, jj * 128:(jj + 1) * 128], lhsT=qpT[:, j, q0:q0 + 128], rhs=keys_sb[:, j, :], start=True, stop=True))(pb_, j, jj, q0),
                                 reads=[f"qp{j}", "keys"], writes=[pk])
                        P.op("scalar", (lambda pb_, j4, blk: lambda e: e.copy(out=S_sb[:, blk, j4 * 4:(j4 + 1) * 4, :].rearrange("p a b -> p (a b)"), in_=pb_[:, :]))(pb_, j4, blk),
                             reads=[pk], writes=[f"S{blk}_{j4 * 4 + jj}" for jj in range(4)])
                for blk in range(NB):
                    pending.extend(topk_pieces(blk, tok0 + blk * 128))
            while pending:
                pending.pop(0)()
            import os as _os
            _mx = _os.environ.get("PROG_MAXOPS")
            print("phase A ops", len(P.ops))
            if _mx:
                for _i, _o in enumerate(P.ops[:int(_mx)][-3:]):
                    print("last ops", _o["eng"], _o["dma"])
            P.emit(int(_mx) if _mx else None)
        nc.all_engine_barrier()
        if stop_after < 2:
            return nc

        with ExitStack() as es:
            sb = lambda name, shape, dt: es.enter_context(nc.sbuf_tensor(name, shape, dt))
            TG = min(tgb, ntok)
            Gbuf = sb("Gbuf", [128, TG, 128], BF16)
            CPD = 4
            utb = [sb(f"utb{i}", [128, 8, CPD * 128], BF16) for i in range(2)]
            vbb = [sb(f"vbb{i}", [128, CPD, D], BF16) for i in range(2)]
            h2g = sb("h2g", [128, 8, TG], BF16)
            x1g = sb("x1g", [128, 8, TG], F32)
            selg = [sb(f"selg{i}", [128, 3, TG], F32) for i in range(2)]
            NAB = 16
            A1 = [sb(f"A1_{i}", [128, 128], BF16) for i in range(NAB)]
            A2 = [sb(f"A2_{i}", [128, 64], BF16) for i in range(NAB)]
            ag = [sb(f"ag{i}", [128, TG], BF16) for i in range(2)]
            wT = [sb(f"wT{i}", [128, TG], BF16) for i in range(2)]
            sq2 = sb("sq2", [128, 8, TG], BF16)
            rstd2 = sb("rstd2", [128, TG], F32)
            peer_sb = sb("peer_sb", [128, TG // 128, D], F32)
            BK = [es.enter_context(nc.psum_tensor(f"bk{i}", [128, 512], F32)) for i in range(8)]
            pm = [BK[6]]
            P = Prog(nc)
            groups = []
            t0 = 0
            while t0 < ntok:
                n = min(TG, ntok - t0)
                groups.append((t0, n))
                t0 += n
            ndma = 0
            bkk = lambda j: [f"bk{j}"]
            cnt_tok = [0]
            cnt_reg = [0]

            def load_sel(gi):
                g0, gn = groups[gi]
                sg = selg[gi % 2]
                P.op("sync", (lambda sg, g0, gn: lambda e: e.dma_start(out=sg[:, :, 0:gn], in_=sels[:, :, g0:g0 + gn]))(sg, g0, gn), writes=[f"selg{gi % 2}"], dma=f"selg{gi % 2}")

            def build_pieces(gi, h):
                g0, gn = groups[gi]
                sg = selg[gi % 2]
                skey = f"selg{gi % 2}"
                out = []
                for tp in range(0, gn, 2):
                    toks = []
                    for t in (tp, tp + 1):
                        i = cnt_tok[0] % NAB
                        cnt_tok[0] += 1
                        toks.append((t, i))
                    def part_a(toks=toks):
                        for (t, i) in toks:
                            P.op("vector", (lambda t, i: lambda e: e.tensor_scalar(out=A2[i][:], in0=iota_b[:, 64 * h:64 * h + 64], scalar1=sg[:, 1, t:t + 1], scalar2=None, op0=ALU.is_equal))(t, i),
                                 reads=[skey], writes=[f"A2_{i}"])
                            if t % 10 < 7:
                                P.op("vector", (lambda t, i: lambda e: e.tensor_scalar(out=A1[i][:], in0=iota_b[:], scalar1=sg[:, 0, t:t + 1], scalar2=None, op0=ALU.is_equal))(t, i),
                                     reads=[skey], writes=[f"A1_{i}"])
                                P.op("scalar", (lambda t, i: lambda e: e.activation(out=A1[i][:], in_=A1[i][:], func=AF.Copy, scale=sg[:, 2, t:t + 1]))(t, i),
                                     reads=[skey, f"A1_{i}"], writes=[f"A1_{i}"])
                            else:
                                P.op("vector", (lambda t, i: lambda e: e.tensor_scalar(out=A1[i][:], in0=iota_b[:], scalar1=sg[:, 0, t:t + 1], scalar2=sg[:, 2, t:t + 1], op0=ALU.is_equal, op1=ALU.mult))(t, i),
                                     reads=[skey], writes=[f"A1_{i}"])
                    out.append((part_a, toks, h))
                return out

            pend_a = []
            pend_b = []

            def emit_b():
                k = 0
                runs = []
                for (toks, h) in pend_b:
                    for (t, i) in toks:
                        P.op("tensor", (lambda k, i: lambda e: e.matmul(BK[7][:, k * 64:(k + 1) * 64], lhsT=A1[i][:], rhs=A2[i][:], start=True, stop=True))(k, i),
                             reads=[f"A1_{i}", f"A2_{i}"], writes=["bk7"])
                        if runs and runs[-1][0] == h and runs[-1][1] + runs[-1][2] == t:
                            runs[-1][2] += 1
                        else:
                            runs.append([h, t, 1, k])
                        k += 1
                for (h, t0_, n_, k0) in runs:
                    P.op("scalar", (lambda h, t0_, n_, k0: lambda e: e.copy(out=Gbuf[:, t0_:t0_ + n_, 64 * h:64 * h + 64], in_=BK[7][:, k0 * 64:(k0 + n_) * 64].rearrange("p (a b) -> p a b", a=n_)))(h, t0_, n_, k0),
                         reads=["bk7"], writes=[f"Gbuf{h}"])
                del pend_b[:]

            def build_step(n):
                if pend_b:
                    emit_b()
                for _ in range(min(n, 4, len(pend_a))):
                    pa_, toks, h = pend_a.pop(0)
                    pa_()
                    pend_b.append((toks, h))

            def build_flush():
                while pend_a or pend_b:
                    build_step(4)

            load_sel(0)
            pend_a.extend(build_pieces(0, 0))
            pend_a.extend(build_pieces(0, 1))
            build_flush()
            for gi, (g0, gn) in enumerate(groups):
                P.op("sync", (lambda g0, gn: lambda e: e.dma_start(out=h2g[:, :, 0:gn], in_=h2s[:, :, g0:g0 + gn]))(g0, gn), writes=["h2g"], dma="h2g")
                P.op("sync", (lambda g0, gn: lambda e: e.dma_start(out=x1g[:, :, 0:gn], in_=x1s[:, :, g0:g0 + gn]))(g0, gn), writes=["x1g"], dma="x1g")
                if gi + 1 < len(groups):
                    load_sel(gi + 1)
                if gi > 0:
                    pend_a.extend(build_pieces(gi, 1))
                per_slot = 3

                def slot_of(c, gi=gi):
                    return (c // CPD + gi * (128 // CPD)) % 2

                def emit_dma(c):
                    slot = slot_of(c)
                    e0 = c * 128
                    P.op("sync", (lambda slot, e0: lambda e: e.dma_start(out=utb[slot][:], in_=UTb[:, e0:e0 + CPD * 128].rearrange("(k p) n -> p k n", p=128)))(slot, e0),
                         writes=[f"utb{slot}"], dma=f"utb{slot}")
                    P.op("sync", (lambda slot, e0: lambda e: e.dma_start(out=vbb[slot][:], in_=Vb[e0:e0 + CPD * 128, :].rearrange("(a p) n -> p a n", p=128)))(slot, e0),
                         writes=[f"vbb{slot}"], dma=f"vbb{slot}")

                def emit_m1(c, gn=gn):
                    slot = slot_of(c)
                    ci = c % CPD
                    for kc in range(8):
                        P.op("tensor", (lambda slot, ci, kc, gn: lambda e: e.matmul(pm[0][:, 0:gn], lhsT=utb[slot][:, kc, ci * 128:(ci + 1) * 128], rhs=h2g[:, kc, 0:gn], start=(kc == 0), stop=(kc == 7)))(slot, ci, kc, gn),
                             reads=[f"utb{slot}", "h2g"], writes=["bk6"])

                def emit_act(c, gn=gn):
                    a_, w_ = ag[c % 2], wT[c % 2]
                    P.op("scalar", (lambda a_, gn: lambda e: e.activation(out=a_[:, 0:gn], in_=pm[0][:, 0:gn], func=AF.Gelu))(a_, gn), reads=["bk6"], writes=[f"ag{c % 2}"])
                    P.op("vector", (lambda a_, w_, c, gn: lambda e: e.tensor_tensor(out=w_[:, 0:gn], in0=a_[:, 0:gn], in1=Gbuf[:, 0:gn, c], op=ALU.mult))(a_, w_, c, gn),
                         reads=[f"ag{c % 2}", f"Gbuf{c // 64}"], writes=[f"wT{c % 2}"])

                def emit_m2(c, gn=gn):
                    slot = slot_of(c)
                    ci = c % CPD
                    w_ = wT[c % 2]
                    for tt in range(gn // 128):
                        for dh in range(2):
                            bi = tt * 2 + dh
                            P.op("tensor", (lambda w_, slot, ci, tt, dh, bi, c: lambda e: e.matmul(BK[bi][:, :], lhsT=w_[:, tt * 128:(tt + 1) * 128], rhs=vbb[slot][:, ci, dh * 512:(dh + 1) * 512],
                                                                                                  start=(c == 0), stop=(c == 127)))(w_, slot, ci, tt, dh, bi, c),
                                 reads=[f"vbb{slot}", f"wT{c % 2}"], writes=[f"bk{bi}"])

                for c in range(128):
                    if c == 64:
                        build_flush()
                        if gi + 1 < len(groups):
                            pend_a.extend(build_pieces(gi + 1, 0))
                    if c % CPD == 0:
                        emit_dma(c)
                    emit_m1(c)
                    if c > 0:
                        emit_m2(c - 1)
                    emit_act(c)
                    build_step(per_slot)
                emit_m2(127)
                build_flush()
                for tt in range(gn // 128):
                    for dh in range(2):
                        bi = tt * 2 + dh
                        if bi % 2 == 0:
                            P.op("scalar", (lambda tt, dh, bi: lambda e: e.copy(out=peer_sb[:, tt, dh * 512:(dh + 1) * 512], in_=BK[bi][:, :]))(tt, dh, bi), reads=[f"bk{bi}"], writes=[f"peer{tt}"])
                        else:
                            P.op("vector", (lambda tt, dh, bi: lambda e: e.tensor_copy(out=peer_sb[:, tt, dh * 512:(dh + 1) * 512], in_=BK[bi][:, :]))(tt, dh, bi), reads=[f"bk{bi}"], writes=[f"peer{tt}"])
                for j in range(8):
                    for tt in range(gn // 128):
                        P.op("tensor", (lambda j, tt: lambda e: e.transpose(out=BK[j][:, tt * 128:(tt + 1) * 128], in_=peer_sb[:, tt, j * 128:(j + 1) * 128], identity=ident_f[:]))(j, tt),
                             reads=[f"peer{tt}"], writes=bkk(j))
                segs = []
                tcur = g0
                while tcur < g0 + gn:
                    bb = tcur // seq
                    tend = min((bb + 1) * seq, g0 + gn)
                    segs.append((bb, tcur - g0, tend - g0))
                    tcur = tend
                for j in range(8):
                    for (bb, s0, s1) in segs:
                        P.op("vector", (lambda j, bb, s0, s1: lambda e: e.scalar_tensor_tensor(out=x1g[:, j, s0:s1], in0=BK[j][:, s0:s1], scalar=dcol(D_ADA + (40 + j) * nseq + bb),
                                                                                               in1=x1g[:, j, s0:s1], op0=ALU.mult, op1=ALU.add))(j, bb, s0, s1),
                             reads=bkk(j) + ["x1g"], writes=["x1g"])
                P.op("scalar", (lambda gn: lambda e: e.activation(out=sq2[:, :, 0:gn], in_=x1g[:, :, 0:gn], func=AF.Square))(gn), reads=["x1g"], writes=["sq2"])
                for kc in range(8):
                    P.op("tensor", (lambda kc, gn: lambda e: e.matmul(pm[0][:, 0:gn], lhsT=ones_bf[:], rhs=sq2[:, kc, 0:gn], start=(kc == 0), stop=(kc == 7)))(kc, gn), reads=["sq2"], writes=["bk6"])
                P.op("scalar", (lambda gn: lambda e: e.activation(out=rstd2[:, 0:gn], in_=pm[0][:, 0:gn], func=AF.Sqrt, bias=eps_t[:], scale=1.0 / D))(gn), reads=["bk6"], writes=["rstd2"])
                P.op("vector", (lambda gn: lambda e: e.reciprocal(out=rstd2[:, 0:gn], in_=rstd2[:, 0:gn]))(gn), reads=["rstd2"], writes=["rstd2"])
                P.op("vector", (lambda gn: lambda e: e.tensor_tensor(out=x1g[:, :, 0:gn], in0=x1g[:, :, 0:gn], in1=rstd2[:, 0:gn].unsqueeze(1).to_broadcast([128, 8, gn]), op=ALU.mult))(gn),
                     reads=["x1g", "rstd2"], writes=["x1g"])
                for j in range(8):
                    P.op("scalar", (lambda j, gn: lambda e: e.activation(out=x1g[:, j, 0:gn], in_=x1g[:, j, 0:gn], func=AF.Copy, scale=pcol(P_GF + j)))(j, gn), reads=["x1g"], writes=["x1g"])
                P.op("sync", (lambda g0, gn: lambda e: e.dma_start(out=yT[:, g0:g0 + gn].rearrange("(c p) t -> p c t", p=128), in_=x1g[:, :, 0:gn]))(g0, gn), reads=["x1g"], dma="yst")
            import os as _os2
            _mxb = _os2.environ.get("PROG_MAXOPS_B")
            print("phase B ops", len(P.ops))
            if _mxb:
                for _i, _o in enumerate(P.ops[:int(_mxb)]):
                    if _i >= int(_mxb) - 14:
                        print("OP", _i, _o["eng"], _o["dma"], "r", _o["r"], "w", _o["w"], "deps", sorted(_o["deps"]))
            P.emit(int(_mxb) if _mxb else None)
    return nc


def host_layout(inputs, core, seq, nseq):
    x = np.asarray(inputs["x"])
    c = np.asarray(inputs["c"])
    b0 = core * nseq
    ntok = seq * nseq
    xT = np.ascontiguousarray(x[b0:b0 + nseq, :seq].reshape(ntok, D).T)
    cT = np.ascontiguousarray(c[b0:b0 + nseq].reshape(nseq, 8, 128).transpose(2, 1, 0))
    return xT, cT


def shared_layout(inputs):
    f = lambda k: np.asarray(inputs[k], dtype=np.float32)
    kcp = lambda w: np.ascontiguousarray(w.reshape(8, 128, w.shape[1]).transpose(1, 0, 2))
    colT = lambda v, n: v.reshape(n, 128).T
    w_in = f("w_in")
    b_in = f("b_in")
    q_c = list(range(0, 512))
    k0, k1 = list(range(512, 576)), list(range(576, 640))
    v_c = list(range(640, 768))
    B_c = list(range(768, 1280))
    C_c = list(range(1280, 1792))
    U_c = list(range(1792, 2304))
    order = q_c + k0 + k0 + k1 + k1 + B_c + C_c + U_c + v_c
    w_in_r = kcp(w_in[:, order])
    b_in_r = b_in[order]
    prm = np.zeros((128, NP), np.float32)
    prm[:, P_BADA:P_BADA + 48] = colT(f("b_ada"), 48)
    prm[:, P_G1:P_G1 + 8] = colT(f("norm1_g"), 8)
    prm[:, P_G2:P_G2 + 8] = colT(f("norm2_g"), 8)
    prm[:, P_GF:P_GF + 8] = colT(f("final_g"), 8)
    prm[:, P_BOUT:P_BOUT + 8] = colT(f("b_out"), 8)
    prm[:, P_BIN:P_BIN + 18] = colT(b_in_r[:2304], 18)
    prm[:, P_AG:P_AG + 4] = colT(f("attn_out_g"), 4)
    prm[:, P_CG:P_CG + 4] = colT(f("conv_out_g"), 4)
    cw = f("conv_w")
    for tap in range(3):
        prm[:, P_CW + tap * 4:P_CW + tap * 4 + 4] = colT(cw[tap], 4)
    prm[:, P_SINK:P_SINK + 4] = np.repeat(f("attn_sinks"), 64).reshape(4, 128).T
    prm[:, P_BV:P_BV + 128] = np.broadcast_to(b_in_r[2304:2432][None, :], (128, 128))
    k1_, k2_ = f("peer_keys1"), f("peer_keys2")
    keys = np.stack([k1_, k2_], axis=1).reshape(16, 128, 128)
    keysT = np.ascontiguousarray(keys.transpose(2, 0, 1))
    U = f("peer_u")
    V = f("peer_v")
    UT = np.ascontiguousarray(U.reshape(128, 128, D).transpose(2, 1, 0).reshape(D, 16384))
    Vr = np.ascontiguousarray(V.reshape(128, 128, D).transpose(1, 0, 2).reshape(16384, D))
    return dict(w_ada=kcp(f("w_ada")), prm=prm, w_in=w_in_r, w_out=kcp(f("w_out")), w_q=kcp(f("w_query")), keysT=keysT, UT=UT, Vr=Vr)


def run(inputs, seq=2048, nseq=2, n_cores=N_CORES, stop_after=9):
    nc = build_nc(seq, nseq, stop_after=stop_after)
    shared = shared_layout(inputs)
    in_maps = []
    for core in range(n_cores):
        xT, cT = host_layout(inputs, core, seq, nseq)
        m = dict(shared)
        m["xT"] = xT
        m["cT"] = cT
        in_maps.append(m)
    res = run_bass_kernel_spmd(nc, in_maps, core_ids=list(range(n_cores)))
    outs = [np.asarray(r["yT"]).T.reshape(nseq, seq, D) for r in res.results]
    return np.concatenate(outs, axis=0).astype(np.float32)


def kernel(**inputs):
    return run(inputs)
```

```python
import numpy as np
from contextlib import ExitStack
import concourse.bass as bass
import concourse.mybir as mybir
from concourse.bass_utils import run_bass_kernel_spmd

F32 = mybir.dt.float32
BF16 = mybir.dt.bfloat16
U32 = mybir.dt.uint32
AF = mybir.ActivationFunctionType
ALU = mybir.AluOpType
AX = mybir.AxisListType

ENGS = ("tensor", "vector", "scalar", "gpsimd", "sync")
N_CORES = 8
D = 1024
EPS = 1e-6
NEGBIG = -1e30


class Prog:
    def __init__(self, nc, same_engine_sync=True):
        self.nc = nc
        self.ops = []
        self.last_w = {}
        self.readers = {}
        self.same_engine_sync = same_engine_sync
        self.dma_groups = {}

    def op(self, eng, fn, reads=(), writes=(), dma=None):
        i = len(self.ops)
        deps = set()
        for r in reads:
            if r in self.last_w:
                deps.add(self.last_w[r])
        for w in writes:
            if w in self.last_w:
                deps.add(self.last_w[w])
            for rd in self.readers.get(w, ()):
                deps.add(rd)
        deps.discard(i)
        for r in reads:
            lst = self.readers.setdefault(r, [])
            if dma is None:
                lst[:] = [q for q in lst if not (self.ops[q]["dma"] is None and self.ops[q]["eng"] == eng)]
            lst.append(i)
        for w in writes:
            self.last_w[w] = i
            self.readers[w] = []
        dma_idx = None
        if dma is not None:
            self.dma_groups[dma] = self.dma_groups.get(dma, 0) + 1
            dma_idx = self.dma_groups[dma]
        self.ops.append(dict(eng=eng, fn=fn, deps=deps, dma=dma, dma_idx=dma_idx, w=list(writes), r=list(reads)))
        return i

    def emit(self, max_ops=None):
        nc = self.nc
        if max_ops is not None:
            self.ops = self.ops[:max_ops]
            self.dma_groups = {}
            for o in self.ops:
                if o["dma"] is not None:
                    self.dma_groups[o["dma"]] = max(self.dma_groups.get(o["dma"], 0), o["dma_idx"])
        ops = self.ops
        ses = self.same_engine_sync

        def skip(p, o):
            return (p["dma"] is None and o["dma"] is None and p["eng"] == o["eng"]
                    and (p["eng"] == "tensor" or not ses))

        needs_inc = [False] * len(ops)
        for o in ops:
            for i in o["deps"]:
                p = ops[i]
                if p["dma"] is None and not skip(p, o):
                    needs_inc[i] = True
        cnt = {e: 0 for e in ENGS}
        sig = [0] * len(ops)
        for i, o in enumerate(ops):
            if o["dma"] is None and needs_inc[i]:
                cnt[o["eng"]] += 1
                sig[i] = cnt[o["eng"]]
        import os as _os
        if _os.environ.get("PROG_VERBOSE"):
            print("Prog: ops", len(ops), "sem counts", cnt, "dma", self.dma_groups)
        with ExitStack() as es:
            esem = {e: es.enter_context(nc.semaphore(f"pe_{e}")) for e in ENGS}
            dsem = {g: es.enter_context(nc.semaphore(f"pd_{g}")) for g in self.dma_groups}
            block = es.enter_context(nc.Block())
            per_eng = {e: [i for i, o in enumerate(ops) if o["eng"] == e] for e in ENGS}

            def make(ename):
                def body(eng):
                    waited = {}
                    for i in per_eng[ename]:
                        o = ops[i]
                        need = {}
                        for d in o["deps"]:
                            p = ops[d]
                            if p["dma"] is not None:
                                key = ("d", p["dma"])
                                val = 16 * p["dma_idx"]
                            else:
                                if skip(p, o):
                                    continue
                                key = ("e", p["eng"])
                                val = sig[d]
                            if val > need.get(key, 0):
                                need[key] = val
                        for key, val in need.items():
                            if waited.get(key, 0) >= val:
                                continue
                            waited[key] = val
                            s = dsem[key[1]] if key[0] == "d" else esem[key[1]]
                            eng.wait_ge(s, val)
                        ins = o["fn"](eng)
                        if o["dma"] is not None:
                            ins.then_inc(dsem[o["dma"]], 16)
                        elif needs_inc[i]:
                            ins.then_inc(esem[ename], 1)
                    if ename == "sync":
                        for g, n in self.dma_groups.items():
                            eng.wait_ge(dsem[g], 16 * n)
                        for e2 in ENGS:
                            if e2 != "sync" and cnt[e2] > 0:
                                eng.wait_ge(esem[e2], cnt[e2])
                return body

            block.tensor(make("tensor"))
            block.vector(make("vector"))
            block.scalar(make("scalar"))
            block.gpsimd(make("gpsimd"))
            block.sync(make("sync"))


P_BADA, P_G1, P_G2, P_GF, P_BOUT, P_BIN, P_AG, P_CG, P_CW, P_SINK, P_BV, NP = 0, 48, 56, 64, 72, 80, 98, 102, 106, 118, 122, 250
D_ADA, D_A1, D_GB1, D_A2, D_ESINK, ND = 0, 96, 112, 128, 144, 148
NIN = 2432


def build_nc(seq, nseq=2, tga=256, tgb=384, stop_after=9):
    ntok = seq * nseq
    TA = min(tga, seq)
    nc = bass.Bass("TRN2", target_bir_lowering=False)
    dt_in = lambda name, shape: nc.dram_tensor(name, shape, F32, kind="ExternalInput").ap()
    xT = dt_in("xT", [D, ntok])
    cT = dt_in("cT", [128, 8, nseq])
    w_ada = dt_in("w_ada", [128, 8, 6144])
    prm = dt_in("prm", [128, NP])
    w_in = dt_in("w_in", [128, 8, NIN])
    w_out = dt_in("w_out", [128, 8, D])
    w_q = dt_in("w_q", [128, 8, 2048])
    keysT = dt_in("keysT", [128, 16, 128])
    UT = dt_in("UT", [D, 16384])
    Vr = dt_in("Vr", [16384, D])
    yT = nc.dram_tensor("yT", [D, ntok], F32, kind="ExternalOutput").ap()
    UTb = nc.dram_tensor("UTb", [D, 16384], BF16, kind="Internal").ap()
    Vb = nc.dram_tensor("Vb", [16384, D], BF16, kind="Internal").ap()
    x1s = nc.dram_tensor("x1s", [128, 8, ntok], F32, kind="Internal").ap()
    h2s = nc.dram_tensor("h2s", [128, 8, ntok], BF16, kind="Internal").ap()
    sels = nc.dram_tensor("sels", [128, 3, ntok], F32, kind="Internal").ap()

    with ExitStack() as g_es:
        gsb = lambda name, shape, dt: g_es.enter_context(nc.sbuf_tensor(name, shape, dt))
        prm_sb = gsb("prm_sb", [128, NP], F32)
        drv = gsb("drv", [128, ND], F32)
        ones_bf = gsb("ones_bf", [128, 128], BF16)
        blk_bf = gsb("blk_bf", [128, 128], BF16)
        mask2 = gsb("mask2", [128, 4, 128], BF16)
        ident_f = gsb("ident_f", [128, 128], F32)
        iota_f = gsb("iota_f", [128, 128], F32)
        iota_b = gsb("iota_b", [128, 128], BF16)
        esink_bc = gsb("esink_bc", [128, 4, 128], F32)
        attng_bc = gsb("attng_bc", [128, 4, 128], F32)
        eps_t = gsb("eps_t", [128, 1], F32)

        def pcol(off, n=1):
            return prm_sb[:, off:off + n]

        def dcol(off, n=1):
            return drv[:, off:off + n]

        with ExitStack() as es:
            sb = lambda name, shape, dt: es.enter_context(nc.sbuf_tensor(name, shape, dt))
            c_sb = sb("c_sb", [128, 8, nseq], F32)
            sc = sb("sc", [128, 8, nseq], F32)
            wbuf = [sb(f"wada{i}", [128, 8, 1024], F32) for i in range(2)]
            tmpf = sb("tmpf", [128, 128], F32)
            tmp16 = sb("tmp16", [128, 8, nseq], F32)
            pa = es.enter_context(nc.psum_tensor("pa", [128, 512], F32))
            P = Prog(nc)
            P.op("sync", lambda e: e.dma_start(out=prm_sb[:], in_=prm), writes=["prm"], dma="prm")
            P.op("sync", lambda e: e.dma_start(out=c_sb[:], in_=cT), writes=["c"], dma="c")
            P.op("vector", lambda e: e.memset(ones_bf[:], 1.0), writes=["ones"])
            P.op("vector", lambda e: e.memset(blk_bf[:], 0.0), writes=["blk"])
            P.op("vector", lambda e: e.memset(blk_bf[0:64, 0:64], 1.0 / 64), reads=["blk"], writes=["blk"])
            P.op("vector", lambda e: e.memset(blk_bf[64:128, 64:128], 1.0 / 64), reads=["blk"], writes=["blk"])
            P.op("vector", lambda e: e.memset(eps_t[:], EPS), writes=["eps"])
            P.op("gpsimd", lambda e: e.iota(iota_f[:], pattern=[[1, 128]], base=0, channel_multiplier=0, allow_small_or_imprecise_dtypes=True), writes=["iota"])
            P.op("vector", lambda e: e.tensor_copy(out=iota_b[:], in_=iota_f[:]), reads=["iota"], writes=["iota_b"])
            P.op("gpsimd", lambda e: e.iota(tmpf[:], pattern=[[1, 128]], base=0, channel_multiplier=-1, allow_small_or_imprecise_dtypes=True), writes=["tmpf"])
            P.op("vector", lambda e: e.tensor_single_scalar(out=ident_f[:], in_=tmpf[:], scalar=0.0, op=ALU.is_equal), reads=["tmpf"], writes=["ident"])
            for hh in range(2):
                P.op("vector", (lambda hh: lambda e: e.tensor_single_scalar(out=mask2[:, 2 * hh, :], in_=tmpf[:], scalar=0.0, op=ALU.is_lt))(hh), reads=["tmpf"], writes=["mask"])
                P.op("vector", (lambda hh: lambda e: e.tensor_single_scalar(out=mask2[:, 2 * hh + 1, :], in_=tmpf[:], scalar=0.0, op=ALU.is_ge))(hh), reads=["tmpf"], writes=["mask"])
            P.op("scalar", lambda e: e.activation(out=sc[:], in_=c_sb[:], func=AF.Silu), reads=["c"], writes=["sc"])
            for piece in range(6):
                wb = wbuf[piece % 2]
                P.op("sync", (lambda piece, wb: lambda e: e.dma_start(out=wb[:], in_=w_ada[:, :, piece * 1024:(piece + 1) * 1024]))(piece, wb),
                     writes=[f"wb{piece % 2}"], dma=f"wada{piece % 2}")
                for jj in range(8):
                    j = piece * 8 + jj
                    for kc in range(8):
                        P.op("tensor", (lambda wb, jj, j, kc: lambda e: e.matmul(pa[:, j * nseq:(j + 1) * nseq], lhsT=wb[:, kc, jj * 128:(jj + 1) * 128],
                                                                                 rhs=sc[:, kc, :], start=(kc == 0), stop=(kc == 7)))(wb, jj, j, kc),
                             reads=[f"wb{piece % 2}", "sc"], writes=["pa"])
            ada_v = drv[:, D_ADA:D_ADA + 48 * nseq].rearrange("p (j b) -> p j b", b=nseq)
            P.op("vector", lambda e: e.tensor_tensor(out=ada_v, in0=pa[:, 0:48 * nseq].rearrange("p (j b) -> p j b", b=nseq),
                                                     in1=prm_sb[:, P_BADA:P_BADA + 48].unsqueeze(2).to_broadcast([128, 48, nseq]), op=ALU.add),
                 reads=["pa", "prm"], writes=["drv"])

            def adav(j0):
                return drv[:, D_ADA + j0 * nseq:D_ADA + (j0 + 8) * nseq].rearrange("p (j b) -> p j b", b=nseq)

            def dv(off):
                return drv[:, off:off + 8 * nseq].rearrange("p (j b) -> p j b", b=nseq)

            def pb(off):
                return prm_sb[:, off:off + 8].unsqueeze(2).to_broadcast([128, 8, nseq])
            P.op("vector", lambda e: e.tensor_scalar(out=tmp16[:], in0=adav(8), scalar1=1.0, scalar2=None, op0=ALU.add), reads=["drv"], writes=["tmp16"])
            P.op("vector", lambda e: e.tensor_tensor(out=dv(D_A1), in0=tmp16[:], in1=pb(P_G1), op=ALU.mult), reads=["tmp16", "prm"], writes=["drvA1"])
            P.op("vector", lambda e: e.tensor_tensor(out=dv(D_GB1), in0=adav(16), in1=pb(P_BOUT), op=ALU.mult), reads=["drv", "prm"], writes=["drvGB1"])
            P.op("vector", lambda e: e.tensor_scalar(out=tmp16[:], in0=adav(32), scalar1=1.0, scalar2=None, op0=ALU.add), reads=["drv", "drvA1"], writes=["tmp16"])
            P.op("vector", lambda e: e.tensor_tensor(out=dv(D_A2), in0=tmp16[:], in1=pb(P_G2), op=ALU.mult), reads=["tmp16", "prm"], writes=["drvA2"])
            P.op("scalar", lambda e: e.activation(out=drv[:, D_ESINK:D_ESINK + 4], in_=prm_sb[:, P_SINK:P_SINK + 4], func=AF.Exp), reads=["prm"], writes=["esink"])
            P.op("vector", lambda e: e.tensor_copy(out=esink_bc[:], in_=drv[:, D_ESINK:D_ESINK + 4].unsqueeze(2).to_broadcast([128, 4, 128])), reads=["esink"], writes=["esink_bc"])
            P.op("vector", lambda e: e.tensor_copy(out=attng_bc[:], in_=prm_sb[:, P_AG:P_AG + 4].unsqueeze(2).to_broadcast([128, 4, 128])), reads=["prm"], writes=["attng_bc"])
            P.emit()
        nc.all_engine_barrier()
        if stop_after < 1:
            return nc

        with ExitStack() as es:
            sb = lambda name, shape, dt: es.enter_context(nc.sbuf_tensor(name, shape, dt))
            win_sb = sb("win_sb", [128, 8, NIN], BF16)
            wout_sb = sb("wout_sb", [128, 8, D], BF16)
            wq_sb = sb("wq_sb", [128, 8, 2048], BF16)
            keys_sb = sb("keys_sb", [128, 16, 128], BF16)
            xt = [sb(f"xt{i}", [128, 8, TA], F32) for i in range(2)]
            sq = sb("sq", [128, 8, TA], BF16)
            rstd = sb("rstd", [128, TA], F32)
            tmpn = sb("tmpn", [128, 8, TA], F32)
            hT = sb("hT", [128, 8, TA], BF16)
            qT = sb("qT", [128, 4, TA], BF16)
            kT = sb("kT", [128, 2, 128 + TA], BF16)
            NB = TA // 128
            vtok = sb("vtok", [128, 1 + NB, 128], BF16)
            Bt = sb("Bt", [128, 4, TA], F32)
            zb = sb("zb", [128, 4, 2 + TA], F32)
            acc = sb("acc", [128, 4, TA], F32)
            Ct = acc
            sqc = sq[:, 0:4, :]
            rsc = sb("rsc", [128, TA], F32)
            catT = sb("catT", [128, 8, TA], BF16)
            pT = [sb(f"pT{i}", [128, 4, 128], BF16) for i in range(4)]
            t1 = sb("t1", [128, 4, 128], F32)
            yat = sb("yat", [128, 4, 128], F32)
            sqa = sb("sqa", [128, 4, 128], BF16)
            t2 = sb("t2", [128, 4, 128], F32)
            otmp = sb("otmp", [128, TA], F32)
            h2T = hT
            qpT = sb("qpT", [128, 16, TA], BF16)
            S_sb = sb("S_sb", [128, NB, 16, 128], F32)
            Vt = sb("Vt", [128, 16, 16], F32)
            It = sb("It", [128, 16, 16], U32)
            Itf = sb("Itf", [128, 16, 16], F32)
            cand = sb("cand", [128, 8, 256], F32)
            SC = sb("SC", [128, 8, 16], F32)
            CI = sb("CI", [128, 8, 16], U32)
            au = sb("au", [128, 8, 16], U32)
            bu = sb("bu", [128, 8, 16], U32)
            af_ = sb("af_", [128, 128], F32)
            bf_ = sb("bf_", [128, 128], F32)
            oh = tmpn[:].rearrange("p c t -> p (c t)").rearrange("p (a b) -> p a b", b=16)
            sel = sb("sel", [128, 3, 128], F32)
            ee = sb("ee", [128, 8, 16], F32)
            zz = sb("zz", [128, 8], F32)
            selT = sb("selT", [128, 3, 128], F32)
            ps = [es.enter_context(nc.psum_tensor(f"ps{i}", [128, 512], F32)) for i in range(8)]
            P = Prog(nc)
            for kc in range(8):
                P.op("gpsimd", (lambda kc: lambda e: e.dma_start(out=win_sb[:, kc, :], in_=w_in[:, kc, :]))(kc), writes=[f"win{kc}"], dma=f"w_in{kc}")
            P.op("gpsimd", lambda e: e.dma_start(out=keys_sb[:], in_=keysT), writes=["keys"], dma="w_k")
            for kc in range(0, 8, 2):
                P.op("gpsimd", (lambda kc: lambda e: e.dma_start(out=wout_sb[:, kc:kc + 2, :], in_=w_out[:, kc:kc + 2, :]))(kc), writes=[f"wout{kc}", f"wout{kc + 1}"], dma=f"w_out{kc}")
            for kc in range(8):
                P.op("gpsimd", (lambda kc: lambda e: e.dma_start(out=wq_sb[:, kc, :], in_=w_q[:, kc, :]))(kc), writes=[f"wq{kc}"], dma=f"w_q{kc}")
            NPC = 16
            for i in range(NPC):
                r0, r1 = i * (D // NPC), (i + 1) * (D // NPC)
                P.op("gpsimd", (lambda r0, r1: lambda e: e.dma_start(out=UTb[r0:r1, :], in_=UT[r0:r1, :]))(r0, r1), dma="cvt")
            for i in range(NPC):
                r0, r1 = i * (16384 // NPC), (i + 1) * (16384 // NPC)
                P.op("gpsimd", (lambda r0, r1: lambda e: e.dma_start(out=Vb[r0:r1, :], in_=Vr[r0:r1, :]))(r0, r1), dma="cvt")
            WIN = [f"win{k}" for k in range(8)]
            WOUT = [f"wout{k}" for k in range(8)]
            WQ = [f"wq{k}" for k in range(8)]
            ntile = ntok // TA
            tiles_per_seq = seq // TA

            def rms_stats(P, src, srckey, pbank, tag):
                P.op("scalar", lambda e: e.activation(out=sq[:], in_=src[:], func=AF.Square), reads=[srckey], writes=["sq"])
                for kc in range(8):
                    P.op("tensor", (lambda kc: lambda e: e.matmul(pbank[:, 0:TA], lhsT=ones_bf[:], rhs=sq[:, kc, :], start=(kc == 0), stop=(kc == 7)))(kc),
                         reads=["sq"], writes=[tag])
                P.op("scalar", lambda e: e.activation(out=rstd[:], in_=pbank[:, 0:TA], func=AF.Sqrt, bias=eps_t[:], scale=1.0 / D), reads=[tag], writes=["rstd"])
                P.op("vector", lambda e: e.reciprocal(out=rstd[:], in_=rstd[:]), reads=["rstd"], writes=["rstd"])

            pending = []

            def drain(n):
                for _ in range(min(n, len(pending))):
                    pending.pop(0)()

            def topk_pieces(blk, tq):
                pcs = []
                S = lambda j: S_sb[:, blk, j, :]
                sk = lambda j: f"S{blk}_{j}"
                J = range(16)
                H = range(8)
                pcs.append(lambda: [P.op("vector", (lambda j: lambda e: e.max(out=Vt[:, j, 0:8], in_=S(j)))(j), reads=[sk(j)], writes=[f"Vta{j}"]) for j in J])
                pcs.append(lambda: [P.op("vector", (lambda j: lambda e: e.max_index(out=It[:, j, 0:8], in_max=Vt[:, j, 0:8], in_values=S(j)))(j), reads=[sk(j), f"Vta{j}"], writes=[f"Ita{j}"]) for j in J])
                pcs.append(lambda: [P.op("vector", (lambda j: lambda e: e.match_replace(out=S(j), in_to_replace=Vt[:, j, 0:8], in_values=S(j), imm_value=NEGBIG))(j), reads=[sk(j), f"Vta{j}"], writes=[sk(j)]) for j in J])
                pcs.append(lambda: [P.op("vector", (lambda j: lambda e: e.max(out=Vt[:, j, 8:16], in_=S(j)))(j), reads=[sk(j)], writes=[f"Vtb{j}"]) for j in J])
                pcs.append(lambda: [P.op("vector", (lambda j: lambda e: e.max_index(out=It[:, j, 8:16], in_max=Vt[:, j, 8:16], in_values=S(j)))(j), reads=[sk(j), f"Vtb{j}"], writes=[f"Itb{j}"]) for j in J])
                VT = [f"Vta{j}" for j in J] + [f"Vtb{j}" for j in J]
                IT = [f"Ita{j}" for j in J] + [f"Itb{j}" for j in J]
                CAND = [f"cand{h}" for h in H]
                Vv = Vt[:].rearrange("p (h s) a -> p h s a", s=2)
                pcs.append(lambda: P.op("vector", lambda e: e.tensor_tensor(out=cand[:].rearrange("p h (a b) -> p h a b", a=16), in0=Vv[:, :, 0, :].unsqueeze(3).to_broadcast([128, 8, 16, 16]),
                                                                          in1=Vv[:, :, 1, :].unsqueeze(2).to_broadcast([128, 8, 16, 16]), op=ALU.add), reads=VT, writes=CAND))
                pcs.append(lambda: [P.op("vector", (lambda h: lambda e: e.max(out=SC[:, h, 0:8], in_=cand[:, h, :]))(h), reads=[f"cand{h}"], writes=[f"SCa{h}"]) for h in H])
                pcs.append(lambda: [P.op("vector", (lambda h: lambda e: e.max_index(out=CI[:, h, 0:8], in_max=SC[:, h, 0:8], in_values=cand[:, h, :]))(h), reads=[f"cand{h}", f"SCa{h}"], writes=[f"CIa{h}"]) for h in H])
                pcs.append(lambda: [P.op("vector", (lambda h: lambda e: e.match_replace(out=cand[:, h, :], in_to_replace=SC[:, h, 0:8], in_values=cand[:, h, :], imm_value=NEGBIG))(h), reads=[f"cand{h}", f"SCa{h}"], writes=[f"cand{h}"]) for h in H])
                pcs.append(lambda: [P.op("vector", (lambda h: lambda e: e.max(out=SC[:, h, 8:16], in_=cand[:, h, :]))(h), reads=[f"cand{h}"], writes=[f"SCb{h}"]) for h in H])
                pcs.append(lambda: [P.op("vector", (lambda h: lambda e: e.max_index(out=CI[:, h, 8:16], in_max=SC[:, h, 8:16], in_values=cand[:, h, :]))(h), reads=[f"cand{h}", f"SCb{h}"], writes=[f"CIb{h}"]) for h in H])
                SCK = [f"SCa{h}" for h in H] + [f"SCb{h}" for h in H]
                CIK = [f"CIa{h}" for h in H] + [f"CIb{h}" for h in H]

                def gates():
                    P.op("vector", lambda e: e.tensor_tensor(out=ee[:], in0=SC[:], in1=SC[:, :, 0:1].to_broadcast([128, 8, 16]), op=ALU.subtract), reads=SCK, writes=["ee"])
                    P.op("scalar", lambda e: e.activation(out=ee[:], in_=ee[:], func=AF.Exp), reads=["ee"], writes=["ee"])
                    P.op("vector", lambda e: e.tensor_reduce(out=zz[:], in_=ee[:], axis=AX.X, op=ALU.add), reads=["ee"], writes=["zz"])
                    P.op("vector", lambda e: e.reciprocal(out=zz[:], in_=zz[:]), reads=["zz"], writes=["zz"])
                    P.op("vector", lambda e: e.tensor_tensor(out=sel[:, 2, :].rearrange("p (h k) -> p h k", h=8), in0=ee[:], in1=zz[:].unsqueeze(2).to_broadcast([128, 8, 16]), op=ALU.mult),
                         reads=["ee", "zz"], writes=["sel2"])
                    P.op("vector", lambda e: e.tensor_single_scalar(out=au[:], in_=CI[:], scalar=4, op=ALU.logical_shift_right), reads=CIK, writes=["au"])
                    P.op("vector", lambda e: e.tensor_single_scalar(out=bu[:], in_=CI[:], scalar=15, op=ALU.bitwise_and), reads=CIK, writes=["bu"])
                    P.op("vector", lambda e: e.tensor_copy(out=af_[:], in_=au[:].rearrange("p h k -> p (h k)")), reads=["au"], writes=["af"])
                    P.op("vector", lambda e: e.tensor_copy(out=bf_[:], in_=bu[:].rearrange("p h k -> p (h k)")), reads=["bu"], writes=["bf"])
                    P.op("vector", lambda e: e.tensor_copy(out=Itf[:], in_=It[:]), reads=IT, writes=["Itf"])
                pcs.append(gates)

                def decode():
                    Iv = Itf[:].rearrange("p (h s) a -> p h s a", s=2)
                    for s_, src in ((0, af_), (1, bf_)):
                        P.op("vector", (lambda src: lambda e: e.tensor_tensor(out=oh, in0=iota_f[:, 0:16].unsqueeze(1).to_broadcast([128, 128, 16]),
                                                                              in1=src[:].unsqueeze(2).to_broadcast([128, 128, 16]), op=ALU.is_equal))(src),
                             reads=["af", "bf"], writes=["tmpn"])
                        P.op("gpsimd", (lambda s_: lambda e: e.tensor_tensor(out=oh.rearrange("p (h k) a -> p h k a", h=8), in0=oh.rearrange("p (h k) a -> p h k a", h=8),
                                                                             in1=Iv[:, :, s_, :].unsqueeze(2).to_broadcast([128, 8, 16, 16]), op=ALU.mult))(s_),
                             reads=["tmpn", "Itf"], writes=["tmpn"])
                        P.op("vector", (lambda s_: lambda e: e.tensor_reduce(out=sel[:, s_, :], in_=oh, axis=AX.X, op=ALU.add))(s_), reads=["tmpn"], writes=[f"sel{s_}"])
                    for s_ in range(3):
                        P.op("tensor", (lambda s_: lambda e: e.transpose(out=ps[6][:, s_ * 128:(s_ + 1) * 128], in_=sel[:, s_, :], identity=ident_f[:]))(s_),
                             reads=[f"sel{s_}"], writes=["ps6"])
                    P.op("scalar", lambda e: e.copy(out=selT[:].rearrange("p a b -> p (a b)"), in_=ps[6][:, 0:384]), reads=["ps6"], writes=["selT"])
                    P.op("sync", (lambda tq: lambda e: e.dma_start(out=sels[:, :, tq:tq + 128], in_=selT[:]))(tq), reads=["selT"], dma="selst")
                pcs.append(decode)
                return pcs

            for m in range(ntile):
                b = m // tiles_per_seq
                first = (m % tiles_per_seq == 0)
                tok0 = m * TA
                x = xt[m % 2]
                xk = f"xt{m % 2}"
                P.op("sync", (lambda x, tok0: lambda e: e.dma_start(out=x[:], in_=xT[:, tok0:tok0 + TA].rearrange("(c p) t -> p c t", p=128)))(x, tok0),
                     writes=[xk], dma=xk)
                rms_stats(P, x, xk, ps[7], "ps7")
                P.op("gpsimd", (lambda x: lambda e: e.tensor_tensor(out=tmpn[:], in0=x[:], in1=rstd[:].unsqueeze(1).to_broadcast([128, 8, TA]), op=ALU.mult))(x),
                     reads=[xk, "rstd"], writes=["tmpn"])
                for c in range(8):
                    P.op("scalar", (lambda c, b: lambda e: e.activation(out=hT[:, c, :], in_=tmpn[:, c, :], func=AF.Identity,
                                                                        scale=dcol(D_A1 + c * nseq + b), bias=dcol(D_ADA + (0 + c) * nseq + b)))(c, b),
                         reads=["tmpn"], writes=[f"hT{c}"])
                HT = [f"hT{c}" for c in range(8)]
                if first:
                    P.op("vector", lambda e: e.memset(zb[:, :, 0:2], 0.0), reads=["zb"], writes=["zb"])
                for j in range(18):
                    if j % 3 == 0 and j < 15:
                        drain(1)
                    pb_ = ps[j % 6]
                    pk = f"ps{j % 6}"
                    for kc in range(8):
                        P.op("tensor", (lambda pb_, j, kc: lambda e: e.matmul(pb_[:, 0:TA], lhsT=win_sb[:, kc, j * 128:(j + 1) * 128], rhs=hT[:, kc, :],
                                                                              start=(kc == 0), stop=(kc == 7)))(pb_, j, kc),
                             reads=[f"win{kc}", f"hT{kc}"], writes=[pk])
                    bias = pcol(P_BIN + j)
                    if j < 4:
                        P.op("scalar", (lambda pb_, j, bias: lambda e: e.activation(out=qT[:, j, :], in_=pb_[:, 0:TA], func=AF.Identity, bias=bias))(pb_, j, bias),
                             reads=[pk], writes=["qT"])
                    elif j < 6:
                        P.op("scalar", (lambda pb_, j, bias: lambda e: e.activation(out=kT[:, j - 4, 128:128 + TA], in_=pb_[:, 0:TA], func=AF.Identity, bias=bias))(pb_, j, bias),
                             reads=[pk], writes=["kTcur"])
                    elif j < 10:
                        P.op("scalar", (lambda pb_, j, bias: lambda e: e.activation(out=Bt[:, j - 6, :], in_=pb_[:, 0:TA], func=AF.Identity, bias=bias))(pb_, j, bias),
                             reads=[pk], writes=["Bt"])
                    elif j < 14:
                        P.op("scalar", (lambda pb_, j, bias: lambda e: e.activation(out=Ct[:, j - 10, :], in_=pb_[:, 0:TA], func=AF.Identity, bias=bias))(pb_, j, bias),
                             reads=[pk], writes=[f"acc{j - 10}"])
                    else:
                        P.op("vector", (lambda pb_, j, bias: lambda e: e.scalar_tensor_tensor(out=zb[:, j - 14, 2:2 + TA], in0=pb_[:, 0:TA], scalar=bias,
                                                                                               in1=Ct[:, j - 14, :], op0=ALU.add, op1=ALU.mult))(pb_, j, bias),
                             reads=[pk, f"acc{j - 14}"], writes=["zb"])
                for blk in range(NB):
                    pb_ = ps[blk % 2]
                    pk = f"ps{blk % 2}"
                    for kc in range(8):
                        P.op("tensor", (lambda pb_, blk, kc: lambda e: e.matmul(pb_[:, 0:128], lhsT=hT[:, kc, blk * 128:(blk + 1) * 128], rhs=win_sb[:, kc, 2304:2432],
                                                                                start=(kc == 0), stop=(kc == 7)))(pb_, blk, kc),
                             reads=[f"win{kc}", f"hT{kc}"], writes=[pk])
                    P.op("vector", (lambda pb_, blk: lambda e: e.tensor_tensor(out=vtok[:, 1 + blk, :], in0=pb_[:, 0:128], in1=prm_sb[:, P_BV:P_BV + 128], op=ALU.add))(pb_, blk),
                         reads=[pk], writes=["vcur"])
                for cc in range(4):
                    P.op("scalar", (lambda cc: lambda e: e.activation(out=acc[:, cc, :], in_=zb[:, cc, 2:2 + TA], func=AF.Copy, scale=pcol(P_CW + 2 * 4 + cc)))(cc),
                         reads=["zb"], writes=[f"acc{cc}"])
                    P.op("vector", (lambda cc: lambda e: e.scalar_tensor_tensor(out=acc[:, cc, :], in0=zb[:, cc, 1:1 + TA], scalar=pcol(P_CW + 1 * 4 + cc), in1=acc[:, cc, :],
                                                                                op0=ALU.mult, op1=ALU.add))(cc), reads=["zb", f"acc{cc}"], writes=[f"acc{cc}"])
                    P.op("vector", (lambda cc: lambda e: e.scalar_tensor_tensor(out=acc[:, cc, :], in0=zb[:, cc, 0:TA], scalar=pcol(P_CW + 0 * 4 + cc), in1=acc[:, cc, :],
                                                                                op0=ALU.mult, op1=ALU.add))(cc), reads=["zb", f"acc{cc}"], writes=[f"acc{cc}"])
                    P.op("gpsimd", (lambda cc: lambda e: e.tensor_tensor(out=acc[:, cc, :], in0=acc[:, cc, :], in1=Bt[:, cc, :], op=ALU.mult))(cc),
                         reads=["Bt", f"acc{cc}"], writes=[f"acc{cc}"])
                ACC = [f"acc{cc}" for cc in range(4)]
                P.op("gpsimd", lambda e: e.tensor_copy(out=zb[:, :, 0:2], in_=zb[:, :, TA:TA + 2]), reads=["zb"] + ACC, writes=["zb"])
                P.op("scalar", lambda e: e.activation(out=sqc, in_=acc[:], func=AF.Square), reads=ACC, writes=["sq"])
                for cc in range(4):
                    pb_ = ps[2 + cc % 2]
                    pk = f"ps{2 + cc % 2}"
                    P.op("tensor", (lambda pb_, cc: lambda e: e.matmul(pb_[:, 0:TA], lhsT=blk_bf[:], rhs=sqc[:, cc, :], start=True, stop=True))(pb_, cc),
                         reads=["sq"], writes=[pk])
                    P.op("scalar", (lambda pb_: lambda e: e.activation(out=rsc[:], in_=pb_[:, 0:TA], func=AF.Sqrt, bias=eps_t[:], scale=1.0))(pb_), reads=[pk], writes=["rsc"])
                    P.op("vector", lambda e: e.reciprocal(out=rsc[:], in_=rsc[:]), reads=["rsc"], writes=["rsc"])
                    P.op("vector", (lambda cc: lambda e: e.scalar_tensor_tensor(out=catT[:, 4 + cc, :], in0=acc[:, cc, :], scalar=pcol(P_CG + cc), in1=rsc[:],
                                                                                op0=ALU.mult, op1=ALU.mult))(cc), reads=[f"acc{cc}", "rsc"], writes=[f"cat{4 + cc}"])
                for blk in range(NB):
                    hasprev = not (first and blk == 0)
                    q0 = blk * 128
                    kprev = slice(q0, q0 + 128)
                    kcur = slice(128 + q0, 256 + q0)
                    vprev, vcur = blk, blk + 1
                    po = ps[4]
                    pd = ps[5]
                    for pp in range(2):
                        g = pp
                        for ci in range(2):
                            c = 2 * pp + ci
                            for hh in range(2):
                                lo, hi = hh * 64, hh * 64 + 64
                                bank = ps[2 * pp + hh]
                                pk = f"ps{2 * pp + hh}"
                                if hasprev:
                                    P.op("tensor", (lambda bank, ci, lo, hi, g, c, kprev, q0: lambda e: e.matmul(bank[:, ci * 256:ci * 256 + 128], lhsT=kT[lo:hi, g, kprev],
                                                                                                               rhs=qT[lo:hi, c, q0:q0 + 128], start=True, stop=True))(bank, ci, lo, hi, g, c, kprev, q0),
                                         reads=["qT", "kTcur", "kTprev"], writes=[pk])
                                P.op("tensor", (lambda bank, ci, lo, hi, g, c, kcur, q0: lambda e: e.matmul(bank[:, ci * 256 + 128:ci * 256 + 256], lhsT=kT[lo:hi, g, kcur],
                                                                                                          rhs=qT[lo:hi, c, q0:q0 + 128], start=True, stop=True))(bank, ci, lo, hi, g, c, kcur, q0),
                                     reads=["qT", "kTcur", "kTprev"], writes=[pk])
                        drain(1)
                        for hh in range(2):
                            bank = ps[2 * pp + hh]
                            pk = f"ps{2 * pp + hh}"
                            pt = pT[2 * pp + hh]
                            ptk = f"pT{2 * pp + hh}"
                            if hasprev:
                                P.op("scalar", (lambda bank, pt: lambda e: e.activation(out=pt[:].rearrange("p a b -> p (a b)"), in_=bank[:, :], func=AF.Exp, scale=0.125))(bank, pt),
                                     reads=[pk], writes=[ptk])
                                P.op("gpsimd", (lambda pt: lambda e: e.tensor_tensor(out=pt[:], in0=pt[:], in1=mask2[:], op=ALU.mult))(pt), reads=[ptk], writes=[ptk])
                            else:
                                psv = bank[:, :].rearrange("p (h a i) -> p h a i", h=2, a=2)[:, :, 1, :]
                                ptv = pt[:].rearrange("p (h a) i -> p h a i", h=2)[:, :, 1, :]
                                mkv = mask2[:].rearrange("p (h a) i -> p h a i", h=2)[:, :, 1, :]
                                P.op("scalar", (lambda psv, ptv: lambda e: e.activation(out=ptv, in_=psv, func=AF.Exp, scale=0.125))(psv, ptv), reads=[pk], writes=[ptk])
                                P.op("gpsimd", (lambda ptv, mkv: lambda e: e.tensor_tensor(out=ptv, in0=ptv, in1=mkv, op=ALU.mult))(ptv, mkv), reads=[ptk], writes=[ptk])
                        for ci in range(2):
                            c = 2 * pp + ci
                            for hh in range(2):
                                lo, hi = hh * 64, hh * 64 + 64
                                pt = pT[2 * pp + hh]
                                ptk = f"pT{2 * pp + hh}"
                                oc = slice(c * 128, (c + 1) * 128)
                                if hasprev:
                                    P.op("tensor", (lambda pt, ci, lo, hi, g, oc, vprev: lambda e: e.matmul(po[lo:hi, oc], lhsT=vtok[:, vprev, g * 64:(g + 1) * 64], rhs=pt[:, 2 * ci, :],
                                                                                                          start=True, stop=False))(pt, ci, lo, hi, g, oc, vprev),
                                         reads=[ptk, "vcur", "vprev"], writes=["ps4"])
                                P.op("tensor", (lambda pt, ci, lo, hi, g, oc, vcur, hasprev: lambda e: e.matmul(po[lo:hi, oc], lhsT=vtok[:, vcur, g * 64:(g + 1) * 64], rhs=pt[:, 2 * ci + 1, :],
                                                                                                              start=(not hasprev), stop=True))(pt, ci, lo, hi, g, oc, vcur, hasprev),
                                     reads=[ptk, "vcur", "vprev"], writes=["ps4"])
                                if hasprev:
                                    P.op("tensor", (lambda pt, ci, lo, hi, oc: lambda e: e.matmul(pd[lo:hi, oc], lhsT=ones_bf[:, 0:64], rhs=pt[:, 2 * ci, :], start=True, stop=False))(pt, ci, lo, hi, oc),
                                         reads=[ptk], writes=["ps5"])
                                P.op("tensor", (lambda pt, ci, lo, hi, oc, hasprev: lambda e: e.matmul(pd[lo:hi, oc], lhsT=ones_bf[:, 0:64], rhs=pt[:, 2 * ci + 1, :], start=(not hasprev), stop=True))(pt, ci, lo, hi, oc, hasprev),
                                     reads=[ptk], writes=["ps5"])
                    pov = po[:, :].rearrange("p (c i) -> p c i", c=4)
                    pdv = pd[:, :].rearrange("p (c i) -> p c i", c=4)
                    P.op("vector", lambda e: e.tensor_tensor(out=t1[:], in0=pdv, in1=esink_bc[:], op=ALU.add), reads=["ps5"], writes=["t1"])
                    P.op("vector", lambda e: e.reciprocal(out=t1[:], in_=t1[:]), reads=["t1"], writes=["t1"])
                    P.op("vector", lambda e: e.tensor_tensor(out=yat[:], in0=pov, in1=t1[:], op=ALU.mult), reads=["ps4", "t1"], writes=["yat"])
                    P.op("scalar", lambda e: e.activation(out=sqa[:], in_=yat[:], func=AF.Square), reads=["yat"], writes=["sqa"])
                    P.op("tensor", lambda e: e.matmul(ps[6][:, :], lhsT=blk_bf[:], rhs=sqa[:].rearrange("p c i -> p (c i)"), start=True, stop=True), reads=["sqa"], writes=["ps6"])
                    P.op("scalar", lambda e: e.activation(out=t2[:].rearrange("p c i -> p (c i)"), in_=ps[6][:, :], func=AF.Sqrt, bias=eps_t[:], scale=1.0), reads=["ps6"], writes=["t2"])
                    P.op("vector", lambda e: e.reciprocal(out=t2[:], in_=t2[:]), reads=["t2"], writes=["t2"])
                    P.op("vector", lambda e: e.tensor_tensor(out=yat[:], in0=yat[:], in1=t2[:], op=ALU.mult), reads=["yat", "t2"], writes=["yat"])
                    P.op("vector", (lambda q0: lambda e: e.tensor_tensor(out=catT[:, 0:4, q0:q0 + 128], in0=yat[:], in1=attng_bc[:], op=ALU.mult))(q0),
                         reads=["yat"], writes=[f"cat{c}" for c in range(4)])
                P.op("gpsimd", lambda e: e.tensor_copy(out=kT[:, :, 0:128], in_=kT[:, :, TA:TA + 128]), reads=["kTcur", "kTprev"], writes=["kTprev", "kTcur"])
                P.op("gpsimd", lambda e: e.tensor_copy(out=vtok[:, 0, :], in_=vtok[:, NB, :]), reads=["vcur", "vprev"], writes=["vprev", "vcur"])
                CAT = [f"cat{c}" for c in range(8)]
                for j in range(8):
                    if j % 2 == 0:
                        drain(1)
                    pb_ = ps[j % 6]
                    pk = f"ps{j % 6}"
                    for kc in range(8):
                        P.op("tensor", (lambda pb_, j, kc: lambda e: e.matmul(pb_[:, 0:TA], lhsT=wout_sb[:, kc, j * 128:(j + 1) * 128], rhs=catT[:, kc, :], start=(kc == 0), stop=(kc == 7)))(pb_, j, kc),
                             reads=[f"wout{kc}", f"cat{kc}"], writes=[pk])
                    P.op("scalar", (lambda pb_, j, b: lambda e: e.activation(out=otmp[:], in_=pb_[:, 0:TA], func=AF.Identity, scale=dcol(D_ADA + (16 + j) * nseq + b),
                                                                             bias=dcol(D_GB1 + j * nseq + b)))(pb_, j, b), reads=[pk], writes=["otmp"])
                    P.op("gpsimd", (lambda x, j: lambda e: e.tensor_tensor(out=x[:, j, :], in0=x[:, j, :], in1=otmp[:], op=ALU.add))(x, j), reads=["otmp", xk], writes=[xk])
                P.op("sync", (lambda x, tok0: lambda e: e.dma_start(out=x1s[:, :, tok0:tok0 + TA], in_=x[:]))(x, tok0), reads=[xk], dma=f"x1st{m % 2}")
                rms_stats(P, x, xk, ps[7], "ps7")
                P.op("gpsimd", (lambda x: lambda e: e.tensor_tensor(out=tmpn[:], in0=x[:], in1=rstd[:].unsqueeze(1).to_broadcast([128, 8, TA]), op=ALU.mult))(x),
                     reads=[xk, "rstd"], writes=["tmpn"])
                for c in range(8):
                    P.op("scalar", (lambda c, b: lambda e: e.activation(out=h2T[:, c, :], in_=tmpn[:, c, :], func=AF.Identity,
                                                                        scale=dcol(D_A2 + c * nseq + b), bias=dcol(D_ADA + (24 + c) * nseq + b)))(c, b),
                         reads=["tmpn"], writes=[f"hT{c}"])
                H2 = [f"hT{c}" for c in range(8)]
                P.op("sync", (lambda tok0: lambda e: e.dma_start(out=h2s[:, :, tok0:tok0 + TA], in_=h2T[:]))(tok0), reads=H2, dma="h2st")
                for j in range(16):
                    if j % 2 == 0:
                        drain(1)
                    pb_ = ps[j % 6]
                    pk = f"ps{j % 6}"
                    for kc in range(8):
                        P.op("tensor", (lambda pb_, j, kc: lambda e: e.matmul(pb_[:, 0:TA], lhsT=wq_sb[:, kc, j * 128:(j + 1) * 128], rhs=h2T[:, kc, :], start=(kc == 0), stop=(kc == 7)))(pb_, j, kc),
                             reads=[f"wq{kc}", f"hT{kc}"], writes=[pk])
                    P.op("scalar", (lambda pb_, j: lambda e: e.copy(out=qpT[:, j, :], in_=pb_[:, 0:TA]))(pb_, j), reads=[pk], writes=[f"qp{j}"])
                drain(len(pending))
                for blk in range(NB):
                    q0 = blk * 128
                    for j4 in range(4):
                        pb_ = ps[2 + j4]
                        pk = f"ps{2 + j4}"
                        for jj in range(4):
                            j = j4 * 4 + jj
                            P.op("tensor", (lambda pb_, j, jj, q0: lambda e: e.matmul(pb_[:, jj * 128:(jj + 1) * 128], lhsT=qpT[:, j, q0:q0 + 128], rhs=keys_sb[:, j, :], start=True, stop=True))(pb_, j, jj, q0),
                                 reads=[f"qp{j}", "keys"], writes=[pk])
                        P.op("scalar", (lambda pb_, j4, blk: lambda e: e.copy(out=S_sb[:, blk, j4 * 4:(j4 + 1) * 4, :].rearrange("p a b -> p (a b)"), in_=pb_[:, :]))(pb_, j4, blk),
                             reads=[pk], writes=[f"S{blk}_{j4 * 4 + jj}" for jj in range(4)])
                for blk in range(NB):
                    pending.extend(topk_pieces(blk, tok0 + blk * 128))
            while pending:
                pending.pop(0)()
            import os as _os
            _mx = _os.environ.get("PROG_MAXOPS")
            print("phase A ops", len(P.ops))
            if _mx:
                for _i, _o in enumerate(P.ops[:int(_mx)][-3:]):
                    print("last ops", _o["eng"], _o["dma"])
            P.emit(int(_mx) if _mx else None)
        nc.all_engine_barrier()
        if stop_after < 2:
            return nc

        with ExitStack() as es:
            sb = lambda name, shape, dt: es.enter_context(nc.sbuf_tensor(name, shape, dt))
            TG = min(tgb, ntok)
            Gbuf = sb("Gbuf", [128, TG, 128], BF16)
            CPD = 4
            utb = [sb(f"utb{i}", [128, 8, CPD * 128], BF16) for i in range(2)]
            vbb = [sb(f"vbb{i}", [128, CPD, D], BF16) for i in range(2)]
            h2g = sb("h2g", [128, 8, TG], BF16)
            x1g = sb("x1g", [128, 8, TG], F32)
            selg = [sb(f"selg{i}", [128, 3, TG], F32) for i in range(2)]
            NAB = 16
            A1 = [sb(f"A1_{i}", [128, 128], BF16) for i in range(NAB)]
            A2 = [sb(f"A2_{i}", [128, 64], BF16) for i in range(NAB)]
            ag = [sb(f"ag{i}", [128, TG], BF16) for i in range(2)]
            wT = [sb(f"wT{i}", [128, TG], BF16) for i in range(2)]
            sq2 = sb("sq2", [128, 8, TG], BF16)
            rstd2 = sb("rstd2", [128, TG], F32)
            peer_sb = sb("peer_sb", [128, TG // 128, D], F32)
            BK = [es.enter_context(nc.psum_tensor(f"bk{i}", [128, 512], F32)) for i in range(8)]
            pm = [BK[6]]
            P = Prog(nc)
            groups = []
            t0 = 0
            while t0 < ntok:
                n = min(TG, ntok - t0)
                groups.append((t0, n))
                t0 += n
            ndma = 0
            bkk = lambda j: [f"bk{j}"]
            cnt_tok = [0]
            cnt_reg = [0]

            def load_sel(gi):
                g0, gn = groups[gi]
                sg = selg[gi % 2]
                P.op("sync", (lambda sg, g0, gn: lambda e: e.dma_start(out=sg[:, :, 0:gn], in_=sels[:, :, g0:g0 + gn]))(sg, g0, gn), writes=[f"selg{gi % 2}"], dma=f"selg{gi % 2}")

            def build_pieces(gi, h):
                g0, gn = groups[gi]
                sg = selg[gi % 2]
                skey = f"selg{gi % 2}"
                out = []
                for tp in range(0, gn, 2):
                    toks = []
                    for t in (tp, tp + 1):
                        i = cnt_tok[0] % NAB
                        cnt_tok[0] += 1
                        toks.append((t, i))
                    def part_a(toks=toks):
                        for (t, i) in toks:
                            P.op("vector", (lambda t, i: lambda e: e.tensor_scalar(out=A2[i][:], in0=iota_b[:, 64 * h:64 * h + 64], scalar1=sg[:, 1, t:t + 1], scalar2=sg[:, 2, t:t + 1],
                                                                                   op0=ALU.is_equal, op1=ALU.mult))(t, i),
                                 reads=[skey], writes=[f"A2_{i}"])
                            P.op("vector", (lambda t, i: lambda e: e.tensor_scalar(out=A1[i][:], in0=iota_b[:], scalar1=sg[:, 0, t:t + 1], scalar2=None, op0=ALU.is_equal))(t, i),
                                 reads=[skey], writes=[f"A1_{i}"])
                    out.append((part_a, toks, h))
                return out

            pend_a = []
            pend_b = []

            def emit_b():
                k = 0
                runs = []
                for (toks, h) in pend_b:
                    for (t, i) in toks:
                        P.op("tensor", (lambda k, i: lambda e: e.matmul(BK[7][:, k * 64:(k + 1) * 64], lhsT=A1[i][:], rhs=A2[i][:], start=True, stop=True))(k, i),
                             reads=[f"A1_{i}", f"A2_{i}"], writes=["bk7"])
                        if runs and runs[-1][0] == h and runs[-1][1] + runs[-1][2] == t:
                            runs[-1][2] += 1
                        else:
                            runs.append([h, t, 1, k])
                        k += 1
                for (h, t0_, n_, k0) in runs:
                    P.op("scalar", (lambda h, t0_, n_, k0: lambda e: e.copy(out=Gbuf[:, t0_:t0_ + n_, 64 * h:64 * h + 64], in_=BK[7][:, k0 * 64:(k0 + n_) * 64].rearrange("p (a b) -> p a b", a=n_)))(h, t0_, n_, k0),
                         reads=["bk7"], writes=[f"Gbuf{h}"])
                del pend_b[:]

            def build_step(n):
                if pend_b:
                    emit_b()
                for _ in range(min(n, 4, len(pend_a))):
                    pa_, toks, h = pend_a.pop(0)
                    pa_()
                    pend_b.append((toks, h))

            def build_flush():
                while pend_a or pend_b:
                    build_step(4)

            load_sel(0)
            pend_a.extend(build_pieces(0, 0))
            pend_a.extend(build_pieces(0, 1))
            build_flush()
            for gi, (g0, gn) in enumerate(groups):
                P.op("sync", (lambda g0, gn: lambda e: e.dma_start(out=h2g[:, :, 0:gn], in_=h2s[:, :, g0:g0 + gn]))(g0, gn), writes=["h2g"], dma="h2g")
                P.op("sync", (lambda g0, gn: lambda e: e.dma_start(out=x1g[:, :, 0:gn], in_=x1s[:, :, g0:g0 + gn]))(g0, gn), writes=["x1g"], dma="x1g")
                if gi + 1 < len(groups):
                    load_sel(gi + 1)
                if gi > 0:
                    pend_a.extend(build_pieces(gi, 1))
                per_slot = 3

                def slot_of(c, gi=gi):
                    return (c // CPD + gi * (128 // CPD)) % 2

                def emit_dma(c):
                    slot = slot_of(c)
                    e0 = c * 128
                    P.op("sync", (lambda slot, e0: lambda e: e.dma_start(out=utb[slot][:], in_=UTb[:, e0:e0 + CPD * 128].rearrange("(k p) n -> p k n", p=128)))(slot, e0),
                         writes=[f"utb{slot}"], dma=f"utb{slot}")
                    P.op("sync", (lambda slot, e0: lambda e: e.dma_start(out=vbb[slot][:], in_=Vb[e0:e0 + CPD * 128, :].rearrange("(a p) n -> p a n", p=128)))(slot, e0),
                         writes=[f"vbb{slot}"], dma=f"vbb{slot}")

                def emit_m1(c, gn=gn):
                    slot = slot_of(c)
                    ci = c % CPD
                    for kc in range(8):
                        P.op("tensor", (lambda slot, ci, kc, gn: lambda e: e.matmul(pm[0][:, 0:gn], lhsT=utb[slot][:, kc, ci * 128:(ci + 1) * 128], rhs=h2g[:, kc, 0:gn], start=(kc == 0), stop=(kc == 7)))(slot, ci, kc, gn),
                             reads=[f"utb{slot}", "h2g"], writes=["bk6"])

                def emit_act(c, gn=gn):
                    a_, w_ = ag[c % 2], wT[c % 2]
                    P.op("scalar", (lambda a_, gn: lambda e: e.activation(out=a_[:, 0:gn], in_=pm[0][:, 0:gn], func=AF.Gelu))(a_, gn), reads=["bk6"], writes=[f"ag{c % 2}"])
                    P.op("vector", (lambda a_, w_, c, gn: lambda e: e.tensor_tensor(out=w_[:, 0:gn], in0=a_[:, 0:gn], in1=Gbuf[:, 0:gn, c], op=ALU.mult))(a_, w_, c, gn),
                         reads=[f"ag{c % 2}", f"Gbuf{c // 64}"], writes=[f"wT{c % 2}"])

                def emit_m2(c, gn=gn):
                    slot = slot_of(c)
                    ci = c % CPD
                    w_ = wT[c % 2]
                    for tt in range(gn // 128):
                        for dh in range(2):
                            bi = tt * 2 + dh
                            P.op("tensor", (lambda w_, slot, ci, tt, dh, bi, c: lambda e: e.matmul(BK[bi][:, :], lhsT=w_[:, tt * 128:(tt + 1) * 128], rhs=vbb[slot][:, ci, dh * 512:(dh + 1) * 512],
                                                                                                  start=(c == 0), stop=(c == 127)))(w_, slot, ci, tt, dh, bi, c),
                                 reads=[f"vbb{slot}", f"wT{c % 2}"], writes=[f"bk{bi}"])

                for c in range(128):
                    if c == 64:
                        build_flush()
                        if gi + 1 < len(groups):
                            pend_a.extend(build_pieces(gi + 1, 0))
                    if c % CPD == 0:
                        emit_dma(c)
                    emit_m1(c)
                    if c > 0:
                        emit_m2(c - 1)
                    emit_act(c)
                    build_step(per_slot)
                emit_m2(127)
                build_flush()
                for tt in range(gn // 128):
                    for dh in range(2):
                        bi = tt * 2 + dh
                        if bi % 2 == 0:
                            P.op("scalar", (lambda tt, dh, bi: lambda e: e.copy(out=peer_sb[:, tt, dh * 512:(dh + 1) * 512], in_=BK[bi][:, :]))(tt, dh, bi), reads=[f"bk{bi}"], writes=[f"peer{tt}"])
                        else:
                            P.op("vector", (lambda tt, dh, bi: lambda e: e.tensor_copy(out=peer_sb[:, tt, dh * 512:(dh + 1) * 512], in_=BK[bi][:, :]))(tt, dh, bi), reads=[f"bk{bi}"], writes=[f"peer{tt}"])
                for j in range(8):
                    for tt in range(gn // 128):
                        P.op("tensor", (lambda j, tt: lambda e: e.transpose(out=BK[j][:, tt * 128:(tt + 1) * 128], in_=peer_sb[:, tt, j * 128:(j + 1) * 128], identity=ident_f[:]))(j, tt),
                             reads=[f"peer{tt}"], writes=bkk(j))
                segs = []
                tcur = g0
                while tcur < g0 + gn:
                    bb = tcur // seq
                    tend = min((bb + 1) * seq, g0 + gn)
                    segs.append((bb, tcur - g0, tend - g0))
                    tcur = tend
                for j in range(8):
                    for (bb, s0, s1) in segs:
                        P.op("vector", (lambda j, bb, s0, s1: lambda e: e.scalar_tensor_tensor(out=x1g[:, j, s0:s1], in0=BK[j][:, s0:s1], scalar=dcol(D_ADA + (40 + j) * nseq + bb),
                                                                                               in1=x1g[:, j, s0:s1], op0=ALU.mult, op1=ALU.add))(j, bb, s0, s1),
                             reads=bkk(j) + ["x1g"], writes=["x1g"])
                P.op("scalar", (lambda gn: lambda e: e.activation(out=sq2[:, :, 0:gn], in_=x1g[:, :, 0:gn], func=AF.Square))(gn), reads=["x1g"], writes=["sq2"])
                for kc in range(8):
                    P.op("tensor", (lambda kc, gn: lambda e: e.matmul(pm[0][:, 0:gn], lhsT=ones_bf[:], rhs=sq2[:, kc, 0:gn], start=(kc == 0), stop=(kc == 7)))(kc, gn), reads=["sq2"], writes=["bk6"])
                P.op("scalar", (lambda gn: lambda e: e.activation(out=rstd2[:, 0:gn], in_=pm[0][:, 0:gn], func=AF.Sqrt, bias=eps_t[:], scale=1.0 / D))(gn), reads=["bk6"], writes=["rstd2"])
                P.op("vector", (lambda gn: lambda e: e.reciprocal(out=rstd2[:, 0:gn], in_=rstd2[:, 0:gn]))(gn), reads=["rstd2"], writes=["rstd2"])
                P.op("vector", (lambda gn: lambda e: e.tensor_tensor(out=x1g[:, :, 0:gn], in0=x1g[:, :, 0:gn], in1=rstd2[:, 0:gn].unsqueeze(1).to_broadcast([128, 8, gn]), op=ALU.mult))(gn),
                     reads=["x1g", "rstd2"], writes=["x1g"])
                for j in range(8):
                    P.op("scalar", (lambda j, gn: lambda e: e.activation(out=x1g[:, j, 0:gn], in_=x1g[:, j, 0:gn], func=AF.Copy, scale=pcol(P_GF + j)))(j, gn), reads=["x1g"], writes=["x1g"])
                P.op("sync", (lambda g0, gn: lambda e: e.dma_start(out=yT[:, g0:g0 + gn].rearrange("(c p) t -> p c t", p=128), in_=x1g[:, :, 0:gn]))(g0, gn), reads=["x1g"], dma="yst")
            import os as _os2
            _mxb = _os2.environ.get("PROG_MAXOPS_B")
            print("phase B ops", len(P.ops))
            if _mxb:
                for _i, _o in enumerate(P.ops[:int(_mxb)]):
                    if _i >= int(_mxb) - 14:
                        print("OP", _i, _o["eng"], _o["dma"], "r", _o["r"], "w", _o["w"], "deps", sorted(_o["deps"]))
            P.emit(int(_mxb) if _mxb else None)
    return nc


def host_layout(inputs, core, seq, nseq):
    x = np.asarray(inputs["x"])
    c = np.asarray(inputs["c"])
    b0 = core * nseq
    ntok = seq * nseq
    xT = np.ascontiguousarray(x[b0:b0 + nseq, :seq].reshape(ntok, D).T)
    cT = np.ascontiguousarray(c[b0:b0 + nseq].reshape(nseq, 8, 128).transpose(2, 1, 0))
    return xT, cT


def shared_layout(inputs):
    f = lambda k: np.asarray(inputs[k], dtype=np.float32)
    kcp = lambda w: np.ascontiguousarray(w.reshape(8, 128, w.shape[1]).transpose(1, 0, 2))
    colT = lambda v, n: v.reshape(n, 128).T
    w_in = f("w_in")
    b_in = f("b_in")
    q_c = list(range(0, 512))
    k0, k1 = list(range(512, 576)), list(range(576, 640))
    v_c = list(range(640, 768))
    B_c = list(range(768, 1280))
    C_c = list(range(1280, 1792))
    U_c = list(range(1792, 2304))
    order = q_c + k0 + k0 + k1 + k1 + B_c + C_c + U_c + v_c
    w_in_r = kcp(w_in[:, order])
    b_in_r = b_in[order]
    prm = np.zeros((128, NP), np.float32)
    prm[:, P_BADA:P_BADA + 48] = colT(f("b_ada"), 48)
    prm[:, P_G1:P_G1 + 8] = colT(f("norm1_g"), 8)
    prm[:, P_G2:P_G2 + 8] = colT(f("norm2_g"), 8)
    prm[:, P_GF:P_GF + 8] = colT(f("final_g"), 8)
    prm[:, P_BOUT:P_BOUT + 8] = colT(f("b_out"), 8)
    prm[:, P_BIN:P_BIN + 18] = colT(b_in_r[:2304], 18)
    prm[:, P_AG:P_AG + 4] = colT(f("attn_out_g"), 4)
    prm[:, P_CG:P_CG + 4] = colT(f("conv_out_g"), 4)
    cw = f("conv_w")
    for tap in range(3):
        prm[:, P_CW + tap * 4:P_CW + tap * 4 + 4] = colT(cw[tap], 4)
    prm[:, P_SINK:P_SINK + 4] = np.repeat(f("attn_sinks"), 64).reshape(4, 128).T
    prm[:, P_BV:P_BV + 128] = np.broadcast_to(b_in_r[2304:2432][None, :], (128, 128))
    k1_, k2_ = f("peer_keys1"), f("peer_keys2")
    keys = np.stack([k1_, k2_], axis=1).reshape(16, 128, 128)
    keysT = np.ascontiguousarray(keys.transpose(2, 0, 1))
    U = f("peer_u")
    V = f("peer_v")
    UT = np.ascontiguousarray(U.reshape(128, 128, D).transpose(2, 1, 0).reshape(D, 16384))
    Vr = np.ascontiguousarray(V.reshape(128, 128, D).transpose(1, 0, 2).reshape(16384, D))
    return dict(w_ada=kcp(f("w_ada")), prm=prm, w_in=w_in_r, w_out=kcp(f("w_out")), w_q=kcp(f("w_query")), keysT=keysT, UT=UT, Vr=Vr)


def run(inputs, seq=2048, nseq=2, n_cores=N_CORES, stop_after=9):
    nc = build_nc(seq, nseq, stop_after=stop_after)
    shared = shared_layout(inputs)
    in_maps = []
    for core in range(n_cores):
        xT, cT = host_layout(inputs, core, seq, nseq)
        m = dict(shared)
        m["xT"] = xT
        m["cT"] = cT
        in_maps.append(m)
    res = run_bass_kernel_spmd(nc, in_maps, core_ids=list(range(n_cores)))
    outs = [np.asarray(r["yT"]).T.reshape(nseq, seq, D) for r in res.results]
    return np.concatenate(outs, axis=0).astype(np.float32)


def kernel(**inputs):
    return run(inputs)
```

```python
import numpy as np
from contextlib import ExitStack
import concourse.bass as bass
import concourse.mybir as mybir
from concourse.bass_utils import run_bass_kernel_spmd

F32 = mybir.dt.float32
BF16 = mybir.dt.bfloat16
U32 = mybir.dt.uint32
AF = mybir.ActivationFunctionType
ALU = mybir.AluOpType
AX = mybir.AxisListType

ENGS = ("tensor", "vector", "scalar", "gpsimd", "sync")
N_CORES = 8
D = 1024
EPS = 1e-6
NEGBIG = -1e30


class Prog:
    def __init__(self, nc, same_engine_sync=True):
        self.nc = nc
        self.ops = []
        self.last_w = {}
        self.readers = {}
        self.same_engine_sync = same_engine_sync
        self.dma_groups = {}

    def op(self, eng, fn, reads=(), writes=(), dma=None):
        i = len(self.ops)
        deps = set()
        for r in reads:
            if r in self.last_w:
                deps.add(self.last_w[r])
        for w in writes:
            if w in self.last_w:
                deps.add(self.last_w[w])
            for rd in self.readers.get(w, ()):
                deps.add(rd)
        deps.discard(i)
        for r in reads:
            lst = self.readers.setdefault(r, [])
            if dma is None:
                lst[:] = [q for q in lst if not (self.ops[q]["dma"] is None and self.ops[q]["eng"] == eng)]
            lst.append(i)
        for w in writes:
            self.last_w[w] = i
            self.readers[w] = []
        dma_idx = None
        if dma is not None:
            self.dma_groups[dma] = self.dma_groups.get(dma, 0) + 1
            dma_idx = self.dma_groups[dma]
        self.ops.append(dict(eng=eng, fn=fn, deps=deps, dma=dma, dma_idx=dma_idx, w=list(writes), r=list(reads)))
        return i

    def emit(self, max_ops=None):
        nc = self.nc
        if max_ops is not None:
            self.ops = self.ops[:max_ops]
            self.dma_groups = {}
            for o in self.ops:
                if o["dma"] is not None:
                    self.dma_groups[o["dma"]] = max(self.dma_groups.get(o["dma"], 0), o["dma_idx"])
        ops = self.ops
        ses = self.same_engine_sync

        def skip(p, o):
            return (p["dma"] is None and o["dma"] is None and p["eng"] == o["eng"]
                    and (p["eng"] == "tensor" or not ses))

        needs_inc = [False] * len(ops)
        for o in ops:
            for i in o["deps"]:
                p = ops[i]
                if p["dma"] is None and not skip(p, o):
                    needs_inc[i] = True
        cnt = {e: 0 for e in ENGS}
        sig = [0] * len(ops)
        for i, o in enumerate(ops):
            if o["dma"] is None and needs_inc[i]:
                cnt[o["eng"]] += 1
                sig[i] = cnt[o["eng"]]
        import os as _os
        if _os.environ.get("PROG_VERBOSE"):
            print("Prog: ops", len(ops), "sem counts", cnt, "dma", self.dma_groups)
        with ExitStack() as es:
            esem = {e: es.enter_context(nc.semaphore(f"pe_{e}")) for e in ENGS}
            dsem = {g: es.enter_context(nc.semaphore(f"pd_{g}")) for g in self.dma_groups}
            block = es.enter_context(nc.Block())
            per_eng = {e: [i for i, o in enumerate(ops) if o["eng"] == e] for e in ENGS}

            def make(ename):
                def body(eng):
                    waited = {}
                    for i in per_eng[ename]:
                        o = ops[i]
                        need = {}
                        for d in o["deps"]:
                            p = ops[d]
                            if p["dma"] is not None:
                                key = ("d", p["dma"])
                                val = 16 * p["dma_idx"]
                            else:
                                if skip(p, o):
                                    continue
                                key = ("e", p["eng"])
                                val = sig[d]
                            if val > need.get(key, 0):
                                need[key] = val
                        for key, val in need.items():
                            if waited.get(key, 0) >= val:
                                continue
                            waited[key] = val
                            s = dsem[key[1]] if key[0] == "d" else esem[key[1]]
                            eng.wait_ge(s, val)
                        ins = o["fn"](eng)
                        if o["dma"] is not None:
                            ins.then_inc(dsem[o["dma"]], 16)
                        elif needs_inc[i]:
                            ins.then_inc(esem[ename], 1)
                    if ename == "sync":
                        for g, n in self.dma_groups.items():
                            eng.wait_ge(dsem[g], 16 * n)
                        for e2 in ENGS:
                            if e2 != "sync" and cnt[e2] > 0:
                                eng.wait_ge(esem[e2], cnt[e2])
                return body

            block.tensor(make("tensor"))
            block.vector(make("vector"))
            block.scalar(make("scalar"))
            block.gpsimd(make("gpsimd"))
            block.sync(make("sync"))


P_BADA, P_G1, P_G2, P_GF, P_BOUT, P_BIN, P_AG, P_CG, P_CW, P_SINK, P_BV, NP = 0, 48, 56, 64, 72, 80, 98, 102, 106, 118, 122, 250
D_ADA, D_A1, D_GB1, D_A2, D_ESINK, ND = 0, 96, 112, 128, 144, 148
NIN = 2432


def build_nc(seq, nseq=2, tga=256, tgb=384, stop_after=9):
    ntok = seq * nseq
    TA = min(tga, seq)
    nc = bass.Bass("TRN2", target_bir_lowering=False)
    dt_in = lambda name, shape: nc.dram_tensor(name, shape, F32, kind="ExternalInput").ap()
    xT = dt_in("xT", [D, ntok])
    cT = dt_in("cT", [128, 8, nseq])
    w_ada = dt_in("w_ada", [128, 8, 6144])
    prm = dt_in("prm", [128, NP])
    w_in = dt_in("w_in", [128, 8, NIN])
    w_out = dt_in("w_out", [128, 8, D])
    w_q = dt_in("w_q", [128, 8, 2048])
    keysT = dt_in("keysT", [128, 16, 128])
    UT = dt_in("UT", [D, 16384])
    Vr = dt_in("Vr", [16384, D])
    yT = nc.dram_tensor("yT", [D, ntok], F32, kind="ExternalOutput").ap()
    UTb = nc.dram_tensor("UTb", [D, 16384], BF16, kind="Internal").ap()
    Vb = nc.dram_tensor("Vb", [16384, D], BF16, kind="Internal").ap()
    x1s = nc.dram_tensor("x1s", [128, 8, ntok], F32, kind="Internal").ap()
    h2s = nc.dram_tensor("h2s", [128, 8, ntok], BF16, kind="Internal").ap()
    sels = nc.dram_tensor("sels", [128, 3, ntok], F32, kind="Internal").ap()

    with ExitStack() as g_es:
        gsb = lambda name, shape, dt: g_es.enter_context(nc.sbuf_tensor(name, shape, dt))
        prm_sb = gsb("prm_sb", [128, NP], F32)
        drv = gsb("drv", [128, ND], F32)
        ones_bf = gsb("ones_bf", [128, 128], BF16)
        blk_bf = gsb("blk_bf", [128, 128], BF16)
        mask2 = gsb("mask2", [128, 4, 128], BF16)
        ident_f = gsb("ident_f", [128, 128], F32)
        iota_f = gsb("iota_f", [128, 128], F32)
        iota_b = gsb("iota_b", [128, 128], BF16)
        esink_bc = gsb("esink_bc", [128, 4, 128], F32)
        attng_bc = gsb("attng_bc", [128, 4, 128], F32)
        eps_t = gsb("eps_t", [128, 1], F32)

        def pcol(off, n=1):
            return prm_sb[:, off:off + n]

        def dcol(off, n=1):
            return drv[:, off:off + n]

        with ExitStack() as es:
            sb = lambda name, shape, dt: es.enter_context(nc.sbuf_tensor(name, shape, dt))
            c_sb = sb("c_sb", [128, 8, nseq], F32)
            sc = sb("sc", [128, 8, nseq], F32)
            wbuf = [sb(f"wada{i}", [128, 8, 1024], F32) for i in range(2)]
            tmpf = sb("tmpf", [128, 128], F32)
            tmp16 = sb("tmp16", [128, 8, nseq], F32)
            pa = es.enter_context(nc.psum_tensor("pa", [128, 512], F32))
            P = Prog(nc)
            P.op("sync", lambda e: e.dma_start(out=prm_sb[:], in_=prm), writes=["prm"], dma="prm")
            P.op("sync", lambda e: e.dma_start(out=c_sb[:], in_=cT), writes=["c"], dma="c")
            P.op("vector", lambda e: e.memset(ones_bf[:], 1.0), writes=["ones"])
            P.op("vector", lambda e: e.memset(blk_bf[:], 0.0), writes=["blk"])
            P.op("vector", lambda e: e.memset(blk_bf[0:64, 0:64], 1.0 / 64), reads=["blk"], writes=["blk"])
            P.op("vector", lambda e: e.memset(blk_bf[64:128, 64:128], 1.0 / 64), reads=["blk"], writes=["blk"])
            P.op("vector", lambda e: e.memset(eps_t[:], EPS), writes=["eps"])
            P.op("gpsimd", lambda e: e.iota(iota_f[:], pattern=[[1, 128]], base=0, channel_multiplier=0, allow_small_or_imprecise_dtypes=True), writes=["iota"])
            P.op("vector", lambda e: e.tensor_copy(out=iota_b[:], in_=iota_f[:]), reads=["iota"], writes=["iota_b"])
            P.op("gpsimd", lambda e: e.iota(tmpf[:], pattern=[[1, 128]], base=0, channel_multiplier=-1, allow_small_or_imprecise_dtypes=True), writes=["tmpf"])
            P.op("vector", lambda e: e.tensor_single_scalar(out=ident_f[:], in_=tmpf[:], scalar=0.0, op=ALU.is_equal), reads=["tmpf"], writes=["ident"])
            for hh in range(2):
                P.op("vector", (lambda hh: lambda e: e.tensor_single_scalar(out=mask2[:, 2 * hh, :], in_=tmpf[:], scalar=0.0, op=ALU.is_lt))(hh), reads=["tmpf"], writes=["mask"])
                P.op("vector", (lambda hh: lambda e: e.tensor_single_scalar(out=mask2[:, 2 * hh + 1, :], in_=tmpf[:], scalar=0.0, op=ALU.is_ge))(hh), reads=["tmpf"], writes=["mask"])
            P.op("scalar", lambda e: e.activation(out=sc[:], in_=c_sb[:], func=AF.Silu), reads=["c"], writes=["sc"])
            for piece in range(6):
                wb = wbuf[piece % 2]
                P.op("sync", (lambda piece, wb: lambda e: e.dma_start(out=wb[:], in_=w_ada[:, :, piece * 1024:(piece + 1) * 1024]))(piece, wb),
                     writes=[f"wb{piece % 2}"], dma=f"wada{piece % 2}")
                for jj in range(8):
                    j = piece * 8 + jj
                    for kc in range(8):
                        P.op("tensor", (lambda wb, jj, j, kc: lambda e: e.matmul(pa[:, j * nseq:(j + 1) * nseq], lhsT=wb[:, kc, jj * 128:(jj + 1) * 128],
                                                                                 rhs=sc[:, kc, :], start=(kc == 0), stop=(kc == 7)))(wb, jj, j, kc),
                             reads=[f"wb{piece % 2}", "sc"], writes=["pa"])
            ada_v = drv[:, D_ADA:D_ADA + 48 * nseq].rearrange("p (j b) -> p j b", b=nseq)
            P.op("vector", lambda e: e.tensor_tensor(out=ada_v, in0=pa[:, 0:48 * nseq].rearrange("p (j b) -> p j b", b=nseq),
                                                     in1=prm_sb[:, P_BADA:P_BADA + 48].unsqueeze(2).to_broadcast([128, 48, nseq]), op=ALU.add),
                 reads=["pa", "prm"], writes=["drv"])

            def adav(j0):
                return drv[:, D_ADA + j0 * nseq:D_ADA + (j0 + 8) * nseq].rearrange("p (j b) -> p j b", b=nseq)

            def dv(off):
                return drv[:, off:off + 8 * nseq].rearrange("p (j b) -> p j b", b=nseq)

            def pb(off):
                return prm_sb[:, off:off + 8].unsqueeze(2).to_broadcast([128, 8, nseq])
            P.op("vector", lambda e: e.tensor_scalar(out=tmp16[:], in0=adav(8), scalar1=1.0, scalar2=None, op0=ALU.add), reads=["drv"], writes=["tmp16"])
            P.op("vector", lambda e: e.tensor_tensor(out=dv(D_A1), in0=tmp16[:], in1=pb(P_G1), op=ALU.mult), reads=["tmp16", "prm"], writes=["drvA1"])
            P.op("vector", lambda e: e.tensor_tensor(out=dv(D_GB1), in0=adav(16), in1=pb(P_BOUT), op=ALU.mult), reads=["drv", "prm"], writes=["drvGB1"])
            P.op("vector", lambda e: e.tensor_scalar(out=tmp16[:], in0=adav(32), scalar1=1.0, scalar2=None, op0=ALU.add), reads=["drv", "drvA1"], writes=["tmp16"])
            P.op("vector", lambda e: e.tensor_tensor(out=dv(D_A2), in0=tmp16[:], in1=pb(P_G2), op=ALU.mult), reads=["tmp16", "prm"], writes=["drvA2"])
            P.op("scalar", lambda e: e.activation(out=drv[:, D_ESINK:D_ESINK + 4], in_=prm_sb[:, P_SINK:P_SINK + 4], func=AF.Exp), reads=["prm"], writes=["esink"])
            P.op("vector", lambda e: e.tensor_copy(out=esink_bc[:], in_=drv[:, D_ESINK:D_ESINK + 4].unsqueeze(2).to_broadcast([128, 4, 128])), reads=["esink"], writes=["esink_bc"])
            P.op("vector", lambda e: e.tensor_copy(out=attng_bc[:], in_=prm_sb[:, P_AG:P_AG + 4].unsqueeze(2).to_broadcast([128, 4, 128])), reads=["prm"], writes=["attng_bc"])
            P.emit()
        nc.all_engine_barrier()
        if stop_after < 1:
            return nc

        with ExitStack() as es:
            sb = lambda name, shape, dt: es.enter_context(nc.sbuf_tensor(name, shape, dt))
            win_sb = sb("win_sb", [128, 8, NIN], BF16)
            wout_sb = sb("wout_sb", [128, 8, D], BF16)
            wq_sb = sb("wq_sb", [128, 8, 2048], BF16)
            keys_sb = sb("keys_sb", [128, 16, 128], BF16)
            xt = [sb(f"xt{i}", [128, 8, TA], F32) for i in range(2)]
            sq = sb("sq", [128, 8, TA], BF16)
            rstd = sb("rstd", [128, TA], F32)
            tmpn = sb("tmpn", [128, 8, TA], F32)
            hT = sb("hT", [128, 8, TA], BF16)
            qT = sb("qT", [128, 4, TA], BF16)
            kT = sb("kT", [128, 2, 128 + TA], BF16)
            NB = TA // 128
            vtok = sb("vtok", [128, 1 + NB, 128], BF16)
            Bt = sb("Bt", [128, 4, TA], F32)
            zb = sb("zb", [128, 4, 2 + TA], F32)
            acc = sb("acc", [128, 4, TA], F32)
            Ct = acc
            sqc = sq[:, 0:4, :]
            rsc = sb("rsc", [128, TA], F32)
            catT = sb("catT", [128, 8, TA], BF16)
            pT = [sb(f"pT{i}", [128, 4, 128], BF16) for i in range(4)]
            t1 = sb("t1", [128, 4, 128], F32)
            yat = sb("yat", [128, 4, 128], F32)
            sqa = sb("sqa", [128, 4, 128], BF16)
            t2 = sb("t2", [128, 4, 128], F32)
            otmp = sb("otmp", [128, TA], F32)
            h2T = hT
            qpT = sb("qpT", [128, 16, TA], BF16)
            S_sb = sb("S_sb", [128, NB, 16, 128], F32)
            Vt = sb("Vt", [128, 16, 16], F32)
            It = sb("It", [128, 16, 16], U32)
            Itf = sb("Itf", [128, 16, 16], F32)
            cand = sb("cand", [128, 8, 256], F32)
            SC = sb("SC", [128, 8, 16], F32)
            CI = sb("CI", [128, 8, 16], U32)
            au = sb("au", [128, 8, 16], U32)
            bu = sb("bu", [128, 8, 16], U32)
            af_ = sb("af_", [128, 128], F32)
            bf_ = sb("bf_", [128, 128], F32)
            oh = tmpn[:].rearrange("p c t -> p (c t)").rearrange("p (a b) -> p a b", b=16)
            sel = sb("sel", [128, 3, 128], F32)
            ee = sb("ee", [128, 8, 16], F32)
            zz = sb("zz", [128, 8], F32)
            selT = sb("selT", [128, 3, 128], F32)
            ps = [es.enter_context(nc.psum_tensor(f"ps{i}", [128, 512], F32)) for i in range(8)]
            P = Prog(nc)
            for kc in range(8):
                P.op("gpsimd", (lambda kc: lambda e: e.dma_start(out=win_sb[:, kc, :], in_=w_in[:, kc, :]))(kc), writes=[f"win{kc}"], dma=f"w_in{kc}")
            P.op("gpsimd", lambda e: e.dma_start(out=keys_sb[:], in_=keysT), writes=["keys"], dma="w_k")
            for kc in range(0, 8, 2):
                P.op("gpsimd", (lambda kc: lambda e: e.dma_start(out=wout_sb[:, kc:kc + 2, :], in_=w_out[:, kc:kc + 2, :]))(kc), writes=[f"wout{kc}", f"wout{kc + 1}"], dma=f"w_out{kc}")
            for kc in range(8):
                P.op("gpsimd", (lambda kc: lambda e: e.dma_start(out=wq_sb[:, kc, :], in_=w_q[:, kc, :]))(kc), writes=[f"wq{kc}"], dma=f"w_q{kc}")
            NPC = 16
            for i in range(NPC):
                r0, r1 = i * (D // NPC), (i + 1) * (D // NPC)
                P.op("gpsimd", (lambda r0, r1: lambda e: e.dma_start(out=UTb[r0:r1, :], in_=UT[r0:r1, :]))(r0, r1), dma="cvt")
            for i in range(NPC):
                r0, r1 = i * (16384 // NPC), (i + 1) * (16384 // NPC)
                P.op("gpsimd", (lambda r0, r1: lambda e: e.dma_start(out=Vb[r0:r1, :], in_=Vr[r0:r1, :]))(r0, r1), dma="cvt")
            WIN = [f"win{k}" for k in range(8)]
            WOUT = [f"wout{k}" for k in range(8)]
            WQ = [f"wq{k}" for k in range(8)]
            ntile = ntok // TA
            tiles_per_seq = seq // TA

            def rms_stats(P, src, srckey, pbank, tag):
                P.op("scalar", lambda e: e.activation(out=sq[:], in_=src[:], func=AF.Square), reads=[srckey], writes=["sq"])
                for kc in range(8):
                    P.op("tensor", (lambda kc: lambda e: e.matmul(pbank[:, 0:TA], lhsT=ones_bf[:], rhs=sq[:, kc, :], start=(kc == 0), stop=(kc == 7)))(kc),
                         reads=["sq"], writes=[tag])
                P.op("scalar", lambda e: e.activation(out=rstd[:], in_=pbank[:, 0:TA], func=AF.Ln, bias=eps_t[:], scale=1.0 / D), reads=[tag], writes=["rstd"])
                P.op("scalar", lambda e: e.activation(out=rstd[:], in_=rstd[:], func=AF.Exp, scale=-0.5), reads=["rstd"], writes=["rstd"])

            pending = []

            def drain(n):
                for _ in range(min(n, len(pending))):
                    pending.pop(0)()

            def topk_pieces(blk, tq):
                pcs = []
                S = lambda j: S_sb[:, blk, j, :]
                sk = lambda j: f"S{blk}_{j}"
                J = range(16)
                H = range(8)
                pcs.append(lambda: [P.op("vector", (lambda j: lambda e: e.max(out=Vt[:, j, 0:8], in_=S(j)))(j), reads=[sk(j)], writes=[f"Vta{j}"]) for j in J])
                pcs.append(lambda: [P.op("vector", (lambda j: lambda e: e.max_index(out=It[:, j, 0:8], in_max=Vt[:, j, 0:8], in_values=S(j)))(j), reads=[sk(j), f"Vta{j}"], writes=[f"Ita{j}"]) for j in J])
                pcs.append(lambda: [P.op("vector", (lambda j: lambda e: e.match_replace(out=S(j), in_to_replace=Vt[:, j, 0:8], in_values=S(j), imm_value=NEGBIG))(j), reads=[sk(j), f"Vta{j}"], writes=[sk(j)]) for j in J])
                pcs.append(lambda: [P.op("vector", (lambda j: lambda e: e.max(out=Vt[:, j, 8:16], in_=S(j)))(j), reads=[sk(j)], writes=[f"Vtb{j}"]) for j in J])
                pcs.append(lambda: [P.op("vector", (lambda j: lambda e: e.max_index(out=It[:, j, 8:16], in_max=Vt[:, j, 8:16], in_values=S(j)))(j), reads=[sk(j), f"Vtb{j}"], writes=[f"Itb{j}"]) for j in J])
                VT = [f"Vta{j}" for j in J] + [f"Vtb{j}" for j in J]
                IT = [f"Ita{j}" for j in J] + [f"Itb{j}" for j in J]
                CAND = [f"cand{h}" for h in H]
                Vv = Vt[:].rearrange("p (h s) a -> p h s a", s=2)
                pcs.append(lambda: P.op("vector", lambda e: e.tensor_tensor(out=cand[:].rearrange("p h (a b) -> p h a b", a=16), in0=Vv[:, :, 0, :].unsqueeze(3).to_broadcast([128, 8, 16, 16]),
                                                                          in1=Vv[:, :, 1, :].unsqueeze(2).to_broadcast([128, 8, 16, 16]), op=ALU.add), reads=VT, writes=CAND))
                pcs.append(lambda: [P.op("vector", (lambda h: lambda e: e.max(out=SC[:, h, 0:8], in_=cand[:, h, :]))(h), reads=[f"cand{h}"], writes=[f"SCa{h}"]) for h in H])
                pcs.append(lambda: [P.op("vector", (lambda h: lambda e: e.max_index(out=CI[:, h, 0:8], in_max=SC[:, h, 0:8], in_values=cand[:, h, :]))(h), reads=[f"cand{h}", f"SCa{h}"], writes=[f"CIa{h}"]) for h in H])
                pcs.append(lambda: [P.op("vector", (lambda h: lambda e: e.match_replace(out=cand[:, h, :], in_to_replace=SC[:, h, 0:8], in_values=cand[:, h, :], imm_value=NEGBIG))(h), reads=[f"cand{h}", f"SCa{h}"], writes=[f"cand{h}"]) for h in H])
                pcs.append(lambda: [P.op("vector", (lambda h: lambda e: e.max(out=SC[:, h, 8:16], in_=cand[:, h, :]))(h), reads=[f"cand{h}"], writes=[f"SCb{h}"]) for h in H])
                pcs.append(lambda: [P.op("vector", (lambda h: lambda e: e.max_index(out=CI[:, h, 8:16], in_max=SC[:, h, 8:16], in_values=cand[:, h, :]))(h), reads=[f"cand{h}", f"SCb{h}"], writes=[f"CIb{h}"]) for h in H])
                SCK = [f"SCa{h}" for h in H] + [f"SCb{h}" for h in H]
                CIK = [f"CIa{h}" for h in H] + [f"CIb{h}" for h in H]

                def gates():
                    P.op("vector", lambda e: e.tensor_tensor(out=ee[:], in0=SC[:], in1=SC[:, :, 0:1].to_broadcast([128, 8, 16]), op=ALU.subtract), reads=SCK, writes=["ee"])
                    P.op("scalar", lambda e: e.activation(out=ee[:], in_=ee[:], func=AF.Exp), reads=["ee"], writes=["ee"])
                    P.op("vector", lambda e: e.tensor_reduce(out=zz[:], in_=ee[:], axis=AX.X, op=ALU.add), reads=["ee"], writes=["zz"])
                    P.op("vector", lambda e: e.reciprocal(out=zz[:], in_=zz[:]), reads=["zz"], writes=["zz"])
                    P.op("vector", lambda e: e.tensor_tensor(out=sel[:, 2, :].rearrange("p (h k) -> p h k", h=8), in0=ee[:], in1=zz[:].unsqueeze(2).to_broadcast([128, 8, 16]), op=ALU.mult),
                         reads=["ee", "zz"], writes=["sel2"])
                    P.op("vector", lambda e: e.tensor_single_scalar(out=au[:], in_=CI[:], scalar=4, op=ALU.logical_shift_right), reads=CIK, writes=["au"])
                    P.op("vector", lambda e: e.tensor_single_scalar(out=bu[:], in_=CI[:], scalar=15, op=ALU.bitwise_and), reads=CIK, writes=["bu"])
                    P.op("vector", lambda e: e.tensor_copy(out=af_[:], in_=au[:].rearrange("p h k -> p (h k)")), reads=["au"], writes=["af"])
                    P.op("vector", lambda e: e.tensor_copy(out=bf_[:], in_=bu[:].rearrange("p h k -> p (h k)")), reads=["bu"], writes=["bf"])
                    P.op("vector", lambda e: e.tensor_copy(out=Itf[:], in_=It[:]), reads=IT, writes=["Itf"])
                pcs.append(gates)

                def decode():
                    Iv = Itf[:].rearrange("p (h s) a -> p h s a", s=2)
                    for s_, src in ((0, af_), (1, bf_)):
                        P.op("vector", (lambda src: lambda e: e.tensor_tensor(out=oh, in0=iota_f[:, 0:16].unsqueeze(1).to_broadcast([128, 128, 16]),
                                                                              in1=src[:].unsqueeze(2).to_broadcast([128, 128, 16]), op=ALU.is_equal))(src),
                             reads=["af", "bf"], writes=["tmpn"])
                        P.op("gpsimd", (lambda s_: lambda e: e.tensor_tensor(out=oh.rearrange("p (h k) a -> p h k a", h=8), in0=oh.rearrange("p (h k) a -> p h k a", h=8),
                                                                             in1=Iv[:, :, s_, :].unsqueeze(2).to_broadcast([128, 8, 16, 16]), op=ALU.mult))(s_),
                             reads=["tmpn", "Itf"], writes=["tmpn"])
                        P.op("vector", (lambda s_: lambda e: e.tensor_reduce(out=sel[:, s_, :], in_=oh, axis=AX.X, op=ALU.add))(s_), reads=["tmpn"], writes=[f"sel{s_}"])
                    for s_ in range(3):
                        P.op("tensor", (lambda s_: lambda e: e.transpose(out=ps[6][:, s_ * 128:(s_ + 1) * 128], in_=sel[:, s_, :], identity=ident_f[:]))(s_),
                             reads=[f"sel{s_}"], writes=["ps6"])
                    P.op("scalar", lambda e: e.copy(out=selT[:].rearrange("p a b -> p (a b)"), in_=ps[6][:, 0:384]), reads=["ps6"], writes=["selT"])
                    P.op("sync", (lambda tq: lambda e: e.dma_start(out=sels[:, :, tq:tq + 128], in_=selT[:]))(tq), reads=["selT"], dma="selst")
                pcs.append(decode)
                return pcs

            for m in range(ntile):
                b = m // tiles_per_seq
                first = (m % tiles_per_seq == 0)
                tok0 = m * TA
                x = xt[m % 2]
                xk = f"xt{m % 2}"
                P.op("sync", (lambda x, tok0: lambda e: e.dma_start(out=x[:], in_=xT[:, tok0:tok0 + TA].rearrange("(c p) t -> p c t", p=128)))(x, tok0),
                     writes=[xk], dma=xk)
                rms_stats(P, x, xk, ps[7], "ps7")
                P.op("gpsimd", (lambda x: lambda e: e.tensor_tensor(out=tmpn[:], in0=x[:], in1=rstd[:].unsqueeze(1).to_broadcast([128, 8, TA]), op=ALU.mult))(x),
                     reads=[xk, "rstd"], writes=["tmpn"])
                for c in range(8):
                    P.op("scalar", (lambda c, b: lambda e: e.activation(out=hT[:, c, :], in_=tmpn[:, c, :], func=AF.Identity,
                                                                        scale=dcol(D_A1 + c * nseq + b), bias=dcol(D_ADA + (0 + c) * nseq + b)))(c, b),
                         reads=["tmpn"], writes=[f"hT{c}"])
                HT = [f"hT{c}" for c in range(8)]
                if first:
                    P.op("vector", lambda e: e.memset(zb[:, :, 0:2], 0.0), reads=["zb"], writes=["zb"])
                for j in range(18):
                    if j % 3 == 0 and j < 15:
                        drain(1)
                    pb_ = ps[j % 6]
                    pk = f"ps{j % 6}"
                    for kc in range(8):
                        P.op("tensor", (lambda pb_, j, kc: lambda e: e.matmul(pb_[:, 0:TA], lhsT=win_sb[:, kc, j * 128:(j + 1) * 128], rhs=hT[:, kc, :],
                                                                              start=(kc == 0), stop=(kc == 7)))(pb_, j, kc),
                             reads=[f"win{kc}", f"hT{kc}"], writes=[pk])
                    bias = pcol(P_BIN + j)
                    if j < 4:
                        P.op("scalar", (lambda pb_, j, bias: lambda e: e.activation(out=qT[:, j, :], in_=pb_[:, 0:TA], func=AF.Identity, bias=bias))(pb_, j, bias),
                             reads=[pk], writes=["qT"])
                    elif j < 6:
                        P.op("scalar", (lambda pb_, j, bias: lambda e: e.activation(out=kT[:, j - 4, 128:128 + TA], in_=pb_[:, 0:TA], func=AF.Identity, bias=bias))(pb_, j, bias),
                             reads=[pk], writes=["kTcur"])
                    elif j < 10:
                        P.op("scalar", (lambda pb_, j, bias: lambda e: e.activation(out=Bt[:, j - 6, :], in_=pb_[:, 0:TA], func=AF.Identity, bias=bias))(pb_, j, bias),
                             reads=[pk], writes=["Bt"])
                    elif j < 14:
                        P.op("scalar", (lambda pb_, j, bias: lambda e: e.activation(out=Ct[:, j - 10, :], in_=pb_[:, 0:TA], func=AF.Identity, bias=bias))(pb_, j, bias),
                             reads=[pk], writes=[f"acc{j - 10}"])
                    else:
                        P.op("vector", (lambda pb_, j, bias: lambda e: e.scalar_tensor_tensor(out=zb[:, j - 14, 2:2 + TA], in0=pb_[:, 0:TA], scalar=bias,
                                                                                               in1=Ct[:, j - 14, :], op0=ALU.add, op1=ALU.mult))(pb_, j, bias),
                             reads=[pk, f"acc{j - 14}"], writes=["zb"])
                for blk in range(NB):
                    pb_ = ps[blk % 2]
                    pk = f"ps{blk % 2}"
                    for kc in range(8):
                        P.op("tensor", (lambda pb_, blk, kc: lambda e: e.matmul(pb_[:, 0:128], lhsT=hT[:, kc, blk * 128:(blk + 1) * 128], rhs=win_sb[:, kc, 2304:2432],
                                                                                start=(kc == 0), stop=(kc == 7)))(pb_, blk, kc),
                             reads=[f"win{kc}", f"hT{kc}"], writes=[pk])
                    P.op("vector", (lambda pb_, blk: lambda e: e.tensor_tensor(out=vtok[:, 1 + blk, :], in0=pb_[:, 0:128], in1=prm_sb[:, P_BV:P_BV + 128], op=ALU.add))(pb_, blk),
                         reads=[pk], writes=["vcur"])
                for cc in range(4):
                    P.op("scalar", (lambda cc: lambda e: e.activation(out=acc[:, cc, :], in_=zb[:, cc, 2:2 + TA], func=AF.Copy, scale=pcol(P_CW + 2 * 4 + cc)))(cc),
                         reads=["zb"], writes=[f"acc{cc}"])
                    P.op("vector", (lambda cc: lambda e: e.scalar_tensor_tensor(out=acc[:, cc, :], in0=zb[:, cc, 1:1 + TA], scalar=pcol(P_CW + 1 * 4 + cc), in1=acc[:, cc, :],
                                                                                op0=ALU.mult, op1=ALU.add))(cc), reads=["zb", f"acc{cc}"], writes=[f"acc{cc}"])
                    P.op("vector", (lambda cc: lambda e: e.scalar_tensor_tensor(out=acc[:, cc, :], in0=zb[:, cc, 0:TA], scalar=pcol(P_CW + 0 * 4 + cc), in1=acc[:, cc, :],
                                                                                op0=ALU.mult, op1=ALU.add))(cc), reads=["zb", f"acc{cc}"], writes=[f"acc{cc}"])
                    P.op("gpsimd", (lambda cc: lambda e: e.tensor_tensor(out=acc[:, cc, :], in0=acc[:, cc, :], in1=Bt[:, cc, :], op=ALU.mult))(cc),
                         reads=["Bt", f"acc{cc}"], writes=[f"acc{cc}"])
                ACC = [f"acc{cc}" for cc in range(4)]
                P.op("gpsimd", lambda e: e.tensor_copy(out=zb[:, :, 0:2], in_=zb[:, :, TA:TA + 2]), reads=["zb"] + ACC, writes=["zb"])
                P.op("scalar", lambda e: e.activation(out=sqc, in_=acc[:], func=AF.Square), reads=ACC, writes=["sq"])
                for cc in range(4):
                    pb_ = ps[2 + cc % 2]
                    pk = f"ps{2 + cc % 2}"
                    P.op("tensor", (lambda pb_, cc: lambda e: e.matmul(pb_[:, 0:TA], lhsT=blk_bf[:], rhs=sqc[:, cc, :], start=True, stop=True))(pb_, cc),
                         reads=["sq"], writes=[pk])
                    P.op("scalar", (lambda pb_: lambda e: e.activation(out=rsc[:], in_=pb_[:, 0:TA], func=AF.Ln, bias=eps_t[:], scale=1.0))(pb_), reads=[pk], writes=["rsc"])
                    P.op("scalar", lambda e: e.activation(out=rsc[:], in_=rsc[:], func=AF.Exp, scale=-0.5), reads=["rsc"], writes=["rsc"])
                    P.op("vector", (lambda cc: lambda e: e.scalar_tensor_tensor(out=catT[:, 4 + cc, :], in0=acc[:, cc, :], scalar=pcol(P_CG + cc), in1=rsc[:],
                                                                                op0=ALU.mult, op1=ALU.mult))(cc), reads=[f"acc{cc}", "rsc"], writes=[f"cat{4 + cc}"])
                for blk in range(NB):
                    hasprev = not (first and blk == 0)
                    q0 = blk * 128
                    kprev = slice(q0, q0 + 128)
                    kcur = slice(128 + q0, 256 + q0)
                    vprev, vcur = blk, blk + 1
                    po = ps[4]
                    pd = ps[5]
                    for pp in range(2):
                        g = pp
                        for ci in range(2):
                            c = 2 * pp + ci
                            for hh in range(2):
                                lo, hi = hh * 64, hh * 64 + 64
                                bank = ps[2 * pp + hh]
                                pk = f"ps{2 * pp + hh}"
                                if hasprev:
                                    P.op("tensor", (lambda bank, ci, lo, hi, g, c, kprev, q0: lambda e: e.matmul(bank[:, ci * 256:ci * 256 + 128], lhsT=kT[lo:hi, g, kprev],
                                                                                                               rhs=qT[lo:hi, c, q0:q0 + 128], start=True, stop=True))(bank, ci, lo, hi, g, c, kprev, q0),
                                         reads=["qT", "kTcur", "kTprev"], writes=[pk])
                                P.op("tensor", (lambda bank, ci, lo, hi, g, c, kcur, q0: lambda e: e.matmul(bank[:, ci * 256 + 128:ci * 256 + 256], lhsT=kT[lo:hi, g, kcur],
                                                                                                          rhs=qT[lo:hi, c, q0:q0 + 128], start=True, stop=True))(bank, ci, lo, hi, g, c, kcur, q0),
                                     reads=["qT", "kTcur", "kTprev"], writes=[pk])
                        drain(1)
                        for hh in range(2):
                            bank = ps[2 * pp + hh]
                            pk = f"ps{2 * pp + hh}"
                            pt = pT[2 * pp + hh]
                            ptk = f"pT{2 * pp + hh}"
                            if hasprev:
                                P.op("scalar", (lambda bank, pt: lambda e: e.activation(out=pt[:].rearrange("p a b -> p (a b)"), in_=bank[:, :], func=AF.Exp, scale=0.125))(bank, pt),
                                     reads=[pk], writes=[ptk])
                                P.op("gpsimd", (lambda pt: lambda e: e.tensor_tensor(out=pt[:], in0=pt[:], in1=mask2[:], op=ALU.mult))(pt), reads=[ptk], writes=[ptk])
                            else:
                                psv = bank[:, :].rearrange("p (h a i) -> p h a i", h=2, a=2)[:, :, 1, :]
                                ptv = pt[:].rearrange("p (h a) i -> p h a i", h=2)[:, :, 1, :]
                                mkv = mask2[:].rearrange("p (h a) i -> p h a i", h=2)[:, :, 1, :]
                                P.op("scalar", (lambda psv, ptv: lambda e: e.activation(out=ptv, in_=psv, func=AF.Exp, scale=0.125))(psv, ptv), reads=[pk], writes=[ptk])
                                P.op("gpsimd", (lambda ptv, mkv: lambda e: e.tensor_tensor(out=ptv, in0=ptv, in1=mkv, op=ALU.mult))(ptv, mkv), reads=[ptk], writes=[ptk])
                        for ci in range(2):
                            c = 2 * pp + ci
                            for hh in range(2):
                                lo, hi = hh * 64, hh * 64 + 64
                                pt = pT[2 * pp + hh]
                                ptk = f"pT{2 * pp + hh}"
                                oc = slice(c * 128, (c + 1) * 128)
                                if hasprev:
                                    P.op("tensor", (lambda pt, ci, lo, hi, g, oc, vprev: lambda e: e.matmul(po[lo:hi, oc], lhsT=vtok[:, vprev, g * 64:(g + 1) * 64], rhs=pt[:, 2 * ci, :],
                                                                                                          start=True, stop=False))(pt, ci, lo, hi, g, oc, vprev),
                                         reads=[ptk, "vcur", "vprev"], writes=["ps4"])
                                P.op("tensor", (lambda pt, ci, lo, hi, g, oc, vcur, hasprev: lambda e: e.matmul(po[lo:hi, oc], lhsT=vtok[:, vcur, g * 64:(g + 1) * 64], rhs=pt[:, 2 * ci + 1, :],
                                                                                                              start=(not hasprev), stop=True))(pt, ci, lo, hi, g, oc, vcur, hasprev),
                                     reads=[ptk, "vcur", "vprev"], writes=["ps4"])
                                if hasprev:
                                    P.op("tensor", (lambda pt, ci, lo, hi, oc: lambda e: e.matmul(pd[lo:hi, oc], lhsT=ones_bf[:, 0:64], rhs=pt[:, 2 * ci, :], start=True, stop=False))(pt, ci, lo, hi, oc),
                                         reads=[ptk], writes=["ps5"])
                                P.op("tensor", (lambda pt, ci, lo, hi, oc, hasprev: lambda e: e.matmul(pd[lo:hi, oc], lhsT=ones_bf[:, 0:64], rhs=pt[:, 2 * ci + 1, :], start=(not hasprev), stop=True))(pt, ci, lo, hi, oc, hasprev),
                                     reads=[ptk], writes=["ps5"])
                    pov = po[:, :].rearrange("p (c i) -> p c i", c=4)
                    pdv = pd[:, :].rearrange("p (c i) -> p c i", c=4)
                    P.op("vector", lambda e: e.tensor_tensor(out=t1[:], in0=pdv, in1=esink_bc[:], op=ALU.add), reads=["ps5"], writes=["t1"])
                    P.op("scalar", lambda e: e.activation(out=t1[:], in_=t1[:], func=AF.Ln), reads=["t1"], writes=["t1"])
                    P.op("scalar", lambda e: e.activation(out=t1[:], in_=t1[:], func=AF.Exp, scale=-1.0), reads=["t1"], writes=["t1"])
                    P.op("vector", lambda e: e.tensor_tensor(out=yat[:], in0=pov, in1=t1[:], op=ALU.mult), reads=["ps4", "t1"], writes=["yat"])
                    P.op("scalar", lambda e: e.activation(out=sqa[:], in_=yat[:], func=AF.Square), reads=["yat"], writes=["sqa"])
                    P.op("tensor", lambda e: e.matmul(ps[6][:, :], lhsT=blk_bf[:], rhs=sqa[:].rearrange("p c i -> p (c i)"), start=True, stop=True), reads=["sqa"], writes=["ps6"])
                    P.op("scalar", lambda e: e.activation(out=t2[:].rearrange("p c i -> p (c i)"), in_=ps[6][:, :], func=AF.Ln, bias=eps_t[:], scale=1.0), reads=["ps6"], writes=["t2"])
                    P.op("scalar", lambda e: e.activation(out=t2[:], in_=t2[:], func=AF.Exp, scale=-0.5), reads=["t2"], writes=["t2"])
                    P.op("vector", lambda e: e.tensor_tensor(out=yat[:], in0=yat[:], in1=t2[:], op=ALU.mult), reads=["yat", "t2"], writes=["yat"])
                    P.op("vector", (lambda q0: lambda e: e.tensor_tensor(out=catT[:, 0:4, q0:q0 + 128], in0=yat[:], in1=attng_bc[:], op=ALU.mult))(q0),
                         reads=["yat"], writes=[f"cat{c}" for c in range(4)])
                P.op("gpsimd", lambda e: e.tensor_copy(out=kT[:, :, 0:128], in_=kT[:, :, TA:TA + 128]), reads=["kTcur", "kTprev"], writes=["kTprev", "kTcur"])
                P.op("gpsimd", lambda e: e.tensor_copy(out=vtok[:, 0, :], in_=vtok[:, NB, :]), reads=["vcur", "vprev"], writes=["vprev", "vcur"])
                CAT = [f"cat{c}" for c in range(8)]
                for j in range(8):
                    if j % 2 == 0:
                        drain(1)
                    pb_ = ps[j % 6]
                    pk = f"ps{j % 6}"
                    for kc in range(8):
                        P.op("tensor", (lambda pb_, j, kc: lambda e: e.matmul(pb_[:, 0:TA], lhsT=wout_sb[:, kc, j * 128:(j + 1) * 128], rhs=catT[:, kc, :], start=(kc == 0), stop=(kc == 7)))(pb_, j, kc),
                             reads=[f"wout{kc}", f"cat{kc}"], writes=[pk])
                    P.op("scalar", (lambda pb_, j, b: lambda e: e.activation(out=otmp[:], in_=pb_[:, 0:TA], func=AF.Identity, scale=dcol(D_ADA + (16 + j) * nseq + b),
                                                                             bias=dcol(D_GB1 + j * nseq + b)))(pb_, j, b), reads=[pk], writes=["otmp"])
                    P.op("gpsimd", (lambda x, j: lambda e: e.tensor_tensor(out=x[:, j, :], in0=x[:, j, :], in1=otmp[:], op=ALU.add))(x, j), reads=["otmp", xk], writes=[xk])
                P.op("sync", (lambda x, tok0: lambda e: e.dma_start(out=x1s[:, :, tok0:tok0 + TA], in_=x[:]))(x, tok0), reads=[xk], dma=f"x1st{m % 2}")
                rms_stats(P, x, xk, ps[7], "ps7")
                P.op("gpsimd", (lambda x: lambda e: e.tensor_tensor(out=tmpn[:], in0=x[:], in1=rstd[:].unsqueeze(1).to_broadcast([128, 8, TA]), op=ALU.mult))(x),
                     reads=[xk, "rstd"], writes=["tmpn"])
                for c in range(8):
                    P.op("scalar", (lambda c, b: lambda e: e.activation(out=h2T[:, c, :], in_=tmpn[:, c, :], func=AF.Identity,
                                                                        scale=dcol(D_A2 + c * nseq + b), bias=dcol(D_ADA + (24 + c) * nseq + b)))(c, b),
                         reads=["tmpn"], writes=[f"hT{c}"])
                H2 = [f"hT{c}" for c in range(8)]
                P.op("sync", (lambda tok0: lambda e: e.dma_start(out=h2s[:, :, tok0:tok0 + TA], in_=h2T[:]))(tok0), reads=H2, dma="h2st")
                for j in range(16):
                    if j % 2 == 0:
                        drain(1)
                    pb_ = ps[j % 6]
                    pk = f"ps{j % 6}"
                    for kc in range(8):
                        P.op("tensor", (lambda pb_, j, kc: lambda e: e.matmul(pb_[:, 0:TA], lhsT=wq_sb[:, kc, j * 128:(j + 1) * 128], rhs=h2T[:, kc, :], start=(kc == 0), stop=(kc == 7)))(pb_, j, kc),
                             reads=[f"wq{kc}", f"hT{kc}"], writes=[pk])
                    P.op("scalar", (lambda pb_, j: lambda e: e.copy(out=qpT[:, j, :], in_=pb_[:, 0:TA]))(pb_, j), reads=[pk], writes=[f"qp{j}"])
                drain(len(pending))
                for blk in range(NB):
                    q0 = blk * 128
                    for j4 in range(4):
                        pb_ = ps[2 + j4]
                        pk = f"ps{2 + j4}"
                        for jj in range(4):
                            j = j4 * 4 + jj
                            P.op("tensor", (lambda pb_, j, jj, q0: lambda e: e.matmul(pb_[:, jj * 128:(jj + 1) * 128], lhsT=qpT[:, j, q0:q0 + 128], rhs=keys_sb[:, j, :], start=True, stop=True))(pb_, j, jj, q0),
                                 reads=[f"qp{j}", "keys"], writes=[pk])
                        P.op("scalar", (lambda pb_, j4, blk: lambda e: e.copy(out=S_sb[:, blk, j4 * 4:(j4 + 1) * 4, :].rearrange("p a b -> p (a b)"), in_=pb_[:, :]))(pb_, j4, blk),
                             reads=[pk], writes=[f"S{blk}_{j4 * 4 + jj}" for jj in range(4)])
                for blk in range(NB):
                    pending.extend(topk_pieces(blk, tok0 + blk * 128))
            while pending:
                pending.pop(0)()
            import os as _os
            _mx = _os.environ.get("PROG_MAXOPS")
            print("phase A ops", len(P.ops))
            if _mx:
                for _i, _o in enumerate(P.ops[:int(_mx)][-3:]):
                    print("last ops", _o["eng"], _o["dma"])
            P.emit(int(_mx) if _mx else None)
        nc.all_engine_barrier()
        if stop_after < 2:
            return nc

        with ExitStack() as es:
            sb = lambda name, shape, dt: es.enter_context(nc.sbuf_tensor(name, shape, dt))
            TG = min(tgb, ntok)
            Gbuf = sb("Gbuf", [128, TG, 128], BF16)
            CPD = 4
            utb = [sb(f"utb{i}", [128, 8, CPD * 128], BF16) for i in range(2)]
            vbb = [sb(f"vbb{i}", [128, CPD, D], BF16) for i in range(2)]
            h2g = sb("h2g", [128, 8, TG], BF16)
            x1g = sb("x1g", [128, 8, TG], F32)
            selg = [sb(f"selg{i}", [128, 3, TG], F32) for i in range(2)]
            NAB = 16
            A1 = [sb(f"A1_{i}", [128, 128], BF16) for i in range(NAB)]
            A2 = [sb(f"A2_{i}", [128, 64], BF16) for i in range(NAB)]
            ag = [sb(f"ag{i}", [128, TG], BF16) for i in range(2)]
            wT = [sb(f"wT{i}", [128, TG], BF16) for i in range(2)]
            sq2 = sb("sq2", [128, 8, TG], BF16)
            rstd2 = sb("rstd2", [128, TG], F32)
            peer_sb = sb("peer_sb", [128, TG // 128, D], F32)
            BK = [es.enter_context(nc.psum_tensor(f"bk{i}", [128, 512], F32)) for i in range(8)]
            pm = [BK[6]]
            P = Prog(nc)
            groups = []
            t0 = 0
            while t0 < ntok:
                n = min(TG, ntok - t0)
                groups.append((t0, n))
                t0 += n
            ndma = 0
            bkk = lambda j: [f"bk{j}"]
            cnt_tok = [0]
            cnt_reg = [0]

            def load_sel(gi):
                g0, gn = groups[gi]
                sg = selg[gi % 2]
                P.op("sync", (lambda sg, g0, gn: lambda e: e.dma_start(out=sg[:, :, 0:gn], in_=sels[:, :, g0:g0 + gn]))(sg, g0, gn), writes=[f"selg{gi % 2}"], dma=f"selg{gi % 2}")

            def build_pieces(gi, h):
                g0, gn = groups[gi]
                sg = selg[gi % 2]
                skey = f"selg{gi % 2}"
                out = []
                for tp in range(0, gn, 2):
                    toks = []
                    for t in (tp, tp + 1):
                        i = cnt_tok[0] % NAB
                        cnt_tok[0] += 1
                        toks.append((t, i))
                    def part_a(toks=toks):
                        for (t, i) in toks:
                            P.op("vector", (lambda t, i: lambda e: e.tensor_scalar(out=A2[i][:], in0=iota_b[:, 64 * h:64 * h + 64], scalar1=sg[:, 1, t:t + 1], scalar2=sg[:, 2, t:t + 1],
                                                                                   op0=ALU.is_equal, op1=ALU.mult))(t, i),
                                 reads=[skey], writes=[f"A2_{i}"])
                            P.op("vector", (lambda t, i: lambda e: e.tensor_scalar(out=A1[i][:], in0=iota_b[:], scalar1=sg[:, 0, t:t + 1], scalar2=None, op0=ALU.is_equal))(t, i),
                                 reads=[skey], writes=[f"A1_{i}"])
                    out.append((part_a, toks, h))
                return out

            pend_a = []
            pend_b = []

            def emit_b():
                k = 0
                runs = []
                for (toks, h) in pend_b:
                    for (t, i) in toks:
                        P.op("tensor", (lambda k, i: lambda e: e.matmul(BK[7][:, k * 64:(k + 1) * 64], lhsT=A1[i][:], rhs=A2[i][:], start=True, stop=True))(k, i),
                             reads=[f"A1_{i}", f"A2_{i}"], writes=["bk7"])
                        if runs and runs[-1][0] == h and runs[-1][1] + runs[-1][2] == t:
                            runs[-1][2] += 1
                        else:
                            runs.append([h, t, 1, k])
                        k += 1
                for (h, t0_, n_, k0) in runs:
                    P.op("scalar", (lambda h, t0_, n_, k0: lambda e: e.copy(out=Gbuf[:, t0_:t0_ + n_, 64 * h:64 * h + 64], in_=BK[7][:, k0 * 64:(k0 + n_) * 64].rearrange("p (a b) -> p a b", a=n_)))(h, t0_, n_, k0),
                         reads=["bk7"], writes=[f"Gbuf{h}"])
                del pend_b[:]

            def build_step(n):
                if pend_b:
                    emit_b()
                for _ in range(min(n, 4, len(pend_a))):
                    pa_, toks, h = pend_a.pop(0)
                    pa_()
                    pend_b.append((toks, h))

            def build_flush():
                while pend_a or pend_b:
                    build_step(4)

            load_sel(0)
            pend_a.extend(build_pieces(0, 0))
            pend_a.extend(build_pieces(0, 1))
            build_flush()
            for gi, (g0, gn) in enumerate(groups):
                P.op("sync", (lambda g0, gn: lambda e: e.dma_start(out=h2g[:, :, 0:gn], in_=h2s[:, :, g0:g0 + gn]))(g0, gn), writes=["h2g"], dma="h2g")
                P.op("sync", (lambda g0, gn: lambda e: e.dma_start(out=x1g[:, :, 0:gn], in_=x1s[:, :, g0:g0 + gn]))(g0, gn), writes=["x1g"], dma="x1g")
                if gi + 1 < len(groups):
                    load_sel(gi + 1)
                if gi > 0:
                    pend_a.extend(build_pieces(gi, 1))
                per_slot = 3

                def slot_of(c, gi=gi):
                    return (c // CPD + gi * (128 // CPD)) % 2

                def emit_dma(c):
                    slot = slot_of(c)
                    e0 = c * 128
                    P.op("sync", (lambda slot, e0: lambda e: e.dma_start(out=utb[slot][:], in_=UTb[:, e0:e0 + CPD * 128].rearrange("(k p) n -> p k n", p=128)))(slot, e0),
                         writes=[f"utb{slot}"], dma=f"utb{slot}")
                    P.op("sync", (lambda slot, e0: lambda e: e.dma_start(out=vbb[slot][:], in_=Vb[e0:e0 + CPD * 128, :].rearrange("(a p) n -> p a n", p=128)))(slot, e0),
                         writes=[f"vbb{slot}"], dma=f"vbb{slot}")

                def emit_m1(c, gn=gn):
                    slot = slot_of(c)
                    ci = c % CPD
                    for kc in range(8):
                        P.op("tensor", (lambda slot, ci, kc, gn: lambda e: e.matmul(pm[0][:, 0:gn], lhsT=utb[slot][:, kc, ci * 128:(ci + 1) * 128], rhs=h2g[:, kc, 0:gn], start=(kc == 0), stop=(kc == 7)))(slot, ci, kc, gn),
                             reads=[f"utb{slot}", "h2g"], writes=["bk6"])

                def emit_act(c, gn=gn):
                    a_, w_ = ag[c % 2], wT[c % 2]
                    P.op("scalar", (lambda a_, gn: lambda e: e.activation(out=a_[:, 0:gn], in_=pm[0][:, 0:gn], func=AF.Gelu))(a_, gn), reads=["bk6"], writes=[f"ag{c % 2}"])
                    P.op("vector", (lambda a_, w_, c, gn: lambda e: e.tensor_tensor(out=w_[:, 0:gn], in0=a_[:, 0:gn], in1=Gbuf[:, 0:gn, c], op=ALU.mult))(a_, w_, c, gn),
                         reads=[f"ag{c % 2}", f"Gbuf{c // 64}"], writes=[f"wT{c % 2}"])

                def emit_m2(c, gn=gn):
                    slot = slot_of(c)
                    ci = c % CPD
                    w_ = wT[c % 2]
                    for tt in range(gn // 128):
                        for dh in range(2):
                            bi = tt * 2 + dh
                            P.op("tensor", (lambda w_, slot, ci, tt, dh, bi, c: lambda e: e.matmul(BK[bi][:, :], lhsT=w_[:, tt * 128:(tt + 1) * 128], rhs=vbb[slot][:, ci, dh * 512:(dh + 1) * 512],
                                                                                                  start=(c == 0), stop=(c == 127)))(w_, slot, ci, tt, dh, bi, c),
                                 reads=[f"vbb{slot}", f"wT{c % 2}"], writes=[f"bk{bi}"])

                for c in range(128):
                    if c == 64:
                        build_flush()
                        if gi + 1 < len(groups):
                            pend_a.extend(build_pieces(gi + 1, 0))
                    if c % CPD == 0:
                        emit_dma(c)
                    emit_m1(c)
                    if c > 0:
                        emit_m2(c - 1)
                    emit_act(c)
                    build_step(per_slot)
                emit_m2(127)
                build_flush()
                for tt in range(gn // 128):
                    for dh in range(2):
                        bi = tt * 2 + dh
                        if bi % 2 == 0:
                            P.op("scalar", (lambda tt, dh, bi: lambda e: e.copy(out=peer_sb[:, tt, dh * 512:(dh + 1) * 512], in_=BK[bi][:, :]))(tt, dh, bi), reads=[f"bk{bi}"], writes=[f"peer{tt}"])
                        else:
                            P.op("vector", (lambda tt, dh, bi: lambda e: e.tensor_copy(out=peer_sb[:, tt, dh * 512:(dh + 1) * 512], in_=BK[bi][:, :]))(tt, dh, bi), reads=[f"bk{bi}"], writes=[f"peer{tt}"])
                for j in range(8):
                    for tt in range(gn // 128):
                        P.op("tensor", (lambda j, tt: lambda e: e.transpose(out=BK[j][:, tt * 128:(tt + 1) * 128], in_=peer_sb[:, tt, j * 128:(j + 1) * 128], identity=ident_f[:]))(j, tt),
                             reads=[f"peer{tt}"], writes=bkk(j))
                segs = []
                tcur = g0
                while tcur < g0 + gn:
                    bb = tcur // seq
                    tend = min((bb + 1) * seq, g0 + gn)
                    segs.append((bb, tcur - g0, tend - g0))
                    tcur = tend
                for j in range(8):
                    for (bb, s0, s1) in segs:
                        P.op("vector", (lambda j, bb, s0, s1: lambda e: e.scalar_tensor_tensor(out=x1g[:, j, s0:s1], in0=BK[j][:, s0:s1], scalar=dcol(D_ADA + (40 + j) * nseq + bb),
                                                                                               in1=x1g[:, j, s0:s1], op0=ALU.mult, op1=ALU.add))(j, bb, s0, s1),
                             reads=bkk(j) + ["x1g"], writes=["x1g"])
                P.op("scalar", (lambda gn: lambda e: e.activation(out=sq2[:, :, 0:gn], in_=x1g[:, :, 0:gn], func=AF.Square))(gn), reads=["x1g"], writes=["sq2"])
                for kc in range(8):
                    P.op("tensor", (lambda kc, gn: lambda e: e.matmul(pm[0][:, 0:gn], lhsT=ones_bf[:], rhs=sq2[:, kc, 0:gn], start=(kc == 0), stop=(kc == 7)))(kc, gn), reads=["sq2"], writes=["bk6"])
                P.op("scalar", (lambda gn: lambda e: e.activation(out=rstd2[:, 0:gn], in_=pm[0][:, 0:gn], func=AF.Ln, bias=eps_t[:], scale=1.0 / D))(gn), reads=["bk6"], writes=["rstd2"])
                P.op("scalar", (lambda gn: lambda e: e.activation(out=rstd2[:, 0:gn], in_=rstd2[:, 0:gn], func=AF.Exp, scale=-0.5))(gn), reads=["rstd2"], writes=["rstd2"])
                P.op("vector", (lambda gn: lambda e: e.tensor_tensor(out=x1g[:, :, 0:gn], in0=x1g[:, :, 0:gn], in1=rstd2[:, 0:gn].unsqueeze(1).to_broadcast([128, 8, gn]), op=ALU.mult))(gn),
                     reads=["x1g", "rstd2"], writes=["x1g"])
                for j in range(8):
                    P.op("scalar", (lambda j, gn: lambda e: e.activation(out=x1g[:, j, 0:gn], in_=x1g[:, j, 0:gn], func=AF.Copy, scale=pcol(P_GF + j)))(j, gn), reads=["x1g"], writes=["x1g"])
                P.op("sync", (lambda g0, gn: lambda e: e.dma_start(out=yT[:, g0:g0 + gn].rearrange("(c p) t -> p c t", p=128), in_=x1g[:, :, 0:gn]))(g0, gn), reads=["x1g"], dma="yst")
            import os as _os2
            _mxb = _os2.environ.get("PROG_MAXOPS_B")
            print("phase B ops", len(P.ops))
            if _mxb:
                for _i, _o in enumerate(P.ops[:int(_mxb)]):
                    if _i >= int(_mxb) - 14:
                        print("OP", _i, _o["eng"], _o["dma"], "r", _o["r"], "w", _o["w"], "deps", sorted(_o["deps"]))
            P.emit(int(_mxb) if _mxb else None)
    return nc


def host_layout(inputs, core, seq, nseq):
    x = np.asarray(inputs["x"])
    c = np.asarray(inputs["c"])
    b0 = core * nseq
    ntok = seq * nseq
    xT = np.ascontiguousarray(x[b0:b0 + nseq, :seq].reshape(ntok, D).T)
    cT = np.ascontiguousarray(c[b0:b0 + nseq].reshape(nseq, 8, 128).transpose(2, 1, 0))
    return xT, cT


def shared_layout(inputs):
    f = lambda k: np.asarray(inputs[k], dtype=np.float32)
    kcp = lambda w: np.ascontiguousarray(w.reshape(8, 128, w.shape[1]).transpose(1, 0, 2))
    colT = lambda v, n: v.reshape(n, 128).T
    w_in = f("w_in")
    b_in = f("b_in")
    q_c = list(range(0, 512))
    k0, k1 = list(range(512, 576)), list(range(576, 640))
    v_c = list(range(640, 768))
    B_c = list(range(768, 1280))
    C_c = list(range(1280, 1792))
    U_c = list(range(1792, 2304))
    order = q_c + k0 + k0 + k1 + k1 + B_c + C_c + U_c + v_c
    w_in_r = kcp(w_in[:, order])
    b_in_r = b_in[order]
    prm = np.zeros((128, NP), np.float32)
    prm[:, P_BADA:P_BADA + 48] = colT(f("b_ada"), 48)
    prm[:, P_G1:P_G1 + 8] = colT(f("norm1_g"), 8)
    prm[:, P_G2:P_G2 + 8] = colT(f("norm2_g"), 8)
    prm[:, P_GF:P_GF + 8] = colT(f("final_g"), 8)
    prm[:, P_BOUT:P_BOUT + 8] = colT(f("b_out"), 8)
    prm[:, P_BIN:P_BIN + 18] = colT(b_in_r[:2304], 18)
    prm[:, P_AG:P_AG + 4] = colT(f("attn_out_g"), 4)
    prm[:, P_CG:P_CG + 4] = colT(f("conv_out_g"), 4)
    cw = f("conv_w")
    for tap in range(3):
        prm[:, P_CW + tap * 4:P_CW + tap * 4 + 4] = colT(cw[tap], 4)
    prm[:, P_SINK:P_SINK + 4] = np.repeat(f("attn_sinks"), 64).reshape(4, 128).T
    prm[:, P_BV:P_BV + 128] = np.broadcast_to(b_in_r[2304:2432][None, :], (128, 128))
    k1_, k2_ = f("peer_keys1"), f("peer_keys2")
    keys = np.stack([k1_, k2_], axis=1).reshape(16, 128, 128)
    keysT = np.ascontiguousarray(keys.transpose(2, 0, 1))
    U = f("peer_u")
    V = f("peer_v")
    UT = np.ascontiguousarray(U.reshape(128, 128, D).transpose(2, 1, 0).reshape(D, 16384))
    Vr = np.ascontiguousarray(V.reshape(128, 128, D).transpose(1, 0, 2).reshape(16384, D))
    return dict(w_ada=kcp(f("w_ada")), prm=prm, w_in=w_in_r, w_out=kcp(f("w_out")), w_q=kcp(f("w_query")), keysT=keysT, UT=UT, Vr=Vr)


def run(inputs, seq=2048, nseq=2, n_cores=N_CORES, stop_after=9):
    nc = build_nc(seq, nseq, stop_after=stop_after)
    shared = shared_layout(inputs)
    in_maps = []
    for core in range(n_cores):
        xT, cT = host_layout(inputs, core, seq, nseq)
        m = dict(shared)
        m["xT"] = xT
        m["cT"] = cT
        in_maps.append(m)
    res = run_bass_kernel_spmd(nc, in_maps, core_ids=list(range(n_cores)))
    outs = [np.asarray(r["yT"]).T.reshape(nseq, seq, D) for r in res.results]
    return np.concatenate(outs, axis=0).astype(np.float32)


def kernel(**inputs):
    return run(inputs)
```

```python
import numpy as np
from contextlib import ExitStack
import concourse.bass as bass
import concourse.mybir as mybir
from concourse.bass_utils import run_bass_kernel_spmd

F32 = mybir.dt.float32
BF16 = mybir.dt.bfloat16
U32 = mybir.dt.uint32
AF = mybir.ActivationFunctionType
ALU = mybir.AluOpType
AX = mybir.AxisListType

ENGS = ("tensor", "vector", "scalar", "gpsimd", "sync")
N_CORES = 8
D = 1024
EPS = 1e-6
NEGBIG = -1e30


class Prog:
    def __init__(self, nc, same_engine_sync=True):
        self.nc = nc
        self.ops = []
        self.last_w = {}
        self.readers = {}
        self.same_engine_sync = same_engine_sync
        self.dma_groups = {}

    def op(self, eng, fn, reads=(), writes=(), dma=None):
        i = len(self.ops)
        deps = set()
        for r in reads:
            if r in self.last_w:
                deps.add(self.last_w[r])
        for w in writes:
            if w in self.last_w:
                deps.add(self.last_w[w])
            for rd in self.readers.get(w, ()):
                deps.add(rd)
        deps.discard(i)
        for r in reads:
            lst = self.readers.setdefault(r, [])
            if dma is None:
                lst[:] = [q for q in lst if not (self.ops[q]["dma"] is None and self.ops[q]["eng"] == eng)]
            lst.append(i)
        for w in writes:
            self.last_w[w] = i
            self.readers[w] = []
        dma_idx = None
        if dma is not None:
            self.dma_groups[dma] = self.dma_groups.get(dma, 0) + 1
            dma_idx = self.dma_groups[dma]
        self.ops.append(dict(eng=eng, fn=fn, deps=deps, dma=dma, dma_idx=dma_idx, w=list(writes), r=list(reads)))
        return i

    def emit(self, max_ops=None):
        nc = self.nc
        if max_ops is not None:
            self.ops = self.ops[:max_ops]
            self.dma_groups = {}
            for o in self.ops:
                if o["dma"] is not None:
                    self.dma_groups[o["dma"]] = max(self.dma_groups.get(o["dma"], 0), o["dma_idx"])
        ops = self.ops
        ses = self.same_engine_sync

        def skip(p, o):
            return (p["dma"] is None and o["dma"] is None and p["eng"] == o["eng"]
                    and (p["eng"] == "tensor" or not ses))

        needs_inc = [False] * len(ops)
        for o in ops:
            for i in o["deps"]:
                p = ops[i]
                if p["dma"] is None and not skip(p, o):
                    needs_inc[i] = True
        cnt = {e: 0 for e in ENGS}
        sig = [0] * len(ops)
        for i, o in enumerate(ops):
            if o["dma"] is None and needs_inc[i]:
                cnt[o["eng"]] += 1
                sig[i] = cnt[o["eng"]]
        import os as _os
        if _os.environ.get("PROG_VERBOSE"):
            print("Prog: ops", len(ops), "sem counts", cnt, "dma", self.dma_groups)
        with ExitStack() as es:
            esem = {e: es.enter_context(nc.semaphore(f"pe_{e}")) for e in ENGS}
            dsem = {g: es.enter_context(nc.semaphore(f"pd_{g}")) for g in self.dma_groups}
            block = es.enter_context(nc.Block())
            per_eng = {e: [i for i, o in enumerate(ops) if o["eng"] == e] for e in ENGS}

            def make(ename):
                def body(eng):
                    waited = {}
                    for i in per_eng[ename]:
                        o = ops[i]
                        need = {}
                        for d in o["deps"]:
                            p = ops[d]
                            if p["dma"] is not None:
                                key = ("d", p["dma"])
                                val = 16 * p["dma_idx"]
                            else:
                                if skip(p, o):
                                    continue
                                key = ("e", p["eng"])
                                val = sig[d]
                            if val > need.get(key, 0):
                                need[key] = val
                        for key, val in need.items():
                            if waited.get(key, 0) >= val:
                                continue
                            waited[key] = val
                            s = dsem[key[1]] if key[0] == "d" else esem[key[1]]
                            eng.wait_ge(s, val)
                        ins = o["fn"](eng)
                        if o["dma"] is not None:
                            ins.then_inc(dsem[o["dma"]], 16)
                        elif needs_inc[i]:
                            ins.then_inc(esem[ename], 1)
                    if ename == "sync":
                        for g, n in self.dma_groups.items():
                            eng.wait_ge(dsem[g], 16 * n)
                        for e2 in ENGS:
                            if e2 != "sync" and cnt[e2] > 0:
                                eng.wait_ge(esem[e2], cnt[e2])
                return body

            block.tensor(make("tensor"))
            block.vector(make("vector"))
            block.scalar(make("scalar"))
            block.gpsimd(make("gpsimd"))
            block.sync(make("sync"))


P_BADA, P_G1, P_G2, P_GF, P_BOUT, P_BIN, P_AG, P_CG, P_CW, P_SINK, P_BV, NP = 0, 48, 56, 64, 72, 80, 98, 102, 106, 118, 122, 250
D_ADA, D_A1, D_GB1, D_A2, D_ESINK, ND = 0, 96, 112, 128, 144, 148
NIN = 2432


def build_nc(seq, nseq=2, tga=256, tgb=384, stop_after=9):
    ntok = seq * nseq
    TA = min(tga, seq)
    nc = bass.Bass("TRN2", target_bir_lowering=False)
    dt_in = lambda name, shape: nc.dram_tensor(name, shape, F32, kind="ExternalInput").ap()
    xT = dt_in("xT", [D, ntok])
    cT = dt_in("cT", [128, 8, nseq])
    w_ada = dt_in("w_ada", [128, 8, 6144])
    prm = dt_in("prm", [128, NP])
    w_in = dt_in("w_in", [128, 8, NIN])
    w_out = dt_in("w_out", [128, 8, D])
    w_q = dt_in("w_q", [128, 8, 2048])
    keysT = dt_in("keysT", [128, 16, 128])
    UT = dt_in("UT", [D, 16384])
    Vr = dt_in("Vr", [16384, D])
    yT = nc.dram_tensor("yT", [D, ntok], F32, kind="ExternalOutput").ap()
    UTb = nc.dram_tensor("UTb", [D, 16384], BF16, kind="Internal").ap()
    Vb = nc.dram_tensor("Vb", [16384, D], BF16, kind="Internal").ap()
    x1s = nc.dram_tensor("x1s", [128, 8, ntok], F32, kind="Internal").ap()
    h2s = nc.dram_tensor("h2s", [128, 8, ntok], BF16, kind="Internal").ap()
    sels = nc.dram_tensor("sels", [128, 3, ntok], F32, kind="Internal").ap()

    with ExitStack() as g_es:
        gsb = lambda name, shape, dt: g_es.enter_context(nc.sbuf_tensor(name, shape, dt))
        prm_sb = gsb("prm_sb", [128, NP], F32)
        drv = gsb("drv", [128, ND], F32)
        ones_bf = gsb("ones_bf", [128, 128], BF16)
        blk_bf = gsb("blk_bf", [128, 128], BF16)
        mask2 = gsb("mask2", [128, 4, 128], BF16)
        ident_f = gsb("ident_f", [128, 128], F32)
        iota_f = gsb("iota_f", [128, 128], F32)
        iota_b = gsb("iota_b", [128, 128], BF16)
        esink_bc = gsb("esink_bc", [128, 4, 128], F32)
        attng_bc = gsb("attng_bc", [128, 4, 128], F32)
        eps_t = gsb("eps_t", [128, 1], F32)

        def pcol(off, n=1):
            return prm_sb[:, off:off + n]

        def dcol(off, n=1):
            return drv[:, off:off + n]

        with ExitStack() as es:
            sb = lambda name, shape, dt: es.enter_context(nc.sbuf_tensor(name, shape, dt))
            c_sb = sb("c_sb", [128, 8, nseq], F32)
            sc = sb("sc", [128, 8, nseq], F32)
            wbuf = [sb(f"wada{i}", [128, 8, 1024], F32) for i in range(2)]
            tmpf = sb("tmpf", [128, 128], F32)
            tmp16 = sb("tmp16", [128, 8, nseq], F32)
            pa = es.enter_context(nc.psum_tensor("pa", [128, 512], F32))
            P = Prog(nc)
            P.op("sync", lambda e: e.dma_start(out=prm_sb[:], in_=prm), writes=["prm"], dma="prm")
            P.op("sync", lambda e: e.dma_start(out=c_sb[:], in_=cT), writes=["c"], dma="c")
            P.op("vector", lambda e: e.memset(ones_bf[:], 1.0), writes=["ones"])
            P.op("vector", lambda e: e.memset(blk_bf[:], 0.0), writes=["blk"])
            P.op("vector", lambda e: e.memset(blk_bf[0:64, 0:64], 1.0 / 64), reads=["blk"], writes=["blk"])
            P.op("vector", lambda e: e.memset(blk_bf[64:128, 64:128], 1.0 / 64), reads=["blk"], writes=["blk"])
            P.op("vector", lambda e: e.memset(eps_t[:], EPS), writes=["eps"])
            P.op("gpsimd", lambda e: e.iota(iota_f[:], pattern=[[1, 128]], base=0, channel_multiplier=0, allow_small_or_imprecise_dtypes=True), writes=["iota"])
            P.op("vector", lambda e: e.tensor_copy(out=iota_b[:], in_=iota_f[:]), reads=["iota"], writes=["iota_b"])
            P.op("gpsimd", lambda e: e.iota(tmpf[:], pattern=[[1, 128]], base=0, channel_multiplier=-1, allow_small_or_imprecise_dtypes=True), writes=["tmpf"])
            P.op("vector", lambda e: e.tensor_single_scalar(out=ident_f[:], in_=tmpf[:], scalar=0.0, op=ALU.is_equal), reads=["tmpf"], writes=["ident"])
            for hh in range(2):
                P.op("vector", (lambda hh: lambda e: e.tensor_single_scalar(out=mask2[:, 2 * hh, :], in_=tmpf[:], scalar=0.0, op=ALU.is_lt))(hh), reads=["tmpf"], writes=["mask"])
                P.op("vector", (lambda hh: lambda e: e.tensor_single_scalar(out=mask2[:, 2 * hh + 1, :], in_=tmpf[:], scalar=0.0, op=ALU.is_ge))(hh), reads=["tmpf"], writes=["mask"])
            P.op("scalar", lambda e: e.activation(out=sc[:], in_=c_sb[:], func=AF.Silu), reads=["c"], writes=["sc"])
            for piece in range(6):
                wb = wbuf[piece % 2]
                P.op("sync", (lambda piece, wb: lambda e: e.dma_start(out=wb[:], in_=w_ada[:, :, piece * 1024:(piece + 1) * 1024]))(piece, wb),
                     writes=[f"wb{piece % 2}"], dma=f"wada{piece % 2}")
                for jj in range(8):
                    j = piece * 8 + jj
                    for kc in range(8):
                        P.op("tensor", (lambda wb, jj, j, kc: lambda e: e.matmul(pa[:, j * nseq:(j + 1) * nseq], lhsT=wb[:, kc, jj * 128:(jj + 1) * 128],
                                                                                 rhs=sc[:, kc, :], start=(kc == 0), stop=(kc == 7)))(wb, jj, j, kc),
                             reads=[f"wb{piece % 2}", "sc"], writes=["pa"])
            ada_v = drv[:, D_ADA:D_ADA + 48 * nseq].rearrange("p (j b) -> p j b", b=nseq)
            P.op("vector", lambda e: e.tensor_tensor(out=ada_v, in0=pa[:, 0:48 * nseq].rearrange("p (j b) -> p j b", b=nseq),
                                                     in1=prm_sb[:, P_BADA:P_BADA + 48].unsqueeze(2).to_broadcast([128, 48, nseq]), op=ALU.add),
                 reads=["pa", "prm"], writes=["drv"])

            def adav(j0):
                return drv[:, D_ADA + j0 * nseq:D_ADA + (j0 + 8) * nseq].rearrange("p (j b) -> p j b", b=nseq)

            def dv(off):
                return drv[:, off:off + 8 * nseq].rearrange("p (j b) -> p j b", b=nseq)

            def pb(off):
                return prm_sb[:, off:off + 8].unsqueeze(2).to_broadcast([128, 8, nseq])
            P.op("vector", lambda e: e.tensor_scalar(out=tmp16[:], in0=adav(8), scalar1=1.0, scalar2=None, op0=ALU.add), reads=["drv"], writes=["tmp16"])
            P.op("vector", lambda e: e.tensor_tensor(out=dv(D_A1), in0=tmp16[:], in1=pb(P_G1), op=ALU.mult), reads=["tmp16", "prm"], writes=["drvA1"])
            P.op("vector", lambda e: e.tensor_tensor(out=dv(D_GB1), in0=adav(16), in1=pb(P_BOUT), op=ALU.mult), reads=["drv", "prm"], writes=["drvGB1"])
            P.op("vector", lambda e: e.tensor_scalar(out=tmp16[:], in0=adav(32), scalar1=1.0, scalar2=None, op0=ALU.add), reads=["drv", "drvA1"], writes=["tmp16"])
            P.op("vector", lambda e: e.tensor_tensor(out=dv(D_A2), in0=tmp16[:], in1=pb(P_G2), op=ALU.mult), reads=["tmp16", "prm"], writes=["drvA2"])
            P.op("scalar", lambda e: e.activation(out=drv[:, D_ESINK:D_ESINK + 4], in_=prm_sb[:, P_SINK:P_SINK + 4], func=AF.Exp), reads=["prm"], writes=["esink"])
            P.op("vector", lambda e: e.tensor_copy(out=esink_bc[:], in_=drv[:, D_ESINK:D_ESINK + 4].unsqueeze(2).to_broadcast([128, 4, 128])), reads=["esink"], writes=["esink_bc"])
            P.op("vector", lambda e: e.tensor_copy(out=attng_bc[:], in_=prm_sb[:, P_AG:P_AG + 4].unsqueeze(2).to_broadcast([128, 4, 128])), reads=["prm"], writes=["attng_bc"])
            P.emit()
        nc.all_engine_barrier()
        if stop_after < 1:
            return nc

        with ExitStack() as es:
            sb = lambda name, shape, dt: es.enter_context(nc.sbuf_tensor(name, shape, dt))
            win_sb = sb("win_sb", [128, 8, NIN], BF16)
            wout_sb = sb("wout_sb", [128, 8, D], BF16)
            wq_sb = sb("wq_sb", [128, 8, 2048], BF16)
            keys_sb = sb("keys_sb", [128, 16, 128], BF16)
            xt = [sb(f"xt{i}", [128, 8, TA], F32) for i in range(2)]
            sq = sb("sq", [128, 8, TA], BF16)
            rstd = sb("rstd", [128, TA], F32)
            tmpn = sb("tmpn", [128, 8, TA], F32)
            hT = sb("hT", [128, 8, TA], BF16)
            qT = sb("qT", [128, 4, TA], BF16)
            kT = sb("kT", [128, 2, 128 + TA], BF16)
            NB = TA // 128
            vtok = sb("vtok", [128, 1 + NB, 128], BF16)
            Bt = sb("Bt", [128, 4, TA], F32)
            zb = sb("zb", [128, 4, 2 + TA], F32)
            acc = sb("acc", [128, 4, TA], F32)
            Ct = acc
            sqc = sq[:, 0:4, :]
            rsc = sb("rsc", [128, TA], F32)
            catT = sb("catT", [128, 8, TA], BF16)
            pT = [sb(f"pT{i}", [128, 4, 128], BF16) for i in range(4)]
            t1 = sb("t1", [128, 4, 128], F32)
            yat = sb("yat", [128, 4, 128], F32)
            sqa = sb("sqa", [128, 4, 128], BF16)
            t2 = sb("t2", [128, 4, 128], F32)
            otmp = sb("otmp", [128, TA], F32)
            h2T = hT
            qpT = sb("qpT", [128, 16, TA], BF16)
            S_sb = sb("S_sb", [128, NB, 16, 128], F32)
            Vt = sb("Vt", [128, 16, 16], F32)
            It = sb("It", [128, 16, 16], U32)
            Itf = sb("Itf", [128, 16, 16], BF16)
            cand = sb("cand", [128, 8, 256], F32)
            SC = sb("SC", [128, 8, 16], F32)
            CI = sb("CI", [128, 8, 16], U32)
            au = sb("au", [128, 8, 16], U32)
            bu = sb("bu", [128, 8, 16], U32)
            af_ = sb("af_", [128, 128], F32)
            bf_ = sb("bf_", [128, 128], F32)
            ohb = tmpn[:].rearrange("p c t -> p (c t)").bitcast(BF16)[:, 0:2048].rearrange("p (a b) -> p a b", b=16)
            sel = sb("sel", [128, 3, 128], F32)
            ee = sb("ee", [128, 8, 16], F32)
            zz = sb("zz", [128, 8], F32)
            selT = sb("selT", [128, 3, 128], F32)
            ps = [es.enter_context(nc.psum_tensor(f"ps{i}", [128, 512], F32)) for i in range(8)]
            P = Prog(nc)
            for kc in range(8):
                P.op("gpsimd", (lambda kc: lambda e: e.dma_start(out=win_sb[:, kc, :], in_=w_in[:, kc, :]))(kc), writes=[f"win{kc}"], dma=f"w_in{kc}")
            P.op("gpsimd", lambda e: e.dma_start(out=keys_sb[:], in_=keysT), writes=["keys"], dma="w_k")
            for kc in range(0, 8, 2):
                P.op("gpsimd", (lambda kc: lambda e: e.dma_start(out=wout_sb[:, kc:kc + 2, :], in_=w_out[:, kc:kc + 2, :]))(kc), writes=[f"wout{kc}", f"wout{kc + 1}"], dma=f"w_out{kc}")
            for kc in range(8):
                P.op("gpsimd", (lambda kc: lambda e: e.dma_start(out=wq_sb[:, kc, :], in_=w_q[:, kc, :]))(kc), writes=[f"wq{kc}"], dma=f"w_q{kc}")
            NPC = 16
            for i in range(NPC):
                r0, r1 = i * (D // NPC), (i + 1) * (D // NPC)
                P.op("gpsimd", (lambda r0, r1: lambda e: e.dma_start(out=UTb[r0:r1, :], in_=UT[r0:r1, :]))(r0, r1), dma="cvt")
            for i in range(NPC):
                r0, r1 = i * (16384 // NPC), (i + 1) * (16384 // NPC)
                P.op("gpsimd", (lambda r0, r1: lambda e: e.dma_start(out=Vb[r0:r1, :], in_=Vr[r0:r1, :]))(r0, r1), dma="cvt")
            WIN = [f"win{k}" for k in range(8)]
            WOUT = [f"wout{k}" for k in range(8)]
            WQ = [f"wq{k}" for k in range(8)]
            ntile = ntok // TA
            tiles_per_seq = seq // TA

            def rms_stats(P, src, srckey, pbank, tag):
                P.op("scalar", lambda e: e.activation(out=sq[:], in_=src[:], func=AF.Square), reads=[srckey], writes=["sq"])
                for kc in range(8):
                    P.op("tensor", (lambda kc: lambda e: e.matmul(pbank[:, 0:TA], lhsT=ones_bf[:], rhs=sq[:, kc, :], start=(kc == 0), stop=(kc == 7)))(kc),
                         reads=["sq"], writes=[tag])
                P.op("scalar", lambda e: e.activation(out=rstd[:], in_=pbank[:, 0:TA], func=AF.Ln, bias=eps_t[:], scale=1.0 / D), reads=[tag], writes=["rstd"])
                P.op("scalar", lambda e: e.activation(out=rstd[:], in_=rstd[:], func=AF.Exp, scale=-0.5), reads=["rstd"], writes=["rstd"])

            pending = []

            def drain(n):
                for _ in range(min(n, len(pending))):
                    pending.pop(0)()

            def topk_pieces(blk, tq):
                pcs = []
                S = lambda j: S_sb[:, blk, j, :]
                sk = lambda j: f"S{blk}_{j}"
                J = range(16)
                H = range(8)
                pcs.append(lambda: [P.op("vector", (lambda j: lambda e: e.max(out=Vt[:, j, 0:8], in_=S(j)))(j), reads=[sk(j)], writes=[f"Vta{j}"]) for j in J])
                pcs.append(lambda: [P.op("vector", (lambda j: lambda e: e.max_index(out=It[:, j, 0:8], in_max=Vt[:, j, 0:8], in_values=S(j)))(j), reads=[sk(j), f"Vta{j}"], writes=[f"Ita{j}"]) for j in J])
                pcs.append(lambda: [P.op("vector", (lambda j: lambda e: e.match_replace(out=S(j), in_to_replace=Vt[:, j, 0:8], in_values=S(j), imm_value=NEGBIG))(j), reads=[sk(j), f"Vta{j}"], writes=[sk(j)]) for j in J])
                pcs.append(lambda: [P.op("vector", (lambda j: lambda e: e.max(out=Vt[:, j, 8:16], in_=S(j)))(j), reads=[sk(j)], writes=[f"Vtb{j}"]) for j in J])
                pcs.append(lambda: [P.op("vector", (lambda j: lambda e: e.max_index(out=It[:, j, 8:16], in_max=Vt[:, j, 8:16], in_values=S(j)))(j), reads=[sk(j), f"Vtb{j}"], writes=[f"Itb{j}"]) for j in J])
                VT = [f"Vta{j}" for j in J] + [f"Vtb{j}" for j in J]
                IT = [f"Ita{j}" for j in J] + [f"Itb{j}" for j in J]
                CAND = [f"cand{h}" for h in H]
                Vv = Vt[:].rearrange("p (h s) a -> p h s a", s=2)
                pcs.append(lambda: P.op("vector", lambda e: e.tensor_tensor(out=cand[:].rearrange("p h (a b) -> p h a b", a=16), in0=Vv[:, :, 0, :].unsqueeze(3).to_broadcast([128, 8, 16, 16]),
                                                                          in1=Vv[:, :, 1, :].unsqueeze(2).to_broadcast([128, 8, 16, 16]), op=ALU.add), reads=VT, writes=CAND))
                pcs.append(lambda: [P.op("vector", (lambda h: lambda e: e.max(out=SC[:, h, 0:8], in_=cand[:, h, :]))(h), reads=[f"cand{h}"], writes=[f"SCa{h}"]) for h in H])
                pcs.append(lambda: [P.op("vector", (lambda h: lambda e: e.max_index(out=CI[:, h, 0:8], in_max=SC[:, h, 0:8], in_values=cand[:, h, :]))(h), reads=[f"cand{h}", f"SCa{h}"], writes=[f"CIa{h}"]) for h in H])
                pcs.append(lambda: [P.op("vector", (lambda h: lambda e: e.match_replace(out=cand[:, h, :], in_to_replace=SC[:, h, 0:8], in_values=cand[:, h, :], imm_value=NEGBIG))(h), reads=[f"cand{h}", f"SCa{h}"], writes=[f"cand{h}"]) for h in H])
                pcs.append(lambda: [P.op("vector", (lambda h: lambda e: e.max(out=SC[:, h, 8:16], in_=cand[:, h, :]))(h), reads=[f"cand{h}"], writes=[f"SCb{h}"]) for h in H])
                pcs.append(lambda: [P.op("vector", (lambda h: lambda e: e.max_index(out=CI[:, h, 8:16], in_max=SC[:, h, 8:16], in_values=cand[:, h, :]))(h), reads=[f"cand{h}", f"SCb{h}"], writes=[f"CIb{h}"]) for h in H])
                SCK = [f"SCa{h}" for h in H] + [f"SCb{h}" for h in H]
                CIK = [f"CIa{h}" for h in H] + [f"CIb{h}" for h in H]

                def gates():
                    P.op("vector", lambda e: e.tensor_tensor(out=ee[:], in0=SC[:], in1=SC[:, :, 0:1].to_broadcast([128, 8, 16]), op=ALU.subtract), reads=SCK, writes=["ee"])
                    P.op("scalar", lambda e: e.activation(out=ee[:], in_=ee[:], func=AF.Exp), reads=["ee"], writes=["ee"])
                    P.op("vector", lambda e: e.tensor_reduce(out=zz[:], in_=ee[:], axis=AX.X, op=ALU.add), reads=["ee"], writes=["zz"])
                    P.op("vector", lambda e: e.reciprocal(out=zz[:], in_=zz[:]), reads=["zz"], writes=["zz"])
                    P.op("vector", lambda e: e.tensor_tensor(out=sel[:, 2, :].rearrange("p (h k) -> p h k", h=8), in0=ee[:], in1=zz[:].unsqueeze(2).to_broadcast([128, 8, 16]), op=ALU.mult),
                         reads=["ee", "zz"], writes=["sel2"])
                    P.op("vector", lambda e: e.tensor_single_scalar(out=au[:], in_=CI[:], scalar=4, op=ALU.logical_shift_right), reads=CIK, writes=["au"])
                    P.op("vector", lambda e: e.tensor_single_scalar(out=bu[:], in_=CI[:], scalar=15, op=ALU.bitwise_and), reads=CIK, writes=["bu"])
                    P.op("vector", lambda e: e.tensor_copy(out=af_[:], in_=au[:].rearrange("p h k -> p (h k)")), reads=["au"], writes=["af"])
                    P.op("vector", lambda e: e.tensor_copy(out=bf_[:], in_=bu[:].rearrange("p h k -> p (h k)")), reads=["bu"], writes=["bf"])
                    P.op("vector", lambda e: e.tensor_copy(out=Itf[:], in_=It[:]), reads=IT, writes=["Itf"])
                pcs.append(gates)

                def decode():
                    Iv = Itf[:].rearrange("p (h s) a -> p h s a", s=2)
                    for s_, src in ((0, af_), (1, bf_)):
                        P.op("vector", (lambda src: lambda e: e.tensor_tensor(out=ohb, in0=iota_f[:, 0:16].unsqueeze(1).to_broadcast([128, 128, 16]),
                                                                              in1=src[:].unsqueeze(2).to_broadcast([128, 128, 16]), op=ALU.is_equal))(src),
                             reads=["af", "bf"], writes=["tmpn"])
                        P.op("vector", (lambda s_: lambda e: e.tensor_tensor(out=ohb.rearrange("p (h k) a -> p h k a", h=8), in0=ohb.rearrange("p (h k) a -> p h k a", h=8),
                                                                             in1=Iv[:, :, s_, :].unsqueeze(2).to_broadcast([128, 8, 16, 16]), op=ALU.mult))(s_),
                             reads=["tmpn", "Itf"], writes=["tmpn"])
                        P.op("vector", (lambda s_: lambda e: e.tensor_reduce(out=sel[:, s_, :], in_=ohb, axis=AX.X, op=ALU.add))(s_), reads=["tmpn"], writes=[f"sel{s_}"])
                    for s_ in range(3):
                        P.op("tensor", (lambda s_: lambda e: e.transpose(out=ps[6][:, s_ * 128:(s_ + 1) * 128], in_=sel[:, s_, :], identity=ident_f[:]))(s_),
                             reads=[f"sel{s_}"], writes=["ps6"])
                    P.op("scalar", lambda e: e.copy(out=selT[:].rearrange("p a b -> p (a b)"), in_=ps[6][:, 0:384]), reads=["ps6"], writes=["selT"])
                    P.op("sync", (lambda tq: lambda e: e.dma_start(out=sels[:, :, tq:tq + 128], in_=selT[:]))(tq), reads=["selT"], dma="selst")
                pcs.append(decode)
                return pcs

            for m in range(ntile):
                b = m // tiles_per_seq
                first = (m % tiles_per_seq == 0)
                tok0 = m * TA
                x = xt[m % 2]
                xk = f"xt{m % 2}"
                P.op("sync", (lambda x, tok0: lambda e: e.dma_start(out=x[:], in_=xT[:, tok0:tok0 + TA].rearrange("(c p) t -> p c t", p=128)))(x, tok0),
                     writes=[xk], dma=xk)
                rms_stats(P, x, xk, ps[7], "ps7")
                P.op("gpsimd", (lambda x: lambda e: e.tensor_tensor(out=tmpn[:], in0=x[:], in1=rstd[:].unsqueeze(1).to_broadcast([128, 8, TA]), op=ALU.mult))(x),
                     reads=[xk, "rstd"], writes=["tmpn"])
                for c in range(8):
                    P.op("scalar", (lambda c, b: lambda e: e.activation(out=hT[:, c, :], in_=tmpn[:, c, :], func=AF.Identity,
                                                                        scale=dcol(D_A1 + c * nseq + b), bias=dcol(D_ADA + (0 + c) * nseq + b)))(c, b),
                         reads=["tmpn"], writes=[f"hT{c}"])
                HT = [f"hT{c}" for c in range(8)]
                if first:
                    P.op("vector", lambda e: e.memset(zb[:, :, 0:2], 0.0), reads=["zb"], writes=["zb"])
                for j in range(18):
                    if j % 3 == 0 and j < 15:
                        drain(1)
                    pb_ = ps[j % 6]
                    pk = f"ps{j % 6}"
                    for kc in range(8):
                        P.op("tensor", (lambda pb_, j, kc: lambda e: e.matmul(pb_[:, 0:TA], lhsT=win_sb[:, kc, j * 128:(j + 1) * 128], rhs=hT[:, kc, :],
                                                                              start=(kc == 0), stop=(kc == 7)))(pb_, j, kc),
                             reads=[f"win{kc}", f"hT{kc}"], writes=[pk])
                    bias = pcol(P_BIN + j)
                    if j < 4:
                        P.op("scalar", (lambda pb_, j, bias: lambda e: e.activation(out=qT[:, j, :], in_=pb_[:, 0:TA], func=AF.Identity, bias=bias))(pb_, j, bias),
                             reads=[pk], writes=["qT"])
                    elif j < 6:
                        P.op("scalar", (lambda pb_, j, bias: lambda e: e.activation(out=kT[:, j - 4, 128:128 + TA], in_=pb_[:, 0:TA], func=AF.Identity, bias=bias))(pb_, j, bias),
                             reads=[pk], writes=["kTcur"])
                    elif j < 10:
                        P.op("scalar", (lambda pb_, j, bias: lambda e: e.activation(out=Bt[:, j - 6, :], in_=pb_[:, 0:TA], func=AF.Identity, bias=bias))(pb_, j, bias),
                             reads=[pk], writes=["Bt"])
                    elif j < 14:
                        P.op("scalar", (lambda pb_, j, bias: lambda e: e.activation(out=Ct[:, j - 10, :], in_=pb_[:, 0:TA], func=AF.Identity, bias=bias))(pb_, j, bias),
                             reads=[pk], writes=[f"acc{j - 10}"])
                    else:
                        P.op("vector", (lambda pb_, j, bias: lambda e: e.scalar_tensor_tensor(out=zb[:, j - 14, 2:2 + TA], in0=pb_[:, 0:TA], scalar=bias,
                                                                                               in1=Ct[:, j - 14, :], op0=ALU.add, op1=ALU.mult))(pb_, j, bias),
                             reads=[pk, f"acc{j - 14}"], writes=["zb"])
                for blk in range(NB):
                    pb_ = ps[blk % 2]
                    pk = f"ps{blk % 2}"
                    for kc in range(8):
                        P.op("tensor", (lambda pb_, blk, kc: lambda e: e.matmul(pb_[:, 0:128], lhsT=hT[:, kc, blk * 128:(blk + 1) * 128], rhs=win_sb[:, kc, 2304:2432],
                                                                                start=(kc == 0), stop=(kc == 7)))(pb_, blk, kc),
                             reads=[f"win{kc}", f"hT{kc}"], writes=[pk])
                    P.op("vector", (lambda pb_, blk: lambda e: e.tensor_tensor(out=vtok[:, 1 + blk, :], in0=pb_[:, 0:128], in1=prm_sb[:, P_BV:P_BV + 128], op=ALU.add))(pb_, blk),
                         reads=[pk], writes=["vcur"])
                for cc in range(4):
                    P.op("scalar", (lambda cc: lambda e: e.activation(out=acc[:, cc, :], in_=zb[:, cc, 2:2 + TA], func=AF.Copy, scale=pcol(P_CW + 2 * 4 + cc)))(cc),
                         reads=["zb"], writes=[f"acc{cc}"])
                    P.op("vector", (lambda cc: lambda e: e.scalar_tensor_tensor(out=acc[:, cc, :], in0=zb[:, cc, 1:1 + TA], scalar=pcol(P_CW + 1 * 4 + cc), in1=acc[:, cc, :],
                                                                                op0=ALU.mult, op1=ALU.add))(cc), reads=["zb", f"acc{cc}"], writes=[f"acc{cc}"])
                    P.op("vector", (lambda cc: lambda e: e.scalar_tensor_tensor(out=acc[:, cc, :], in0=zb[:, cc, 0:TA], scalar=pcol(P_CW + 0 * 4 + cc), in1=acc[:, cc, :],
                                                                                op0=ALU.mult, op1=ALU.add))(cc), reads=["zb", f"acc{cc}"], writes=[f"acc{cc}"])
                    P.op("gpsimd", (lambda cc: lambda e: e.tensor_tensor(out=acc[:, cc, :], in0=acc[:, cc, :], in1=Bt[:, cc, :], op=ALU.mult))(cc),
                         reads=["Bt", f"acc{cc}"], writes=[f"acc{cc}"])
                ACC = [f"acc{cc}" for cc in range(4)]
                P.op("gpsimd", lambda e: e.tensor_copy(out=zb[:, :, 0:2], in_=zb[:, :, TA:TA + 2]), reads=["zb"] + ACC, writes=["zb"])
                P.op("scalar", lambda e: e.activation(out=sqc, in_=acc[:], func=AF.Square), reads=ACC, writes=["sq"])
                for cc in range(4):
                    pb_ = ps[2 + cc % 2]
                    pk = f"ps{2 + cc % 2}"
                    P.op("tensor", (lambda pb_, cc: lambda e: e.matmul(pb_[:, 0:TA], lhsT=blk_bf[:], rhs=sqc[:, cc, :], start=True, stop=True))(pb_, cc),
                         reads=["sq"], writes=[pk])
                    P.op("scalar", (lambda pb_: lambda e: e.activation(out=rsc[:], in_=pb_[:, 0:TA], func=AF.Ln, bias=eps_t[:], scale=1.0))(pb_), reads=[pk], writes=["rsc"])
                    P.op("scalar", lambda e: e.activation(out=rsc[:], in_=rsc[:], func=AF.Exp, scale=-0.5), reads=["rsc"], writes=["rsc"])
                    P.op("vector", (lambda cc: lambda e: e.scalar_tensor_tensor(out=catT[:, 4 + cc, :], in0=acc[:, cc, :], scalar=pcol(P_CG + cc), in1=rsc[:],
                                                                                op0=ALU.mult, op1=ALU.mult))(cc), reads=[f"acc{cc}", "rsc"], writes=[f"cat{4 + cc}"])
                for blk in range(NB):
                    hasprev = not (first and blk == 0)
                    q0 = blk * 128
                    kprev = slice(q0, q0 + 128)
                    kcur = slice(128 + q0, 256 + q0)
                    vprev, vcur = blk, blk + 1
                    po = ps[4]
                    pd = ps[5]
                    for pp in range(2):
                        g = pp
                        for ci in range(2):
                            c = 2 * pp + ci
                            for hh in range(2):
                                lo, hi = hh * 64, hh * 64 + 64
                                bank = ps[2 * pp + hh]
                                pk = f"ps{2 * pp + hh}"
                                if hasprev:
                                    P.op("tensor", (lambda bank, ci, lo, hi, g, c, kprev, q0: lambda e: e.matmul(bank[:, ci * 256:ci * 256 + 128], lhsT=kT[lo:hi, g, kprev],
                                                                                                               rhs=qT[lo:hi, c, q0:q0 + 128], start=True, stop=True))(bank, ci, lo, hi, g, c, kprev, q0),
                                         reads=["qT", "kTcur", "kTprev"], writes=[pk])
                                P.op("tensor", (lambda bank, ci, lo, hi, g, c, kcur, q0: lambda e: e.matmul(bank[:, ci * 256 + 128:ci * 256 + 256], lhsT=kT[lo:hi, g, kcur],
                                                                                                          rhs=qT[lo:hi, c, q0:q0 + 128], start=True, stop=True))(bank, ci, lo, hi, g, c, kcur, q0),
                                     reads=["qT", "kTcur", "kTprev"], writes=[pk])
                        drain(1)
                        for hh in range(2):
                            bank = ps[2 * pp + hh]
                            pk = f"ps{2 * pp + hh}"
                            pt = pT[2 * pp + hh]
                            ptk = f"pT{2 * pp + hh}"
                            if hasprev:
                                P.op("scalar", (lambda bank, pt: lambda e: e.activation(out=pt[:].rearrange("p a b -> p (a b)"), in_=bank[:, :], func=AF.Exp, scale=0.125))(bank, pt),
                                     reads=[pk], writes=[ptk])
                                P.op("vector", (lambda pt: lambda e: e.tensor_tensor(out=pt[:], in0=pt[:], in1=mask2[:], op=ALU.mult))(pt), reads=[ptk], writes=[ptk])
                            else:
                                psv = bank[:, :].rearrange("p (h a i) -> p h a i", h=2, a=2)[:, :, 1, :]
                                ptv = pt[:].rearrange("p (h a) i -> p h a i", h=2)[:, :, 1, :]
                                mkv = mask2[:].rearrange("p (h a) i -> p h a i", h=2)[:, :, 1, :]
                                P.op("scalar", (lambda psv, ptv: lambda e: e.activation(out=ptv, in_=psv, func=AF.Exp, scale=0.125))(psv, ptv), reads=[pk], writes=[ptk])
                                P.op("vector", (lambda ptv, mkv: lambda e: e.tensor_tensor(out=ptv, in0=ptv, in1=mkv, op=ALU.mult))(ptv, mkv), reads=[ptk], writes=[ptk])
                        for ci in range(2):
                            c = 2 * pp + ci
                            for hh in range(2):
                                lo, hi = hh * 64, hh * 64 + 64
                                pt = pT[2 * pp + hh]
                                ptk = f"pT{2 * pp + hh}"
                                oc = slice(c * 128, (c + 1) * 128)
                                if hasprev:
                                    P.op("tensor", (lambda pt, ci, lo, hi, g, oc, vprev: lambda e: e.matmul(po[lo:hi, oc], lhsT=vtok[:, vprev, g * 64:(g + 1) * 64], rhs=pt[:, 2 * ci, :],
                                                                                                          start=True, stop=False))(pt, ci, lo, hi, g, oc, vprev),
                                         reads=[ptk, "vcur", "vprev"], writes=["ps4"])
                                P.op("tensor", (lambda pt, ci, lo, hi, g, oc, vcur, hasprev: lambda e: e.matmul(po[lo:hi, oc], lhsT=vtok[:, vcur, g * 64:(g + 1) * 64], rhs=pt[:, 2 * ci + 1, :],
                                                                                                              start=(not hasprev), stop=True))(pt, ci, lo, hi, g, oc, vcur, hasprev),
                                     reads=[ptk, "vcur", "vprev"], writes=["ps4"])
                                if hasprev:
                                    P.op("tensor", (lambda pt, ci, lo, hi, oc: lambda e: e.matmul(pd[lo:hi, oc], lhsT=ones_bf[:, 0:64], rhs=pt[:, 2 * ci, :], start=True, stop=False))(pt, ci, lo, hi, oc),
                                         reads=[ptk], writes=["ps5"])
                                P.op("tensor", (lambda pt, ci, lo, hi, oc, hasprev: lambda e: e.matmul(pd[lo:hi, oc], lhsT=ones_bf[:, 0:64], rhs=pt[:, 2 * ci + 1, :], start=(not hasprev), stop=True))(pt, ci, lo, hi, oc, hasprev),
                                     reads=[ptk], writes=["ps5"])
                    pov = po[:, :].rearrange("p (c i) -> p c i", c=4)
                    pdv = pd[:, :].rearrange("p (c i) -> p c i", c=4)
                    P.op("vector", lambda e: e.tensor_tensor(out=t1[:], in0=pdv, in1=esink_bc[:], op=ALU.add), reads=["ps5"], writes=["t1"])
                    P.op("scalar", lambda e: e.activation(out=t1[:], in_=t1[:], func=AF.Ln), reads=["t1"], writes=["t1"])
                    P.op("scalar", lambda e: e.activation(out=t1[:], in_=t1[:], func=AF.Exp, scale=-1.0), reads=["t1"], writes=["t1"])
                    P.op("vector", lambda e: e.tensor_tensor(out=yat[:], in0=pov, in1=t1[:], op=ALU.mult), reads=["ps4", "t1"], writes=["yat"])
                    P.op("scalar", lambda e: e.activation(out=sqa[:], in_=yat[:], func=AF.Square), reads=["yat"], writes=["sqa"])
                    P.op("tensor", lambda e: e.matmul(ps[6][:, :], lhsT=blk_bf[:], rhs=sqa[:].rearrange("p c i -> p (c i)"), start=True, stop=True), reads=["sqa"], writes=["ps6"])
                    P.op("scalar", lambda e: e.activation(out=t2[:].rearrange("p c i -> p (c i)"), in_=ps[6][:, :], func=AF.Ln, bias=eps_t[:], scale=1.0), reads=["ps6"], writes=["t2"])
                    P.op("scalar", lambda e: e.activation(out=t2[:], in_=t2[:], func=AF.Exp, scale=-0.5), reads=["t2"], writes=["t2"])
                    P.op("vector", lambda e: e.tensor_tensor(out=yat[:], in0=yat[:], in1=t2[:], op=ALU.mult), reads=["yat", "t2"], writes=["yat"])
                    P.op("vector", (lambda q0: lambda e: e.tensor_tensor(out=catT[:, 0:4, q0:q0 + 128], in0=yat[:], in1=attng_bc[:], op=ALU.mult))(q0),
                         reads=["yat"], writes=[f"cat{c}" for c in range(4)])
                P.op("gpsimd", lambda e: e.tensor_copy(out=kT[:, :, 0:128], in_=kT[:, :, TA:TA + 128]), reads=["kTcur", "kTprev"], writes=["kTprev", "kTcur"])
                P.op("gpsimd", lambda e: e.tensor_copy(out=vtok[:, 0, :], in_=vtok[:, NB, :]), reads=["vcur", "vprev"], writes=["vprev", "vcur"])
                CAT = [f"cat{c}" for c in range(8)]
                for j in range(8):
                    if j % 2 == 0:
                        drain(1)
                    pb_ = ps[j % 6]
                    pk = f"ps{j % 6}"
                    for kc in range(8):
                        P.op("tensor", (lambda pb_, j, kc: lambda e: e.matmul(pb_[:, 0:TA], lhsT=wout_sb[:, kc, j * 128:(j + 1) * 128], rhs=catT[:, kc, :], start=(kc == 0), stop=(kc == 7)))(pb_, j, kc),
                             reads=[f"wout{kc}", f"cat{kc}"], writes=[pk])
                    P.op("scalar", (lambda pb_, j, b: lambda e: e.activation(out=otmp[:], in_=pb_[:, 0:TA], func=AF.Identity, scale=dcol(D_ADA + (16 + j) * nseq + b),
                                                                             bias=dcol(D_GB1 + j * nseq + b)))(pb_, j, b), reads=[pk], writes=["otmp"])
                    P.op("gpsimd", (lambda x, j: lambda e: e.tensor_tensor(out=x[:, j, :], in0=x[:, j, :], in1=otmp[:], op=ALU.add))(x, j), reads=["otmp", xk], writes=[xk])
                P.op("sync", (lambda x, tok0: lambda e: e.dma_start(out=x1s[:, :, tok0:tok0 + TA], in_=x[:]))(x, tok0), reads=[xk], dma=f"x1st{m % 2}")
                rms_stats(P, x, xk, ps[7], "ps7")
                P.op("gpsimd", (lambda x: lambda e: e.tensor_tensor(out=tmpn[:], in0=x[:], in1=rstd[:].unsqueeze(1).to_broadcast([128, 8, TA]), op=ALU.mult))(x),
                     reads=[xk, "rstd"], writes=["tmpn"])
                for c in range(8):
                    P.op("scalar", (lambda c, b: lambda e: e.activation(out=h2T[:, c, :], in_=tmpn[:, c, :], func=AF.Identity,
                                                                        scale=dcol(D_A2 + c * nseq + b), bias=dcol(D_ADA + (24 + c) * nseq + b)))(c, b),
                         reads=["tmpn"], writes=[f"hT{c}"])
                H2 = [f"hT{c}" for c in range(8)]
                P.op("sync", (lambda tok0: lambda e: e.dma_start(out=h2s[:, :, tok0:tok0 + TA], in_=h2T[:]))(tok0), reads=H2, dma="h2st")
                for j in range(16):
                    if j % 2 == 0:
                        drain(1)
                    pb_ = ps[j % 6]
                    pk = f"ps{j % 6}"
                    for kc in range(8):
                        P.op("tensor", (lambda pb_, j, kc: lambda e: e.matmul(pb_[:, 0:TA], lhsT=wq_sb[:, kc, j * 128:(j + 1) * 128], rhs=h2T[:, kc, :], start=(kc == 0), stop=(kc == 7)))(pb_, j, kc),
                             reads=[f"wq{kc}", f"hT{kc}"], writes=[pk])
                    P.op("scalar", (lambda pb_, j: lambda e: e.copy(out=qpT[:, j, :], in_=pb_[:, 0:TA]))(pb_, j), reads=[pk], writes=[f"qp{j}"])
                drain(len(pending))
                for blk in range(NB):
                    q0 = blk * 128
                    for j4 in range(4):
                        pb_ = ps[2 + j4]
                        pk = f"ps{2 + j4}"
                        for jj in range(4):
                            j = j4 * 4 + jj
                            P.op("tensor", (lambda pb_, j, jj, q0: lambda e: e.matmul(pb_[:, jj * 128:(jj + 1) * 128], lhsT=qpT[:, j, q0:q0 + 128], rhs=keys_sb[:, j, :], start=True, stop=True))(pb_, j, jj, q0),
                                 reads=[f"qp{j}", "keys"], writes=[pk])
                        P.op("scalar", (lambda pb_, j4, blk: lambda e: e.copy(out=S_sb[:, blk, j4 * 4:(j4 + 1) * 4, :].rearrange("p a b -> p (a b)"), in_=pb_[:, :]))(pb_, j4, blk),
                             reads=[pk], writes=[f"S{blk}_{j4 * 4 + jj}" for jj in range(4)])
                for blk in range(NB):
                    pending.extend(topk_pieces(blk, tok0 + blk * 128))
            while pending:
                pending.pop(0)()
            import os as _os
            _mx = _os.environ.get("PROG_MAXOPS")
            print("phase A ops", len(P.ops))
            if _mx:
                for _i, _o in enumerate(P.ops[:int(_mx)][-3:]):
                    print("last ops", _o["eng"], _o["dma"])
            P.emit(int(_mx) if _mx else None)
        nc.all_engine_barrier()
        if stop_after < 2:
            return nc

        with ExitStack() as es:
            sb = lambda name, shape, dt: es.enter_context(nc.sbuf_tensor(name, shape, dt))
            TG = min(tgb, ntok)
            Gbuf = sb("Gbuf", [128, TG, 128], BF16)
            CPD = 4
            utb = [sb(f"utb{i}", [128, 8, CPD * 128], BF16) for i in range(2)]
            vbb = [sb(f"vbb{i}", [128, CPD, D], BF16) for i in range(2)]
            h2g = sb("h2g", [128, 8, TG], BF16)
            x1g = sb("x1g", [128, 8, TG], F32)
            selg = [sb(f"selg{i}", [128, 3, TG], F32) for i in range(2)]
            NAB = 16
            A1 = [sb(f"A1_{i}", [128, 128], BF16) for i in range(NAB)]
            A2 = [sb(f"A2_{i}", [128, 64], BF16) for i in range(NAB)]
            ag = [sb(f"ag{i}", [128, TG], BF16) for i in range(2)]
            wT = [sb(f"wT{i}", [128, TG], BF16) for i in range(2)]
            sq2 = sb("sq2", [128, 8, TG], BF16)
            rstd2 = sb("rstd2", [128, TG], F32)
            peer_sb = sb("peer_sb", [128, TG // 128, D], F32)
            BK = [es.enter_context(nc.psum_tensor(f"bk{i}", [128, 512], F32)) for i in range(8)]
            pm = [BK[6]]
            P = Prog(nc)
            groups = []
            t0 = 0
            while t0 < ntok:
                n = min(TG, ntok - t0)
                groups.append((t0, n))
                t0 += n
            ndma = 0
            bkk = lambda j: [f"bk{j}"]
            cnt_tok = [0]
            cnt_reg = [0]

            def load_sel(gi):
                g0, gn = groups[gi]
                sg = selg[gi % 2]
                P.op("sync", (lambda sg, g0, gn: lambda e: e.dma_start(out=sg[:, :, 0:gn], in_=sels[:, :, g0:g0 + gn]))(sg, g0, gn), writes=[f"selg{gi % 2}"], dma=f"selg{gi % 2}")

            def build_pieces(gi, h):
                g0, gn = groups[gi]
                sg = selg[gi % 2]
                skey = f"selg{gi % 2}"
                out = []
                for tp in range(0, gn, 2):
                    toks = []
                    for t in (tp, tp + 1):
                        i = cnt_tok[0] % NAB
                        cnt_tok[0] += 1
                        toks.append((t, i))
                    def part_a(toks=toks):
                        for (t, i) in toks:
                            P.op("vector", (lambda t, i: lambda e: e.tensor_scalar(out=A2[i][:], in0=iota_b[:, 64 * h:64 * h + 64], scalar1=sg[:, 1, t:t + 1], scalar2=sg[:, 2, t:t + 1],
                                                                                   op0=ALU.is_equal, op1=ALU.mult))(t, i),
                                 reads=[skey], writes=[f"A2_{i}"])
                            P.op("vector", (lambda t, i: lambda e: e.tensor_scalar(out=A1[i][:], in0=iota_b[:], scalar1=sg[:, 0, t:t + 1], scalar2=None, op0=ALU.is_equal))(t, i),
                                 reads=[skey], writes=[f"A1_{i}"])
                    out.append((part_a, toks, h))
                return out

            pend_a = []
            pend_b = []

            def emit_b():
                k = 0
                runs = []
                for (toks, h) in pend_b:
                    for (t, i) in toks:
                        P.op("tensor", (lambda k, i: lambda e: e.matmul(BK[7][:, k * 64:(k + 1) * 64], lhsT=A1[i][:], rhs=A2[i][:], start=True, stop=True))(k, i),
                             reads=[f"A1_{i}", f"A2_{i}"], writes=["bk7"])
                        if runs and runs[-1][0] == h and runs[-1][1] + runs[-1][2] == t:
                            runs[-1][2] += 1
                        else:
                            runs.append([h, t, 1, k])
                        k += 1
                for (h, t0_, n_, k0) in runs:
                    P.op("scalar", (lambda h, t0_, n_, k0: lambda e: e.copy(out=Gbuf[:, t0_:t0_ + n_, 64 * h:64 * h + 64], in_=BK[7][:, k0 * 64:(k0 + n_) * 64].rearrange("p (a b) -> p a b", a=n_)))(h, t0_, n_, k0),
                         reads=["bk7"], writes=[f"Gbuf{h}"])
                del pend_b[:]

            def build_step(n):
                if pend_b:
                    emit_b()
                for _ in range(min(n, 4, len(pend_a))):
                    pa_, toks, h = pend_a.pop(0)
                    pa_()
                    pend_b.append((toks, h))

            def build_flush():
                while pend_a or pend_b:
                    build_step(4)

            load_sel(0)
            pend_a.extend(build_pieces(0, 0))
            pend_a.extend(build_pieces(0, 1))
            build_flush()
            for gi, (g0, gn) in enumerate(groups):
                P.op("sync", (lambda g0, gn: lambda e: e.dma_start(out=h2g[:, :, 0:gn], in_=h2s[:, :, g0:g0 + gn]))(g0, gn), writes=["h2g"], dma="h2g")
                P.op("sync", (lambda g0, gn: lambda e: e.dma_start(out=x1g[:, :, 0:gn], in_=x1s[:, :, g0:g0 + gn]))(g0, gn), writes=["x1g"], dma="x1g")
                if gi + 1 < len(groups):
                    load_sel(gi + 1)
                if gi > 0:
                    pend_a.extend(build_pieces(gi, 1))
                per_slot = 3

                def slot_of(c, gi=gi):
                    return (c // CPD + gi * (128 // CPD)) % 2

                def emit_dma(c):
                    slot = slot_of(c)
                    e0 = c * 128
                    P.op("sync", (lambda slot, e0: lambda e: e.dma_start(out=utb[slot][:], in_=UTb[:, e0:e0 + CPD * 128].rearrange("(k p) n -> p k n", p=128)))(slot, e0),
                         writes=[f"utb{slot}"], dma=f"utb{slot}")
                    P.op("sync", (lambda slot, e0: lambda e: e.dma_start(out=vbb[slot][:], in_=Vb[e0:e0 + CPD * 128, :].rearrange("(a p) n -> p a n", p=128)))(slot, e0),
                         writes=[f"vbb{slot}"], dma=f"vbb{slot}")

                def emit_m1(c, gn=gn):
                    slot = slot_of(c)
                    ci = c % CPD
                    for kc in range(8):
                        P.op("tensor", (lambda slot, ci, kc, gn: lambda e: e.matmul(pm[0][:, 0:gn], lhsT=utb[slot][:, kc, ci * 128:(ci + 1) * 128], rhs=h2g[:, kc, 0:gn], start=(kc == 0), stop=(kc == 7)))(slot, ci, kc, gn),
                             reads=[f"utb{slot}", "h2g"], writes=["bk6"])

                def emit_act(c, gn=gn):
                    a_, w_ = ag[c % 2], wT[c % 2]
                    P.op("scalar", (lambda a_, gn: lambda e: e.activation(out=a_[:, 0:gn], in_=pm[0][:, 0:gn], func=AF.Gelu))(a_, gn), reads=["bk6"], writes=[f"ag{c % 2}"])
                    P.op("vector", (lambda a_, w_, c, gn: lambda e: e.tensor_tensor(out=w_[:, 0:gn], in0=a_[:, 0:gn], in1=Gbuf[:, 0:gn, c], op=ALU.mult))(a_, w_, c, gn),
                         reads=[f"ag{c % 2}", f"Gbuf{c // 64}"], writes=[f"wT{c % 2}"])

                def emit_m2(c, gn=gn):
                    slot = slot_of(c)
                    ci = c % CPD
                    w_ = wT[c % 2]
                    for tt in range(gn // 128):
                        for dh in range(2):
                            bi = tt * 2 + dh
                            P.op("tensor", (lambda w_, slot, ci, tt, dh, bi, c: lambda e: e.matmul(BK[bi][:, :], lhsT=w_[:, tt * 128:(tt + 1) * 128], rhs=vbb[slot][:, ci, dh * 512:(dh + 1) * 512],
                                                                                                  start=(c == 0), stop=(c == 127)))(w_, slot, ci, tt, dh, bi, c),
                                 reads=[f"vbb{slot}", f"wT{c % 2}"], writes=[f"bk{bi}"])

                for c in range(128):
                    if c == 64:
                        build_flush()
                        if gi + 1 < len(groups):
                            pend_a.extend(build_pieces(gi + 1, 0))
                    if c % CPD == 0:
                        emit_dma(c)
                    emit_m1(c)
                    if c > 0:
                        emit_m2(c - 1)
                    emit_act(c)
                    build_step(per_slot)
                emit_m2(127)
                build_flush()
                for tt in range(gn // 128):
                    for dh in range(2):
                        bi = tt * 2 + dh
                        if bi % 2 == 0:
                            P.op("scalar", (lambda tt, dh, bi: lambda e: e.copy(out=peer_sb[:, tt, dh * 512:(dh + 1) * 512], in_=BK[bi][:, :]))(tt, dh, bi), reads=[f"bk{bi}"], writes=[f"peer{tt}"])
                        else:
                            P.op("vector", (lambda tt, dh, bi: lambda e: e.tensor_copy(out=peer_sb[:, tt, dh * 512:(dh + 1) * 512], in_=BK[bi][:, :]))(tt, dh, bi), reads=[f"bk{bi}"], writes=[f"peer{tt}"])
                for j in range(8):
                    for tt in range(gn // 128):
                        P.op("tensor", (lambda j, tt: lambda e: e.transpose(out=BK[j][:, tt * 128:(tt + 1) * 128], in_=peer_sb[:, tt, j * 128:(j + 1) * 128], identity=ident_f[:]))(j, tt),
                             reads=[f"peer{tt}"], writes=bkk(j))
                segs = []
                tcur = g0
                while tcur < g0 + gn:
                    bb = tcur // seq
                    tend = min((bb + 1) * seq, g0 + gn)
                    segs.append((bb, tcur - g0, tend - g0))
                    tcur = tend
                for j in range(8):
                    for (bb, s0, s1) in segs:
                        P.op("vector", (lambda j, bb, s0, s1: lambda e: e.scalar_tensor_tensor(out=x1g[:, j, s0:s1], in0=BK[j][:, s0:s1], scalar=dcol(D_ADA + (40 + j) * nseq + bb),
                                                                                               in1=x1g[:, j, s0:s1], op0=ALU.mult, op1=ALU.add))(j, bb, s0, s1),
                             reads=bkk(j) + ["x1g"], writes=["x1g"])
                P.op("scalar", (lambda gn: lambda e: e.activation(out=sq2[:, :, 0:gn], in_=x1g[:, :, 0:gn], func=AF.Square))(gn), reads=["x1g"], writes=["sq2"])
                for kc in range(8):
                    P.op("tensor", (lambda kc, gn: lambda e: e.matmul(pm[0][:, 0:gn], lhsT=ones_bf[:], rhs=sq2[:, kc, 0:gn], start=(kc == 0), stop=(kc == 7)))(kc, gn), reads=["sq2"], writes=["bk6"])
                P.op("scalar", (lambda gn: lambda e: e.activation(out=rstd2[:, 0:gn], in_=pm[0][:, 0:gn], func=AF.Ln, bias=eps_t[:], scale=1.0 / D))(gn), reads=["bk6"], writes=["rstd2"])
                P.op("scalar", (lambda gn: lambda e: e.activation(out=rstd2[:, 0:gn], in_=rstd2[:, 0:gn], func=AF.Exp, scale=-0.5))(gn), reads=["rstd2"], writes=["rstd2"])
                P.op("vector", (lambda gn: lambda e: e.tensor_tensor(out=x1g[:, :, 0:gn], in0=x1g[:, :, 0:gn], in1=rstd2[:, 0:gn].unsqueeze(1).to_broadcast([128, 8, gn]), op=ALU.mult))(gn),
                     reads=["x1g", "rstd2"], writes=["x1g"])
                for j in range(8):
                    P.op("scalar", (lambda j, gn: lambda e: e.activation(out=x1g[:, j, 0:gn], in_=x1g[:, j, 0:gn], func=AF.Copy, scale=pcol(P_GF + j)))(j, gn), reads=["x1g"], writes=["x1g"])
                P.op("sync", (lambda g0, gn: lambda e: e.dma_start(out=yT[:, g0:g0 + gn].rearrange("(c p) t -> p c t", p=128), in_=x1g[:, :, 0:gn]))(g0, gn), reads=["x1g"], dma="yst")
            import os as _os2
            _mxb = _os2.environ.get("PROG_MAXOPS_B")
            print("phase B ops", len(P.ops))
            if _mxb:
                for _i, _o in enumerate(P.ops[:int(_mxb)]):
                    if _i >= int(_mxb) - 14:
                        print("OP", _i, _o["eng"], _o["dma"], "r", _o["r"], "w", _o["w"], "deps", sorted(_o["deps"]))
            P.emit(int(_mxb) if _mxb else None)
    return nc


def host_layout(inputs, core, seq, nseq):
    x = np.asarray(inputs["x"])
    c = np.asarray(inputs["c"])
    b0 = core * nseq
    ntok = seq * nseq
    xT = np.ascontiguousarray(x[b0:b0 + nseq, :seq].reshape(ntok, D).T)
    cT = np.ascontiguousarray(c[b0:b0 + nseq].reshape(nseq, 8, 128).transpose(2, 1, 0))
    return xT, cT


def shared_layout(inputs):
    f = lambda k: np.asarray(inputs[k], dtype=np.float32)
    kcp = lambda w: np.ascontiguousarray(w.reshape(8, 128, w.shape[1]).transpose(1, 0, 2))
    colT = lambda v, n: v.reshape(n, 128).T
    w_in = f("w_in")
    b_in = f("b_in")
    q_c = list(range(0, 512))
    k0, k1 = list(range(512, 576)), list(range(576, 640))
    v_c = list(range(640, 768))
    B_c = list(range(768, 1280))
    C_c = list(range(1280, 1792))
    U_c = list(range(1792, 2304))
    order = q_c + k0 + k0 + k1 + k1 + B_c + C_c + U_c + v_c
    w_in_r = kcp(w_in[:, order])
    b_in_r = b_in[order]
    prm = np.zeros((128, NP), np.float32)
    prm[:, P_BADA:P_BADA + 48] = colT(f("b_ada"), 48)
    prm[:, P_G1:P_G1 + 8] = colT(f("norm1_g"), 8)
    prm[:, P_G2:P_G2 + 8] = colT(f("norm2_g"), 8)
    prm[:, P_GF:P_GF + 8] = colT(f("final_g"), 8)
    prm[:, P_BOUT:P_BOUT + 8] = colT(f("b_out"), 8)
    prm[:, P_BIN:P_BIN + 18] = colT(b_in_r[:2304], 18)
    prm[:, P_AG:P_AG + 4] = colT(f("attn_out_g"), 4)
    prm[:, P_CG:P_CG + 4] = colT(f("conv_out_g"), 4)
    cw = f("conv_w")
    for tap in range(3):
        prm[:, P_CW + tap * 4:P_CW + tap * 4 + 4] = colT(cw[tap], 4)
    prm[:, P_SINK:P_SINK + 4] = np.repeat(f("attn_sinks"), 64).reshape(4, 128).T
    prm[:, P_BV:P_BV + 128] = np.broadcast_to(b_in_r[2304:2432][None, :], (128, 128))
    k1_, k2_ = f("peer_keys1"), f("peer_keys2")
    keys = np.stack([k1_, k2_], axis=1).reshape(16, 128, 128)
    keysT = np.ascontiguousarray(keys.transpose(2, 0, 1))
    U = f("peer_u")
    V = f("peer_v")
    UT = np.ascontiguousarray(U.reshape(128, 128, D).transpose(2, 1, 0).reshape(D, 16384))
    Vr = np.ascontiguousarray(V.reshape(128, 128, D).transpose(1, 0, 2).reshape(16384, D))
    return dict(w_ada=kcp(f("w_ada")), prm=prm, w_in=w_in_r, w_out=kcp(f("w_out")), w_q=kcp(f("w_query")), keysT=keysT, UT=UT, Vr=Vr)


def run(inputs, seq=2048, nseq=2, n_cores=N_CORES, stop_after=9):
    nc = build_nc(seq, nseq, stop_after=stop_after)
    shared = shared_layout(inputs)
    in_maps = []
    for core in range(n_cores):
        xT, cT = host_layout(inputs, core, seq, nseq)
        m = dict(shared)
        m["xT"] = xT
        m["cT"] = cT
        in_maps.append(m)
    res = run_bass_kernel_spmd(nc, in_maps, core_ids=list(range(n_cores)))
    outs = [np.asarray(r["yT"]).T.reshape(nseq, seq, D) for r in res.results]
    return np.concatenate(outs, axis=0).astype(np.float32)


def kernel(**inputs):
    return run(inputs)
```

```python
import numpy as np
from contextlib import ExitStack
import concourse.bass as bass
import concourse.mybir as mybir
from concourse.bass_utils import run_bass_kernel_spmd

F32 = mybir.dt.float32
BF16 = mybir.dt.bfloat16
U32 = mybir.dt.uint32
AF = mybir.ActivationFunctionType
ALU = mybir.AluOpType
AX = mybir.AxisListType

ENGS = ("tensor", "vector", "scalar", "gpsimd", "sync")
N_CORES = 8
D = 1024
EPS = 1e-6
NEGBIG = -1e30


class Prog:
    def __init__(self, nc, same_engine_sync=True):
        self.nc = nc
        self.ops = []
        self.last_w = {}
        self.readers = {}
        self.same_engine_sync = same_engine_sync
        self.dma_groups = {}

    def op(self, eng, fn, reads=(), writes=(), dma=None):
        i = len(self.ops)
        deps = set()
        for r in reads:
            if r in self.last_w:
                deps.add(self.last_w[r])
        for w in writes:
            if w in self.last_w:
                deps.add(self.last_w[w])
            for rd in self.readers.get(w, ()):
                deps.add(rd)
        deps.discard(i)
        for r in reads:
            lst = self.readers.setdefault(r, [])
            if dma is None:
                lst[:] = [q for q in lst if not (self.ops[q]["dma"] is None and self.ops[q]["eng"] == eng)]
            lst.append(i)
        for w in writes:
            self.last_w[w] = i
            self.readers[w] = []
        dma_idx = None
        if dma is not None:
            self.dma_groups[dma] = self.dma_groups.get(dma, 0) + 1
            dma_idx = self.dma_groups[dma]
        self.ops.append(dict(eng=eng, fn=fn, deps=deps, dma=dma, dma_idx=dma_idx, w=list(writes), r=list(reads)))
        return i

    def emit(self, max_ops=None):
        nc = self.nc
        if max_ops is not None:
            self.ops = self.ops[:max_ops]
            self.dma_groups = {}
            for o in self.ops:
                if o["dma"] is not None:
                    self.dma_groups[o["dma"]] = max(self.dma_groups.get(o["dma"], 0), o["dma_idx"])
        ops = self.ops
        ses = self.same_engine_sync

        def skip(p, o):
            return (p["dma"] is None and o["dma"] is None and p["eng"] == o["eng"]
                    and (p["eng"] == "tensor" or not ses))

        needs_inc = [False] * len(ops)
        for o in ops:
            for i in o["deps"]:
                p = ops[i]
                if p["dma"] is None and not skip(p, o):
                    needs_inc[i] = True
        cnt = {e: 0 for e in ENGS}
        sig = [0] * len(ops)
        for i, o in enumerate(ops):
            if o["dma"] is None and needs_inc[i]:
                cnt[o["eng"]] += 1
                sig[i] = cnt[o["eng"]]
        import os as _os
        if _os.environ.get("PROG_VERBOSE"):
            print("Prog: ops", len(ops), "sem counts", cnt, "dma", self.dma_groups)
        with ExitStack() as es:
            esem = {e: es.enter_context(nc.semaphore(f"pe_{e}")) for e in ENGS}
            dsem = {g: es.enter_context(nc.semaphore(f"pd_{g}")) for g in self.dma_groups}
            block = es.enter_context(nc.Block())
            per_eng = {e: [i for i, o in enumerate(ops) if o["eng"] == e] for e in ENGS}

            def make(ename):
                def body(eng):
                    waited = {}
                    for i in per_eng[ename]:
                        o = ops[i]
                        need = {}
                        for d in o["deps"]:
                            p = ops[d]
                            if p["dma"] is not None:
                                key = ("d", p["dma"])
                                val = 16 * p["dma_idx"]
                            else:
                                if skip(p, o):
                                    continue
                                key = ("e", p["eng"])
                                val = sig[d]
                            if val > need.get(key, 0):
                                need[key] = val
                        for key, val in need.items():
                            if waited.get(key, 0) >= val:
                                continue
                            waited[key] = val
                            s = dsem[key[1]] if key[0] == "d" else esem[key[1]]
                            eng.wait_ge(s, val)
                        ins = o["fn"](eng)
                        if o["dma"] is not None:
                            ins.then_inc(dsem[o["dma"]], 16)
                        elif needs_inc[i]:
                            ins.then_inc(esem[ename], 1)
                    if ename == "sync":
                        for g, n in self.dma_groups.items():
                            eng.wait_ge(dsem[g], 16 * n)
                        for e2 in ENGS:
                            if e2 != "sync" and cnt[e2] > 0:
                                eng.wait_ge(esem[e2], cnt[e2])
                return body

            block.tensor(make("tensor"))
            block.vector(make("vector"))
            block.scalar(make("scalar"))
            block.gpsimd(make("gpsimd"))
            block.sync(make("sync"))


P_BADA, P_G1, P_G2, P_GF, P_BOUT, P_BIN, P_AG, P_CG, P_CW, P_SINK, P_BV, NP = 0, 48, 56, 64, 72, 80, 98, 102, 106, 118, 122, 250
D_ADA, D_A1, D_GB1, D_A2, D_ESINK, ND = 0, 96, 112, 128, 144, 148
NIN = 2432


def build_nc(seq, nseq=2, tga=256, tgb=384, stop_after=9):
    ntok = seq * nseq
    TA = min(tga, seq)
    nc = bass.Bass("TRN2", target_bir_lowering=False)
    dt_in = lambda name, shape: nc.dram_tensor(name, shape, F32, kind="ExternalInput").ap()
    xT = dt_in("xT", [D, ntok])
    cT = dt_in("cT", [128, 8, nseq])
    w_ada = dt_in("w_ada", [128, 8, 6144])
    prm = dt_in("prm", [128, NP])
    w_in = dt_in("w_in", [128, 8, NIN])
    w_out = dt_in("w_out", [128, 8, D])
    w_q = dt_in("w_q", [128, 8, 2048])
    keysT = dt_in("keysT", [128, 16, 128])
    UT = dt_in("UT", [D, 16384])
    Vr = dt_in("Vr", [16384, D])
    yT = nc.dram_tensor("yT", [D, ntok], F32, kind="ExternalOutput").ap()
    UTb = nc.dram_tensor("UTb", [D, 16384], BF16, kind="Internal").ap()
    Vb = nc.dram_tensor("Vb", [16384, D], BF16, kind="Internal").ap()
    x1s = nc.dram_tensor("x1s", [128, 8, ntok], F32, kind="Internal").ap()
    h2s = nc.dram_tensor("h2s", [128, 8, ntok], BF16, kind="Internal").ap()
    sels = nc.dram_tensor("sels", [128, 3, ntok], F32, kind="Internal").ap()

    with ExitStack() as g_es:
        gsb = lambda name, shape, dt: g_es.enter_context(nc.sbuf_tensor(name, shape, dt))
        prm_sb = gsb("prm_sb", [128, NP], F32)
        drv = gsb("drv", [128, ND], F32)
        ones_bf = gsb("ones_bf", [128, 128], BF16)
        blk_bf = gsb("blk_bf", [128, 128], BF16)
        mask2 = gsb("mask2", [128, 4, 128], BF16)
        ident_f = gsb("ident_f", [128, 128], F32)
        iota_f = gsb("iota_f", [128, 128], F32)
        iota_b = gsb("iota_b", [128, 128], BF16)
        esink_bc = gsb("esink_bc", [128, 4, 128], F32)
        attng_bc = gsb("attng_bc", [128, 4, 128], F32)
        eps_t = gsb("eps_t", [128, 1], F32)

        def pcol(off, n=1):
            return prm_sb[:, off:off + n]

        def dcol(off, n=1):
            return drv[:, off:off + n]

        with ExitStack() as es:
            sb = lambda name, shape, dt: es.enter_context(nc.sbuf_tensor(name, shape, dt))
            c_sb = sb("c_sb", [128, 8, nseq], F32)
            sc = sb("sc", [128, 8, nseq], F32)
            wbuf = [sb(f"wada{i}", [128, 8, 1024], F32) for i in range(2)]
            tmpf = sb("tmpf", [128, 128], F32)
            tmp16 = sb("tmp16", [128, 8, nseq], F32)
            pa = es.enter_context(nc.psum_tensor("pa", [128, 512], F32))
            P = Prog(nc)
            P.op("sync", lambda e: e.dma_start(out=prm_sb[:], in_=prm), writes=["prm"], dma="prm")
            P.op("sync", lambda e: e.dma_start(out=c_sb[:], in_=cT), writes=["c"], dma="c")
            P.op("vector", lambda e: e.memset(ones_bf[:], 1.0), writes=["ones"])
            P.op("vector", lambda e: e.memset(blk_bf[:], 0.0), writes=["blk"])
            P.op("vector", lambda e: e.memset(blk_bf[0:64, 0:64], 1.0 / 64), reads=["blk"], writes=["blk"])
            P.op("vector", lambda e: e.memset(blk_bf[64:128, 64:128], 1.0 / 64), reads=["blk"], writes=["blk"])
            P.op("vector", lambda e: e.memset(eps_t[:], EPS), writes=["eps"])
            P.op("gpsimd", lambda e: e.iota(iota_f[:], pattern=[[1, 128]], base=0, channel_multiplier=0, allow_small_or_imprecise_dtypes=True), writes=["iota"])
            P.op("vector", lambda e: e.tensor_copy(out=iota_b[:], in_=iota_f[:]), reads=["iota"], writes=["iota_b"])
            P.op("gpsimd", lambda e: e.iota(tmpf[:], pattern=[[1, 128]], base=0, channel_multiplier=-1, allow_small_or_imprecise_dtypes=True), writes=["tmpf"])
            P.op("vector", lambda e: e.tensor_single_scalar(out=ident_f[:], in_=tmpf[:], scalar=0.0, op=ALU.is_equal), reads=["tmpf"], writes=["ident"])
            for hh in range(2):
                P.op("vector", (lambda hh: lambda e: e.tensor_single_scalar(out=mask2[:, 2 * hh, :], in_=tmpf[:], scalar=0.0, op=ALU.is_lt))(hh), reads=["tmpf"], writes=["mask"])
                P.op("vector", (lambda hh: lambda e: e.tensor_single_scalar(out=mask2[:, 2 * hh + 1, :], in_=tmpf[:], scalar=0.0, op=ALU.is_ge))(hh), reads=["tmpf"], writes=["mask"])
            P.op("scalar", lambda e: e.activation(out=sc[:], in_=c_sb[:], func=AF.Silu), reads=["c"], writes=["sc"])
            for piece in range(6):
                wb = wbuf[piece % 2]
                P.op("sync", (lambda piece, wb: lambda e: e.dma_start(out=wb[:], in_=w_ada[:, :, piece * 1024:(piece + 1) * 1024]))(piece, wb),
                     writes=[f"wb{piece % 2}"], dma=f"wada{piece % 2}")
                for jj in range(8):
                    j = piece * 8 + jj
                    for kc in range(8):
                        P.op("tensor", (lambda wb, jj, j, kc: lambda e: e.matmul(pa[:, j * nseq:(j + 1) * nseq], lhsT=wb[:, kc, jj * 128:(jj + 1) * 128],
                                                                                 rhs=sc[:, kc, :], start=(kc == 0), stop=(kc == 7)))(wb, jj, j, kc),
                             reads=[f"wb{piece % 2}", "sc"], writes=["pa"])
            ada_v = drv[:, D_ADA:D_ADA + 48 * nseq].rearrange("p (j b) -> p j b", b=nseq)
            P.op("vector", lambda e: e.tensor_tensor(out=ada_v, in0=pa[:, 0:48 * nseq].rearrange("p (j b) -> p j b", b=nseq),
                                                     in1=prm_sb[:, P_BADA:P_BADA + 48].unsqueeze(2).to_broadcast([128, 48, nseq]), op=ALU.add),
                 reads=["pa", "prm"], writes=["drv"])

            def adav(j0):
                return drv[:, D_ADA + j0 * nseq:D_ADA + (j0 + 8) * nseq].rearrange("p (j b) -> p j b", b=nseq)

            def dv(off):
                return drv[:, off:off + 8 * nseq].rearrange("p (j b) -> p j b", b=nseq)

            def pb(off):
                return prm_sb[:, off:off + 8].unsqueeze(2).to_broadcast([128, 8, nseq])
            P.op("vector", lambda e: e.tensor_scalar(out=tmp16[:], in0=adav(8), scalar1=1.0, scalar2=None, op0=ALU.add), reads=["drv"], writes=["tmp16"])
            P.op("vector", lambda e: e.tensor_tensor(out=dv(D_A1), in0=tmp16[:], in1=pb(P_G1), op=ALU.mult), reads=["tmp16", "prm"], writes=["drvA1"])
            P.op("vector", lambda e: e.tensor_tensor(out=dv(D_GB1), in0=adav(16), in1=pb(P_BOUT), op=ALU.mult), reads=["drv", "prm"], writes=["drvGB1"])
            P.op("vector", lambda e: e.tensor_scalar(out=tmp16[:], in0=adav(32), scalar1=1.0, scalar2=None, op0=ALU.add), reads=["drv", "drvA1"], writes=["tmp16"])
            P.op("vector", lambda e: e.tensor_tensor(out=dv(D_A2), in0=tmp16[:], in1=pb(P_G2), op=ALU.mult), reads=["tmp16", "prm"], writes=["drvA2"])
            P.op("scalar", lambda e: e.activation(out=drv[:, D_ESINK:D_ESINK + 4], in_=prm_sb[:, P_SINK:P_SINK + 4], func=AF.Exp), reads=["prm"], writes=["esink"])
            P.op("vector", lambda e: e.tensor_copy(out=esink_bc[:], in_=drv[:, D_ESINK:D_ESINK + 4].unsqueeze(2).to_broadcast([128, 4, 128])), reads=["esink"], writes=["esink_bc"])
            P.op("vector", lambda e: e.tensor_copy(out=attng_bc[:], in_=prm_sb[:, P_AG:P_AG + 4].unsqueeze(2).to_broadcast([128, 4, 128])), reads=["prm"], writes=["attng_bc"])
            P.emit()
        nc.all_engine_barrier()
        if stop_after < 1:
            return nc

        with ExitStack() as es:
            sb = lambda name, shape, dt: es.enter_context(nc.sbuf_tensor(name, shape, dt))
            win_sb = sb("win_sb", [128, 8, NIN], BF16)
            wout_sb = sb("wout_sb", [128, 8, D], BF16)
            wq_sb = sb("wq_sb", [128, 8, 2048], BF16)
            keys_sb = sb("keys_sb", [128, 16, 128], BF16)
            xt = [sb(f"xt{i}", [128, 8, TA], F32) for i in range(2)]
            sq = sb("sq", [128, 8, TA], BF16)
            rstd = sb("rstd", [128, TA], F32)
            tmpn = sb("tmpn", [128, 8, TA], F32)
            hT = sb("hT", [128, 8, TA], BF16)
            qT = sb("qT", [128, 4, TA], BF16)
            kT = sb("kT", [128, 2, 128 + TA], BF16)
            NB = TA // 128
            vtok = sb("vtok", [128, 1 + NB, 128], BF16)
            Bt = sb("Bt", [128, 4, TA], F32)
            zb = sb("zb", [128, 4, 2 + TA], F32)
            acc = sb("acc", [128, 4, TA], F32)
            Ct = acc
            sqc = sq[:, 0:4, :]
            rsc = sb("rsc", [128, TA], F32)
            catT = sb("catT", [128, 8, TA], BF16)
            pT = [sb(f"pT{i}", [128, 4, 128], BF16) for i in range(4)]
            t1 = sb("t1", [128, 4, 128], F32)
            yat = sb("yat", [128, 4, 128], F32)
            sqa = sb("sqa", [128, 4, 128], BF16)
            t2 = sb("t2", [128, 4, 128], F32)
            otmp = sb("otmp", [128, TA], F32)
            h2T = hT
            qpT = sb("qpT", [128, 16, TA], BF16)
            S_sb = sb("S_sb", [128, NB, 16, 128], F32)
            Vt = sb("Vt", [128, 16, 16], F32)
            It = sb("It", [128, 16, 16], U32)
            Itf = sb("Itf", [128, 16, 16], BF16)
            cand = sb("cand", [128, 8, 256], F32)
            SC = sb("SC", [128, 8, 16], F32)
            CI = sb("CI", [128, 8, 16], U32)
            au = sb("au", [128, 8, 16], U32)
            bu = sb("bu", [128, 8, 16], U32)
            af_ = sb("af_", [128, 128], F32)
            bf_ = sb("bf_", [128, 128], F32)
            ohb = tmpn[:].rearrange("p c t -> p (c t)").bitcast(BF16)[:, 0:2048].rearrange("p (a b) -> p a b", b=16)
            sel = sb("sel", [128, 3, 128], F32)
            ee = sb("ee", [128, 8, 16], F32)
            zz = sb("zz", [128, 8], F32)
            selT = sb("selT", [128, 3, 128], F32)
            ps = [es.enter_context(nc.psum_tensor(f"ps{i}", [128, 512], F32)) for i in range(8)]
            P = Prog(nc)
            for kc in range(8):
                P.op("gpsimd", (lambda kc: lambda e: e.dma_start(out=win_sb[:, kc, :], in_=w_in[:, kc, :]))(kc), writes=[f"win{kc}"], dma=f"w_in{kc}")
            P.op("gpsimd", lambda e: e.dma_start(out=keys_sb[:], in_=keysT), writes=["keys"], dma="w_k")
            for kc in range(0, 8, 2):
                P.op("gpsimd", (lambda kc: lambda e: e.dma_start(out=wout_sb[:, kc:kc + 2, :], in_=w_out[:, kc:kc + 2, :]))(kc), writes=[f"wout{kc}", f"wout{kc + 1}"], dma=f"w_out{kc}")
            for kc in range(8):
                P.op("gpsimd", (lambda kc: lambda e: e.dma_start(out=wq_sb[:, kc, :], in_=w_q[:, kc, :]))(kc), writes=[f"wq{kc}"], dma=f"w_q{kc}")
            NPC = 16
            for i in range(NPC):
                r0, r1 = i * (D // NPC), (i + 1) * (D // NPC)
                P.op("gpsimd", (lambda r0, r1: lambda e: e.dma_start(out=UTb[r0:r1, :], in_=UT[r0:r1, :]))(r0, r1), dma="cvt")
            for i in range(NPC):
                r0, r1 = i * (16384 // NPC), (i + 1) * (16384 // NPC)
                P.op("gpsimd", (lambda r0, r1: lambda e: e.dma_start(out=Vb[r0:r1, :], in_=Vr[r0:r1, :]))(r0, r1), dma="cvt")
            WIN = [f"win{k}" for k in range(8)]
            WOUT = [f"wout{k}" for k in range(8)]
            WQ = [f"wq{k}" for k in range(8)]
            ntile = ntok // TA
            tiles_per_seq = seq // TA

            def rms_stats(P, src, srckey, pbank, tag):
                P.op("scalar", lambda e: e.activation(out=sq[:], in_=src[:], func=AF.Square), reads=[srckey], writes=["sq"])
                for kc in range(8):
                    P.op("tensor", (lambda kc: lambda e: e.matmul(pbank[:, 0:TA], lhsT=ones_bf[:], rhs=sq[:, kc, :], start=(kc == 0), stop=(kc == 7)))(kc),
                         reads=["sq"], writes=[tag])
                P.op("scalar", lambda e: e.activation(out=rstd[:], in_=pbank[:, 0:TA], func=AF.Ln, bias=eps_t[:], scale=1.0 / D), reads=[tag], writes=["rstd"])
                P.op("scalar", lambda e: e.activation(out=rstd[:], in_=rstd[:], func=AF.Exp, scale=-0.5), reads=["rstd"], writes=["rstd"])

            pending = []

            def drain(n):
                for _ in range(min(n, len(pending))):
                    pending.pop(0)()

            def topk_pieces(blk, tq):
                pcs = []
                S = lambda j: S_sb[:, blk, j, :]
                sk = lambda j: f"S{blk}_{j}"
                J = range(16)
                H = range(8)
                pcs.append(lambda: [P.op("vector", (lambda j: lambda e: e.max(out=Vt[:, j, 0:8], in_=S(j)))(j), reads=[sk(j)], writes=[f"Vta{j}"]) for j in J])
                pcs.append(lambda: [P.op("vector", (lambda j: lambda e: e.max_index(out=It[:, j, 0:8], in_max=Vt[:, j, 0:8], in_values=S(j)))(j), reads=[sk(j), f"Vta{j}"], writes=[f"Ita{j}"]) for j in J])
                pcs.append(lambda: [P.op("vector", (lambda j: lambda e: e.match_replace(out=S(j), in_to_replace=Vt[:, j, 0:8], in_values=S(j), imm_value=NEGBIG))(j), reads=[sk(j), f"Vta{j}"], writes=[sk(j)]) for j in J])
                pcs.append(lambda: [P.op("vector", (lambda j: lambda e: e.max(out=Vt[:, j, 8:16], in_=S(j)))(j), reads=[sk(j)], writes=[f"Vtb{j}"]) for j in J])
                pcs.append(lambda: [P.op("vector", (lambda j: lambda e: e.max_index(out=It[:, j, 8:16], in_max=Vt[:, j, 8:16], in_values=S(j)))(j), reads=[sk(j), f"Vtb{j}"], writes=[f"Itb{j}"]) for j in J])
                VT = [f"Vta{j}" for j in J] + [f"Vtb{j}" for j in J]
                IT = [f"Ita{j}" for j in J] + [f"Itb{j}" for j in J]
                CAND = [f"cand{h}" for h in H]
                Vv = Vt[:].rearrange("p (h s) a -> p h s a", s=2)
                pcs.append(lambda: P.op("vector", lambda e: e.tensor_tensor(out=cand[:].rearrange("p h (a b) -> p h a b", a=16), in0=Vv[:, :, 0, :].unsqueeze(3).to_broadcast([128, 8, 16, 16]),
                                                                          in1=Vv[:, :, 1, :].unsqueeze(2).to_broadcast([128, 8, 16, 16]), op=ALU.add), reads=VT, writes=CAND))
                pcs.append(lambda: [P.op("vector", (lambda h: lambda e: e.max(out=SC[:, h, 0:8], in_=cand[:, h, :]))(h), reads=[f"cand{h}"], writes=[f"SCa{h}"]) for h in H])
                pcs.append(lambda: [P.op("vector", (lambda h: lambda e: e.max_index(out=CI[:, h, 0:8], in_max=SC[:, h, 0:8], in_values=cand[:, h, :]))(h), reads=[f"cand{h}", f"SCa{h}"], writes=[f"CIa{h}"]) for h in H])
                pcs.append(lambda: [P.op("vector", (lambda h: lambda e: e.match_replace(out=cand[:, h, :], in_to_replace=SC[:, h, 0:8], in_values=cand[:, h, :], imm_value=NEGBIG))(h), reads=[f"cand{h}", f"SCa{h}"], writes=[f"cand{h}"]) for h in H])
                pcs.append(lambda: [P.op("vector", (lambda h: lambda e: e.max(out=SC[:, h, 8:16], in_=cand[:, h, :]))(h), reads=[f"cand{h}"], writes=[f"SCb{h}"]) for h in H])
                pcs.append(lambda: [P.op("vector", (lambda h: lambda e: e.max_index(out=CI[:, h, 8:16], in_max=SC[:, h, 8:16], in_values=cand[:, h, :]))(h), reads=[f"cand{h}", f"SCb{h}"], writes=[f"CIb{h}"]) for h in H])
                SCK = [f"SCa{h}" for h in H] + [f"SCb{h}" for h in H]
                CIK = [f"CIa{h}" for h in H] + [f"CIb{h}" for h in H]

                def gates():
                    P.op("vector", lambda e: e.tensor_tensor(out=ee[:], in0=SC[:], in1=SC[:, :, 0:1].to_broadcast([128, 8, 16]), op=ALU.subtract), reads=SCK, writes=["ee"])
                    P.op("scalar", lambda e: e.activation(out=ee[:], in_=ee[:], func=AF.Exp), reads=["ee"], writes=["ee"])
                    P.op("vector", lambda e: e.tensor_reduce(out=zz[:], in_=ee[:], axis=AX.X, op=ALU.add), reads=["ee"], writes=["zz"])
                    P.op("vector", lambda e: e.reciprocal(out=zz[:], in_=zz[:]), reads=["zz"], writes=["zz"])
                    P.op("vector", lambda e: e.tensor_tensor(out=sel[:, 2, :].rearrange("p (h k) -> p h k", h=8), in0=ee[:], in1=zz[:].unsqueeze(2).to_broadcast([128, 8, 16]), op=ALU.mult),
                         reads=["ee", "zz"], writes=["sel2"])
                    P.op("vector", lambda e: e.tensor_single_scalar(out=au[:], in_=CI[:], scalar=4, op=ALU.logical_shift_right), reads=CIK, writes=["au"])
                    P.op("vector", lambda e: e.tensor_single_scalar(out=bu[:], in_=CI[:], scalar=15, op=ALU.bitwise_and), reads=CIK, writes=["bu"])
                    P.op("vector", lambda e: e.tensor_copy(out=af_[:], in_=au[:].rearrange("p h k -> p (h k)")), reads=["au"], writes=["af"])
                    P.op("vector", lambda e: e.tensor_copy(out=bf_[:], in_=bu[:].rearrange("p h k -> p (h k)")), reads=["bu"], writes=["bf"])
                    P.op("vector", lambda e: e.tensor_copy(out=Itf[:], in_=It[:]), reads=IT, writes=["Itf"])
                pcs.append(gates)

                def decode():
                    Iv = Itf[:].rearrange("p (h s) a -> p h s a", s=2)
                    for s_, src in ((0, af_), (1, bf_)):
                        P.op("vector", (lambda src: lambda e: e.tensor_tensor(out=ohb, in0=iota_f[:, 0:16].unsqueeze(1).to_broadcast([128, 128, 16]),
                                                                              in1=src[:].unsqueeze(2).to_broadcast([128, 128, 16]), op=ALU.is_equal))(src),
                             reads=["af", "bf"], writes=["tmpn"])
                        P.op("vector", (lambda s_: lambda e: e.tensor_tensor(out=ohb.rearrange("p (h k) a -> p h k a", h=8), in0=ohb.rearrange("p (h k) a -> p h k a", h=8),
                                                                             in1=Iv[:, :, s_, :].unsqueeze(2).to_broadcast([128, 8, 16, 16]), op=ALU.mult))(s_),
                             reads=["tmpn", "Itf"], writes=["tmpn"])
                        P.op("vector", (lambda s_: lambda e: e.tensor_reduce(out=sel[:, s_, :], in_=ohb, axis=AX.X, op=ALU.add))(s_), reads=["tmpn"], writes=[f"sel{s_}"])
                    for s_ in range(3):
                        P.op("tensor", (lambda s_: lambda e: e.transpose(out=ps[6][:, s_ * 128:(s_ + 1) * 128], in_=sel[:, s_, :], identity=ident_f[:]))(s_),
                             reads=[f"sel{s_}"], writes=["ps6"])
                    P.op("scalar", lambda e: e.copy(out=selT[:].rearrange("p a b -> p (a b)"), in_=ps[6][:, 0:384]), reads=["ps6"], writes=["selT"])
                    P.op("sync", (lambda tq: lambda e: e.dma_start(out=sels[:, :, tq:tq + 128], in_=selT[:]))(tq), reads=["selT"], dma="selst")
                pcs.append(decode)
                return pcs

            for m in range(ntile):
                b = m // tiles_per_seq
                first = (m % tiles_per_seq == 0)
                tok0 = m * TA
                x = xt[m % 2]
                xk = f"xt{m % 2}"
                P.op("sync", (lambda x, tok0: lambda e: e.dma_start(out=x[:], in_=xT[:, tok0:tok0 + TA].rearrange("(c p) t -> p c t", p=128)))(x, tok0),
                     writes=[xk], dma=xk)
                rms_stats(P, x, xk, ps[7], "ps7")
                P.op("gpsimd", (lambda x: lambda e: e.tensor_tensor(out=tmpn[:], in0=x[:], in1=rstd[:].unsqueeze(1).to_broadcast([128, 8, TA]), op=ALU.mult))(x),
                     reads=[xk, "rstd"], writes=["tmpn"])
                for c in range(8):
                    P.op("scalar", (lambda c, b: lambda e: e.activation(out=hT[:, c, :], in_=tmpn[:, c, :], func=AF.Identity,
                                                                        scale=dcol(D_A1 + c * nseq + b), bias=dcol(D_ADA + (0 + c) * nseq + b)))(c, b),
                         reads=["tmpn"], writes=[f"hT{c}"])
                HT = [f"hT{c}" for c in range(8)]
                if first:
                    P.op("vector", lambda e: e.memset(zb[:, :, 0:2], 0.0), reads=["zb"], writes=["zb"])
                for j in range(18):
                    if j % 3 == 0 and j < 15:
                        drain(1)
                    pb_ = ps[j % 6]
                    pk = f"ps{j % 6}"
                    for kc in range(8):
                        P.op("tensor", (lambda pb_, j, kc: lambda e: e.matmul(pb_[:, 0:TA], lhsT=win_sb[:, kc, j * 128:(j + 1) * 128], rhs=hT[:, kc, :],
                                                                              start=(kc == 0), stop=(kc == 7)))(pb_, j, kc),
                             reads=[f"win{kc}", f"hT{kc}"], writes=[pk])
                    bias = pcol(P_BIN + j)
                    if j < 4:
                        P.op("scalar", (lambda pb_, j, bias: lambda e: e.activation(out=qT[:, j, :], in_=pb_[:, 0:TA], func=AF.Identity, bias=bias))(pb_, j, bias),
                             reads=[pk], writes=["qT"])
                    elif j < 6:
                        P.op("scalar", (lambda pb_, j, bias: lambda e: e.activation(out=kT[:, j - 4, 128:128 + TA], in_=pb_[:, 0:TA], func=AF.Identity, bias=bias))(pb_, j, bias),
                             reads=[pk], writes=["kTcur"])
                    elif j < 10:
                        P.op("scalar", (lambda pb_, j, bias: lambda e: e.activation(out=Bt[:, j - 6, :], in_=pb_[:, 0:TA], func=AF.Identity, bias=bias))(pb_, j, bias),
                             reads=[pk], writes=["Bt"])
                    elif j < 14:
                        P.op("scalar", (lambda pb_, j, bias: lambda e: e.activation(out=Ct[:, j - 10, :], in_=pb_[:, 0:TA], func=AF.Identity, bias=bias))(pb_, j, bias),
                             reads=[pk], writes=[f"acc{j - 10}"])
                    else:
                        P.op("vector", (lambda pb_, j, bias: lambda e: e.scalar_tensor_tensor(out=zb[:, j - 14, 2:2 + TA], in0=pb_[:, 0:TA], scalar=bias,
                                                                                               in1=Ct[:, j - 14, :], op0=ALU.add, op1=ALU.mult))(pb_, j, bias),
                             reads=[pk, f"acc{j - 14}"], writes=["zb"])
                for blk in range(NB):
                    pb_ = ps[blk % 2]
                    pk = f"ps{blk % 2}"
                    for kc in range(8):
                        P.op("tensor", (lambda pb_, blk, kc: lambda e: e.matmul(pb_[:, 0:128], lhsT=hT[:, kc, blk * 128:(blk + 1) * 128], rhs=win_sb[:, kc, 2304:2432],
                                                                                start=(kc == 0), stop=(kc == 7)))(pb_, blk, kc),
                             reads=[f"win{kc}", f"hT{kc}"], writes=[pk])
                    P.op("vector", (lambda pb_, blk: lambda e: e.tensor_tensor(out=vtok[:, 1 + blk, :], in0=pb_[:, 0:128], in1=prm_sb[:, P_BV:P_BV + 128], op=ALU.add))(pb_, blk),
                         reads=[pk], writes=["vcur"])
                for cc in range(4):
                    P.op("scalar", (lambda cc: lambda e: e.activation(out=acc[:, cc, :], in_=zb[:, cc, 2:2 + TA], func=AF.Copy, scale=pcol(P_CW + 2 * 4 + cc)))(cc),
                         reads=["zb"], writes=[f"acc{cc}"])
                    P.op("vector", (lambda cc: lambda e: e.scalar_tensor_tensor(out=acc[:, cc, :], in0=zb[:, cc, 1:1 + TA], scalar=pcol(P_CW + 1 * 4 + cc), in1=acc[:, cc, :],
                                                                                op0=ALU.mult, op1=ALU.add))(cc), reads=["zb", f"acc{cc}"], writes=[f"acc{cc}"])
                    P.op("vector", (lambda cc: lambda e: e.scalar_tensor_tensor(out=acc[:, cc, :], in0=zb[:, cc, 0:TA], scalar=pcol(P_CW + 0 * 4 + cc), in1=acc[:, cc, :],
                                                                                op0=ALU.mult, op1=ALU.add))(cc), reads=["zb", f"acc{cc}"], writes=[f"acc{cc}"])
                    P.op("gpsimd", (lambda cc: lambda e: e.tensor_tensor(out=acc[:, cc, :], in0=acc[:, cc, :], in1=Bt[:, cc, :], op=ALU.mult))(cc),
                         reads=["Bt", f"acc{cc}"], writes=[f"acc{cc}"])
                ACC = [f"acc{cc}" for cc in range(4)]
                P.op("gpsimd", lambda e: e.tensor_copy(out=zb[:, :, 0:2], in_=zb[:, :, TA:TA + 2]), reads=["zb"] + ACC, writes=["zb"])
                P.op("scalar", lambda e: e.activation(out=sqc, in_=acc[:], func=AF.Square), reads=ACC, writes=["sq"])
                for cc in range(4):
                    pb_ = ps[2 + cc % 2]
                    pk = f"ps{2 + cc % 2}"
                    P.op("tensor", (lambda pb_, cc: lambda e: e.matmul(pb_[:, 0:TA], lhsT=blk_bf[:], rhs=sqc[:, cc, :], start=True, stop=True))(pb_, cc),
                         reads=["sq"], writes=[pk])
                    P.op("scalar", (lambda pb_: lambda e: e.activation(out=rsc[:], in_=pb_[:, 0:TA], func=AF.Ln, bias=eps_t[:], scale=1.0))(pb_), reads=[pk], writes=["rsc"])
                    P.op("scalar", lambda e: e.activation(out=rsc[:], in_=rsc[:], func=AF.Exp, scale=-0.5), reads=["rsc"], writes=["rsc"])
                    P.op("vector", (lambda cc: lambda e: e.scalar_tensor_tensor(out=catT[:, 4 + cc, :], in0=acc[:, cc, :], scalar=pcol(P_CG + cc), in1=rsc[:],
                                                                                op0=ALU.mult, op1=ALU.mult))(cc), reads=[f"acc{cc}", "rsc"], writes=[f"cat{4 + cc}"])
                for blk in range(NB):
                    hasprev = not (first and blk == 0)
                    q0 = blk * 128
                    kprev = slice(q0, q0 + 128)
                    kcur = slice(128 + q0, 256 + q0)
                    vprev, vcur = blk, blk + 1
                    po = ps[4]
                    pd = ps[5]
                    for pp in range(2):
                        g = pp
                        for ci in range(2):
                            c = 2 * pp + ci
                            for hh in range(2):
                                lo, hi = hh * 64, hh * 64 + 64
                                bank = ps[2 * pp + hh]
                                pk = f"ps{2 * pp + hh}"
                                if hasprev:
                                    P.op("tensor", (lambda bank, ci, lo, hi, g, c, kprev, q0: lambda e: e.matmul(bank[:, ci * 256:ci * 256 + 128], lhsT=kT[lo:hi, g, kprev],
                                                                                                               rhs=qT[lo:hi, c, q0:q0 + 128], start=True, stop=True))(bank, ci, lo, hi, g, c, kprev, q0),
                                         reads=["qT", "kTcur", "kTprev"], writes=[pk])
                                P.op("tensor", (lambda bank, ci, lo, hi, g, c, kcur, q0: lambda e: e.matmul(bank[:, ci * 256 + 128:ci * 256 + 256], lhsT=kT[lo:hi, g, kcur],
                                                                                                          rhs=qT[lo:hi, c, q0:q0 + 128], start=True, stop=True))(bank, ci, lo, hi, g, c, kcur, q0),
                                     reads=["qT", "kTcur", "kTprev"], writes=[pk])
                        drain(1)
                        for hh in range(2):
                            bank = ps[2 * pp + hh]
                            pk = f"ps{2 * pp + hh}"
                            pt = pT[2 * pp + hh]
                            ptk = f"pT{2 * pp + hh}"
                            if hasprev:
                                P.op("scalar", (lambda bank, pt: lambda e: e.activation(out=pt[:].rearrange("p a b -> p (a b)"), in_=bank[:, :], func=AF.Exp, scale=0.125))(bank, pt),
                                     reads=[pk], writes=[ptk])
                                P.op("vector", (lambda pt: lambda e: e.tensor_tensor(out=pt[:], in0=pt[:], in1=mask2[:], op=ALU.mult))(pt), reads=[ptk], writes=[ptk])
                            else:
                                psv = bank[:, :].rearrange("p (h a i) -> p h a i", h=2, a=2)[:, :, 1, :]
                                ptv = pt[:].rearrange("p (h a) i -> p h a i", h=2)[:, :, 1, :]
                                mkv = mask2[:].rearrange("p (h a) i -> p h a i", h=2)[:, :, 1, :]
                                P.op("scalar", (lambda psv, ptv: lambda e: e.activation(out=ptv, in_=psv, func=AF.Exp, scale=0.125))(psv, ptv), reads=[pk], writes=[ptk])
                                P.op("vector", (lambda ptv, mkv: lambda e: e.tensor_tensor(out=ptv, in0=ptv, in1=mkv, op=ALU.mult))(ptv, mkv), reads=[ptk], writes=[ptk])
                        for ci in range(2):
                            c = 2 * pp + ci
                            for hh in range(2):
                                lo, hi = hh * 64, hh * 64 + 64
                                pt = pT[2 * pp + hh]
                                ptk = f"pT{2 * pp + hh}"
                                oc = slice(c * 128, (c + 1) * 128)
                                if hasprev:
                                    P.op("tensor", (lambda pt, ci, lo, hi, g, oc, vprev: lambda e: e.matmul(po[lo:hi, oc], lhsT=vtok[:, vprev, g * 64:(g + 1) * 64], rhs=pt[:, 2 * ci, :],
                                                                                                          start=True, stop=False))(pt, ci, lo, hi, g, oc, vprev),
                                         reads=[ptk, "vcur", "vprev"], writes=["ps4"])
                                P.op("tensor", (lambda pt, ci, lo, hi, g, oc, vcur, hasprev: lambda e: e.matmul(po[lo:hi, oc], lhsT=vtok[:, vcur, g * 64:(g + 1) * 64], rhs=pt[:, 2 * ci + 1, :],
                                                                                                              start=(not hasprev), stop=True))(pt, ci, lo, hi, g, oc, vcur, hasprev),
                                     reads=[ptk, "vcur", "vprev"], writes=["ps4"])
                                if hasprev:
                                    P.op("tensor", (lambda pt, ci, lo, hi, oc: lambda e: e.matmul(pd[lo:hi, oc], lhsT=ones_bf[:, 0:64], rhs=pt[:, 2 * ci, :], start=True, stop=False))(pt, ci, lo, hi, oc),
                                         reads=[ptk], writes=["ps5"])
                                P.op("tensor", (lambda pt, ci, lo, hi, oc, hasprev: lambda e: e.matmul(pd[lo:hi, oc], lhsT=ones_bf[:, 0:64], rhs=pt[:, 2 * ci + 1, :], start=(not hasprev), stop=True))(pt, ci, lo, hi, oc, hasprev),
                                     reads=[ptk], writes=["ps5"])
                    pov = po[:, :].rearrange("p (c i) -> p c i", c=4)
                    pdv = pd[:, :].rearrange("p (c i) -> p c i", c=4)
                    P.op("vector", lambda e: e.tensor_tensor(out=t1[:], in0=pdv, in1=esink_bc[:], op=ALU.add), reads=["ps5"], writes=["t1"])
                    P.op("scalar", lambda e: e.activation(out=t1[:], in_=t1[:], func=AF.Ln), reads=["t1"], writes=["t1"])
                    P.op("scalar", lambda e: e.activation(out=t1[:], in_=t1[:], func=AF.Exp, scale=-1.0), reads=["t1"], writes=["t1"])
                    P.op("vector", lambda e: e.tensor_tensor(out=yat[:], in0=pov, in1=t1[:], op=ALU.mult), reads=["ps4", "t1"], writes=["yat"])
                    P.op("scalar", lambda e: e.activation(out=sqa[:], in_=yat[:], func=AF.Square), reads=["yat"], writes=["sqa"])
                    P.op("tensor", lambda e: e.matmul(ps[6][:, :], lhsT=blk_bf[:], rhs=sqa[:].rearrange("p c i -> p (c i)"), start=True, stop=True), reads=["sqa"], writes=["ps6"])
                    P.op("scalar", lambda e: e.activation(out=t2[:].rearrange("p c i -> p (c i)"), in_=ps[6][:, :], func=AF.Ln, bias=eps_t[:], scale=1.0), reads=["ps6"], writes=["t2"])
                    P.op("scalar", lambda e: e.activation(out=t2[:], in_=t2[:], func=AF.Exp, scale=-0.5), reads=["t2"], writes=["t2"])
                    P.op("vector", lambda e: e.tensor_tensor(out=yat[:], in0=yat[:], in1=t2[:], op=ALU.mult), reads=["yat", "t2"], writes=["yat"])
                    P.op("vector", (lambda q0: lambda e: e.tensor_tensor(out=catT[:, 0:4, q0:q0 + 128], in0=yat[:], in1=attng_bc[:], op=ALU.mult))(q0),
                         reads=["yat"], writes=[f"cat{c}" for c in range(4)])
                P.op("gpsimd", lambda e: e.tensor_copy(out=kT[:, :, 0:128], in_=kT[:, :, TA:TA + 128]), reads=["kTcur", "kTprev"], writes=["kTprev", "kTcur"])
                P.op("gpsimd", lambda e: e.tensor_copy(out=vtok[:, 0, :], in_=vtok[:, NB, :]), reads=["vcur", "vprev"], writes=["vprev", "vcur"])
                CAT = [f"cat{c}" for c in range(8)]
                for j in range(8):
                    if j % 2 == 0:
                        drain(1)
                    pb_ = ps[j % 6]
                    pk = f"ps{j % 6}"
                    for kc in range(8):
                        P.op("tensor", (lambda pb_, j, kc: lambda e: e.matmul(pb_[:, 0:TA], lhsT=wout_sb[:, kc, j * 128:(j + 1) * 128], rhs=catT[:, kc, :], start=(kc == 0), stop=(kc == 7)))(pb_, j, kc),
                             reads=[f"wout{kc}", f"cat{kc}"], writes=[pk])
                    P.op("scalar", (lambda pb_, j, b: lambda e: e.activation(out=otmp[:], in_=pb_[:, 0:TA], func=AF.Identity, scale=dcol(D_ADA + (16 + j) * nseq + b),
                                                                             bias=dcol(D_GB1 + j * nseq + b)))(pb_, j, b), reads=[pk], writes=["otmp"])
                    P.op("gpsimd", (lambda x, j: lambda e: e.tensor_tensor(out=x[:, j, :], in0=x[:, j, :], in1=otmp[:], op=ALU.add))(x, j), reads=["otmp", xk], writes=[xk])
                P.op("sync", (lambda x, tok0: lambda e: e.dma_start(out=x1s[:, :, tok0:tok0 + TA], in_=x[:]))(x, tok0), reads=[xk], dma=f"x1st{m % 2}")
                rms_stats(P, x, xk, ps[7], "ps7")
                P.op("gpsimd", (lambda x: lambda e: e.tensor_tensor(out=tmpn[:], in0=x[:], in1=rstd[:].unsqueeze(1).to_broadcast([128, 8, TA]), op=ALU.mult))(x),
                     reads=[xk, "rstd"], writes=["tmpn"])
                for c in range(8):
                    P.op("scalar", (lambda c, b: lambda e: e.activation(out=h2T[:, c, :], in_=tmpn[:, c, :], func=AF.Identity,
                                                                        scale=dcol(D_A2 + c * nseq + b), bias=dcol(D_ADA + (24 + c) * nseq + b)))(c, b),
                         reads=["tmpn"], writes=[f"hT{c}"])
                H2 = [f"hT{c}" for c in range(8)]
                P.op("sync", (lambda tok0: lambda e: e.dma_start(out=h2s[:, :, tok0:tok0 + TA], in_=h2T[:]))(tok0), reads=H2, dma="h2st")
                for j in range(16):
                    if j % 2 == 0:
                        drain(1)
                    pb_ = ps[j % 6]
                    pk = f"ps{j % 6}"
                    for kc in range(8):
                        P.op("tensor", (lambda pb_, j, kc: lambda e: e.matmul(pb_[:, 0:TA], lhsT=wq_sb[:, kc, j * 128:(j + 1) * 128], rhs=h2T[:, kc, :], start=(kc == 0), stop=(kc == 7)))(pb_, j, kc),
                             reads=[f"wq{kc}", f"hT{kc}"], writes=[pk])
                    P.op("scalar", (lambda pb_, j: lambda e: e.copy(out=qpT[:, j, :], in_=pb_[:, 0:TA]))(pb_, j), reads=[pk], writes=[f"qp{j}"])
                drain(len(pending))
                for blk in range(NB):
                    q0 = blk * 128
                    for j4 in range(4):
                        pb_ = ps[2 + j4]
                        pk = f"ps{2 + j4}"
                        for jj in range(4):
                            j = j4 * 4 + jj
                            P.op("tensor", (lambda pb_, j, jj, q0: lambda e: e.matmul(pb_[:, jj * 128:(jj + 1) * 128], lhsT=qpT[:, j, q0:q0 + 128], rhs=keys_sb[:, j, :], start=True, stop=True))(pb_, j, jj, q0),
                                 reads=[f"qp{j}", "keys"], writes=[pk])
                        P.op("scalar", (lambda pb_, j4, blk: lambda e: e.copy(out=S_sb[:, blk, j4 * 4:(j4 + 1) * 4, :].rearrange("p a b -> p (a b)"), in_=pb_[:, :]))(pb_, j4, blk),
                             reads=[pk], writes=[f"S{blk}_{j4 * 4 + jj}" for jj in range(4)])
                for blk in range(NB):
                    pending.extend(topk_pieces(blk, tok0 + blk * 128))
            while pending:
                pending.pop(0)()
            import os as _os
            _mx = _os.environ.get("PROG_MAXOPS")
            print("phase A ops", len(P.ops))
            if _mx:
                for _i, _o in enumerate(P.ops[:int(_mx)][-3:]):
                    print("last ops", _o["eng"], _o["dma"])
            P.emit(int(_mx) if _mx else None)
        nc.all_engine_barrier()
        if stop_after < 2:
            return nc

        with ExitStack() as es:
            sb = lambda name, shape, dt: es.enter_context(nc.sbuf_tensor(name, shape, dt))
            TG = min(tgb, ntok)
            Gbuf = sb("Gbuf", [128, 128, TG], BF16)
            CPD = 4
            utb = [sb(f"utb{i}", [128, 8, CPD * 128], BF16) for i in range(2)]
            vbb = [sb(f"vbb{i}", [128, CPD, D], BF16) for i in range(2)]
            h2g = sb("h2g", [128, 8, TG], BF16)
            x1g = sb("x1g", [128, 8, TG], F32)
            selg = [sb(f"selg{i}", [128, 3, TG], F32) for i in range(2)]
            NAB = 16
            A1 = [sb(f"A1_{i}", [128, 128], BF16) for i in range(NAB)]
            A2 = [sb(f"A2_{i}", [128, 64], BF16) for i in range(NAB)]
            ag = [sb(f"ag{i}", [128, TG], BF16) for i in range(2)]
            wT = [sb(f"wT{i}", [128, TG], BF16) for i in range(2)]
            sq2 = sb("sq2", [128, 8, TG], BF16)
            rstd2 = sb("rstd2", [128, TG], F32)
            peer_sb = sb("peer_sb", [128, TG // 128, D], F32)
            BK = [es.enter_context(nc.psum_tensor(f"bk{i}", [128, 512], F32)) for i in range(8)]
            pm = [BK[6]]
            P = Prog(nc)
            groups = []
            t0 = 0
            while t0 < ntok:
                n = min(TG, ntok - t0)
                groups.append((t0, n))
                t0 += n
            ndma = 0
            bkk = lambda j: [f"bk{j}"]
            cnt_tok = [0]
            cnt_reg = [0]

            def load_sel(gi):
                g0, gn = groups[gi]
                sg = selg[gi % 2]
                P.op("sync", (lambda sg, g0, gn: lambda e: e.dma_start(out=sg[:, :, 0:gn], in_=sels[:, :, g0:g0 + gn]))(sg, g0, gn), writes=[f"selg{gi % 2}"], dma=f"selg{gi % 2}")

            def build_pieces(gi, h):
                g0, gn = groups[gi]
                sg = selg[gi % 2]
                skey = f"selg{gi % 2}"
                out = []
                for tp in range(0, gn, 2):
                    toks = []
                    for t in (tp, tp + 1):
                        i = cnt_tok[0] % NAB
                        cnt_tok[0] += 1
                        toks.append((t, i))
                    def part_a(toks=toks):
                        for (t, i) in toks:
                            P.op("vector", (lambda t, i: lambda e: e.tensor_scalar(out=A2[i][:], in0=iota_b[:, 64 * h:64 * h + 64], scalar1=sg[:, 1, t:t + 1], scalar2=sg[:, 2, t:t + 1],
                                                                                   op0=ALU.is_equal, op1=ALU.mult))(t, i),
                                 reads=[skey], writes=[f"A2_{i}"])
                            P.op("vector", (lambda t, i: lambda e: e.tensor_scalar(out=A1[i][:], in0=iota_b[:], scalar1=sg[:, 0, t:t + 1], scalar2=None, op0=ALU.is_equal))(t, i),
                                 reads=[skey], writes=[f"A1_{i}"])
                    out.append((part_a, toks, h))
                return out

            pend_a = []
            pend_b = []

            def emit_b():
                k = 0
                runs = []
                for (toks, h) in pend_b:
                    for (t, i) in toks:
                        P.op("tensor", (lambda k, i: lambda e: e.matmul(BK[7][:, k * 64:(k + 1) * 64], lhsT=A1[i][:], rhs=A2[i][:], start=True, stop=True))(k, i),
                             reads=[f"A1_{i}", f"A2_{i}"], writes=["bk7"])
                        if runs and runs[-1][0] == h and runs[-1][1] + runs[-1][2] == t:
                            runs[-1][2] += 1
                        else:
                            runs.append([h, t, 1, k])
                        k += 1
                for (h, t0_, n_, k0) in runs:
                    P.op("scalar", (lambda h, t0_, n_, k0: lambda e: e.copy(out=Gbuf[:, 64 * h:64 * h + 64, t0_:t0_ + n_], in_=BK[7][:, k0 * 64:(k0 + n_) * 64].rearrange("p (a b) -> p b a", a=n_)))(h, t0_, n_, k0),
                         reads=["bk7"], writes=[f"Gbuf{h}"])
                del pend_b[:]

            def build_step(n):
                if pend_b:
                    emit_b()
                for _ in range(min(n, 4, len(pend_a))):
                    pa_, toks, h = pend_a.pop(0)
                    pa_()
                    pend_b.append((toks, h))

            def build_flush():
                while pend_a or pend_b:
                    build_step(4)

            load_sel(0)
            pend_a.extend(build_pieces(0, 0))
            pend_a.extend(build_pieces(0, 1))
            build_flush()
            for gi, (g0, gn) in enumerate(groups):
                P.op("sync", (lambda g0, gn: lambda e: e.dma_start(out=h2g[:, :, 0:gn], in_=h2s[:, :, g0:g0 + gn]))(g0, gn), writes=["h2g"], dma="h2g")
                P.op("sync", (lambda g0, gn: lambda e: e.dma_start(out=x1g[:, :, 0:gn], in_=x1s[:, :, g0:g0 + gn]))(g0, gn), writes=["x1g"], dma="x1g")
                if gi + 1 < len(groups):
                    load_sel(gi + 1)
                if gi > 0:
                    pend_a.extend(build_pieces(gi, 1))
                per_slot = 3

                def slot_of(c, gi=gi):
                    return (c // CPD + gi * (128 // CPD)) % 2

                def emit_dma(c):
                    slot = slot_of(c)
                    e0 = c * 128
                    P.op("sync", (lambda slot, e0: lambda e: e.dma_start(out=utb[slot][:], in_=UTb[:, e0:e0 + CPD * 128].rearrange("(k p) n -> p k n", p=128)))(slot, e0),
                         writes=[f"utb{slot}"], dma=f"utb{slot}")
                    P.op("sync", (lambda slot, e0: lambda e: e.dma_start(out=vbb[slot][:], in_=Vb[e0:e0 + CPD * 128, :].rearrange("(a p) n -> p a n", p=128)))(slot, e0),
                         writes=[f"vbb{slot}"], dma=f"vbb{slot}")

                def emit_m1(c, gn=gn):
                    slot = slot_of(c)
                    ci = c % CPD
                    for kc in range(8):
                        P.op("tensor", (lambda slot, ci, kc, gn: lambda e: e.matmul(pm[0][:, 0:gn], lhsT=utb[slot][:, kc, ci * 128:(ci + 1) * 128], rhs=h2g[:, kc, 0:gn], start=(kc == 0), stop=(kc == 7)))(slot, ci, kc, gn),
                             reads=[f"utb{slot}", "h2g"], writes=["bk6"])

                def emit_act(c, gn=gn):
                    a_, w_ = ag[c % 2], wT[c % 2]
                    P.op("scalar", (lambda a_, gn: lambda e: e.activation(out=a_[:, 0:gn], in_=pm[0][:, 0:gn], func=AF.Gelu))(a_, gn), reads=["bk6"], writes=[f"ag{c % 2}"])
                    P.op("vector", (lambda a_, w_, c, gn: lambda e: e.tensor_tensor(out=w_[:, 0:gn], in0=a_[:, 0:gn], in1=Gbuf[:, c, 0:gn], op=ALU.mult))(a_, w_, c, gn),
                         reads=[f"ag{c % 2}", f"Gbuf{c // 64}"], writes=[f"wT{c % 2}"])

                def emit_m2(c, gn=gn):
                    slot = slot_of(c)
                    ci = c % CPD
                    w_ = wT[c % 2]
                    for tt in range(gn // 128):
                        for dh in range(2):
                            bi = tt * 2 + dh
                            P.op("tensor", (lambda w_, slot, ci, tt, dh, bi, c: lambda e: e.matmul(BK[bi][:, :], lhsT=w_[:, tt * 128:(tt + 1) * 128], rhs=vbb[slot][:, ci, dh * 512:(dh + 1) * 512],
                                                                                                  start=(c == 0), stop=(c == 127)))(w_, slot, ci, tt, dh, bi, c),
                                 reads=[f"vbb{slot}", f"wT{c % 2}"], writes=[f"bk{bi}"])

                for c in range(128):
                    if c == 64:
                        build_flush()
                        if gi + 1 < len(groups):
                            pend_a.extend(build_pieces(gi + 1, 0))
                    if c % CPD == 0:
                        emit_dma(c)
                    emit_m1(c)
                    if c > 0:
                        emit_m2(c - 1)
                    emit_act(c)
                    build_step(per_slot)
                emit_m2(127)
                build_flush()
                for tt in range(gn // 128):
                    for dh in range(2):
                        bi = tt * 2 + dh
                        if bi % 2 == 0:
                            P.op("scalar", (lambda tt, dh, bi: lambda e: e.copy(out=peer_sb[:, tt, dh * 512:(dh + 1) * 512], in_=BK[bi][:, :]))(tt, dh, bi), reads=[f"bk{bi}"], writes=[f"peer{tt}"])
                        else:
                            P.op("vector", (lambda tt, dh, bi: lambda e: e.tensor_copy(out=peer_sb[:, tt, dh * 512:(dh + 1) * 512], in_=BK[bi][:, :]))(tt, dh, bi), reads=[f"bk{bi}"], writes=[f"peer{tt}"])
                for j in range(8):
                    for tt in range(gn // 128):
                        P.op("tensor", (lambda j, tt: lambda e: e.transpose(out=BK[j][:, tt * 128:(tt + 1) * 128], in_=peer_sb[:, tt, j * 128:(j + 1) * 128], identity=ident_f[:]))(j, tt),
                             reads=[f"peer{tt}"], writes=bkk(j))
                segs = []
                tcur = g0
                while tcur < g0 + gn:
                    bb = tcur // seq
                    tend = min((bb + 1) * seq, g0 + gn)
                    segs.append((bb, tcur - g0, tend - g0))
                    tcur = tend
                for j in range(8):
                    for (bb, s0, s1) in segs:
                        P.op("vector", (lambda j, bb, s0, s1: lambda e: e.scalar_tensor_tensor(out=x1g[:, j, s0:s1], in0=BK[j][:, s0:s1], scalar=dcol(D_ADA + (40 + j) * nseq + bb),
                                                                                               in1=x1g[:, j, s0:s1], op0=ALU.mult, op1=ALU.add))(j, bb, s0, s1),
                             reads=bkk(j) + ["x1g"], writes=["x1g"])
                P.op("scalar", (lambda gn: lambda e: e.activation(out=sq2[:, :, 0:gn], in_=x1g[:, :, 0:gn], func=AF.Square))(gn), reads=["x1g"], writes=["sq2"])
                for kc in range(8):
                    P.op("tensor", (lambda kc, gn: lambda e: e.matmul(pm[0][:, 0:gn], lhsT=ones_bf[:], rhs=sq2[:, kc, 0:gn], start=(kc == 0), stop=(kc == 7)))(kc, gn), reads=["sq2"], writes=["bk6"])
                P.op("scalar", (lambda gn: lambda e: e.activation(out=rstd2[:, 0:gn], in_=pm[0][:, 0:gn], func=AF.Ln, bias=eps_t[:], scale=1.0 / D))(gn), reads=["bk6"], writes=["rstd2"])
                P.op("scalar", (lambda gn: lambda e: e.activation(out=rstd2[:, 0:gn], in_=rstd2[:, 0:gn], func=AF.Exp, scale=-0.5))(gn), reads=["rstd2"], writes=["rstd2"])
                P.op("vector", (lambda gn: lambda e: e.tensor_tensor(out=x1g[:, :, 0:gn], in0=x1g[:, :, 0:gn], in1=rstd2[:, 0:gn].unsqueeze(1).to_broadcast([128, 8, gn]), op=ALU.mult))(gn),
                     reads=["x1g", "rstd2"], writes=["x1g"])
                for j in range(8):
                    P.op("scalar", (lambda j, gn: lambda e: e.activation(out=x1g[:, j, 0:gn], in_=x1g[:, j, 0:gn], func=AF.Copy, scale=pcol(P_GF + j)))(j, gn), reads=["x1g"], writes=["x1g"])
                P.op("sync", (lambda g0, gn: lambda e: e.dma_start(out=yT[:, g0:g0 + gn].rearrange("(c p) t -> p c t", p=128), in_=x1g[:, :, 0:gn]))(g0, gn), reads=["x1g"], dma="yst")
            import os as _os2
            _mxb = _os2.environ.get("PROG_MAXOPS_B")
            print("phase B ops", len(P.ops))
            if _mxb:
                for _i, _o in enumerate(P.ops[:int(_mxb)]):
                    if _i >= int(_mxb) - 14:
                        print("OP", _i, _o["eng"], _o["dma"], "r", _o["r"], "w", _o["w"], "deps", sorted(_o["deps"]))
            P.emit(int(_mxb) if _mxb else None)
    return nc


def host_layout(inputs, core, seq, nseq):
    x = np.asarray(inputs["x"])
    c = np.asarray(inputs["c"])
    b0 = core * nseq
    ntok = seq * nseq
    xT = np.ascontiguousarray(x[b0:b0 + nseq, :seq].reshape(ntok, D).T)
    cT = np.ascontiguousarray(c[b0:b0 + nseq].reshape(nseq, 8, 128).transpose(2, 1, 0))
    return xT, cT


def shared_layout(inputs):
    f = lambda k: np.asarray(inputs[k], dtype=np.float32)
    kcp = lambda w: np.ascontiguousarray(w.reshape(8, 128, w.shape[1]).transpose(1, 0, 2))
    colT = lambda v, n: v.reshape(n, 128).T
    w_in = f("w_in")
    b_in = f("b_in")
    q_c = list(range(0, 512))
    k0, k1 = list(range(512, 576)), list(range(576, 640))
    v_c = list(range(640, 768))
    B_c = list(range(768, 1280))
    C_c = list(range(1280, 1792))
    U_c = list(range(1792, 2304))
    order = q_c + k0 + k0 + k1 + k1 + B_c + C_c + U_c + v_c
    w_in_r = kcp(w_in[:, order])
    b_in_r = b_in[order]
    prm = np.zeros((128, NP), np.float32)
    prm[:, P_BADA:P_BADA + 48] = colT(f("b_ada"), 48)
    prm[:, P_G1:P_G1 + 8] = colT(f("norm1_g"), 8)
    prm[:, P_G2:P_G2 + 8] = colT(f("norm2_g"), 8)
    prm[:, P_GF:P_GF + 8] = colT(f("final_g"), 8)
    prm[:, P_BOUT:P_BOUT + 8] = colT(f("b_out"), 8)
    prm[:, P_BIN:P_BIN + 18] = colT(b_in_r[:2304], 18)
    prm[:, P_AG:P_AG + 4] = colT(f("attn_out_g"), 4)
    prm[:, P_CG:P_CG + 4] = colT(f("conv_out_g"), 4)
    cw = f("conv_w")
    for tap in range(3):
        prm[:, P_CW + tap * 4:P_CW + tap * 4 + 4] = colT(cw[tap], 4)
    prm[:, P_SINK:P_SINK + 4] = np.repeat(f("attn_sinks"), 64).reshape(4, 128).T
    prm[:, P_BV:P_BV + 128] = np.broadcast_to(b_in_r[2304:2432][None, :], (128, 128))
    k1_, k2_ = f("peer_keys1"), f("peer_keys2")
    keys = np.stack([k1_, k2_], axis=1).reshape(16, 128, 128)
    keysT = np.ascontiguousarray(keys.transpose(2, 0, 1))
    U = f("peer_u")
    V = f("peer_v")
    UT = np.ascontiguousarray(U.reshape(128, 128, D).transpose(2, 1, 0).reshape(D, 16384))
    Vr = np.ascontiguousarray(V.reshape(128, 128, D).transpose(1, 0, 2).reshape(16384, D))
    return dict(w_ada=kcp(f("w_ada")), prm=prm, w_in=w_in_r, w_out=kcp(f("w_out")), w_q=kcp(f("w_query")), keysT=keysT, UT=UT, Vr=Vr)


def run(inputs, seq=2048, nseq=2, n_cores=N_CORES, stop_after=9):
    nc = build_nc(seq, nseq, stop_after=stop_after)
    shared = shared_layout(inputs)
    in_maps = []
    for core in range(n_cores):
        xT, cT = host_layout(inputs, core, seq, nseq)
        m = dict(shared)
        m["xT"] = xT
        m["cT"] = cT
        in_maps.append(m)
    res = run_bass_kernel_spmd(nc, in_maps, core_ids=list(range(n_cores)))
    outs = [np.asarray(r["yT"]).T.reshape(nseq, seq, D) for r in res.results]
    return np.concatenate(outs, axis=0).astype(np.float32)


def kernel(**inputs):
    return run(inputs)
```
